# Optimizing a Trainium2 kernel written in Bass

```python
import jax, jax.numpy as jnp
from jax import lax
import numpy as np

D_MODEL = 1024
BATCH = 8
SEQ = 4096
DEPTH = 2

HEAD_DIM = 64
CONV_WIDTH = D_MODEL // 4
RET_WIDTH = D_MODEL // 4
ATT_WIDTH = D_MODEL // 2
MIX_WIDTH = CONV_WIDTH + RET_WIDTH + ATT_WIDTH
RET_HEADS = RET_WIDTH // HEAD_DIM
ATT_HEADS = ATT_WIDTH // HEAD_DIM
CONV_K = 31
RET_CHUNK = 128
MOBA_BLOCK = 256
MOBA_TOPK = 3
Q_CHUNK = 32
RMS_EPS = 1e-6
LN_EPS = 1e-5
NEG = -1e30
IN_SIZES = (CONV_WIDTH, CONV_WIDTH, CONV_WIDTH,
            RET_WIDTH, RET_WIDTH, RET_WIDTH, RET_WIDTH,
            ATT_WIDTH, ATT_WIDTH, ATT_WIDTH, ATT_WIDTH)
IN_WIDTH = sum(IN_SIZES)
SPLIT_POINTS = tuple(int(s) for s in np.cumsum(IN_SIZES)[:-1])

kernel_name = "hymba_conv_retention_moba_hybrid"


def rmsnorm(x, g):
    xf = x.astype(jnp.float32)
    y = xf * lax.rsqrt(jnp.mean(xf * xf, axis=-1, keepdims=True) + RMS_EPS)
    return (y * g.astype(jnp.float32)).astype(x.dtype)


def split_heads(t, n_heads):
    b, s, _ = t.shape
    return t.reshape(b, s, n_heads, -1).transpose(0, 2, 1, 3)


def merge_heads(t):
    b, n, s, d = t.shape
    return t.transpose(0, 2, 1, 3).reshape(b, s, n * d)


def conformer_conv(val, glu_gate, conv_w, conv_b, ln_g, ln_b, pw_w, pw_b):
    u = val * jax.nn.sigmoid(glu_gate)
    y = lax.conv_general_dilated(u, conv_w[:, None, :], window_strides=(1,),
                                 padding=[(CONV_K - 1, 0)],
                                 dimension_numbers=('NWC', 'WIO', 'NWC'),
                                 feature_group_count=CONV_WIDTH) + conv_b
    yf = y.astype(jnp.float32)
    mu = jnp.mean(yf, axis=-1, keepdims=True)
    var = jnp.mean(jnp.square(yf - mu), axis=-1, keepdims=True)
    yn = ((yf - mu) * lax.rsqrt(var + LN_EPS) * ln_g + ln_b).astype(val.dtype)
    return jax.nn.silu(yn) @ pw_w + pw_b


def retention(q, k, v):
    out_dtype = q.dtype
    q, k, v = (t.astype(jnp.float32) for t in (q, k, v))
    b, h, s, d = q.shape
    c = RET_CHUNK
    n = s // c
    k = k * (d ** -0.5)
    log_g = jnp.log(1.0 - jnp.exp2(-5.0 - jnp.arange(h, dtype=jnp.float32)))
    i = jnp.arange(c, dtype=jnp.float32)
    diff = i[:, None] - i[None, :]
    dec = jnp.where(diff >= 0, jnp.exp(log_g[:, None, None] * jnp.maximum(diff, 0.0)), 0.0)
    qc = q.reshape(b, h, n, c, d)
    kc = k.reshape(b, h, n, c, d)
    vc = v.reshape(b, h, n, c, d)
    scores = jnp.einsum('bhnid,bhnjd->bhnij', qc, kc) * dec[None, :, None]
    intra = jnp.einsum('bhnij,bhnje->bhnie', scores, vc)
    zeta = jnp.exp(log_g[:, None] * (c - 1 - i))
    kv = jnp.einsum('bhnjd,bhnje->nbhde', kc * zeta[None, :, None, :, None], vc)
    g_chunk = jnp.exp(log_g * c)[None, :, None, None]

    def step(state, kv_n):
        return kv_n + g_chunk * state, state

    _, r_prev = lax.scan(step, jnp.zeros((b, h, d, d), jnp.float32), kv)
    xi = jnp.exp(log_g[:, None] * (i + 1.0))
    cross = jnp.einsum('bhnid,nbhde->bhnie', qc, r_prev) * xi[None, :, None, :, None]
    o = (intra + cross).reshape(b, h, s, d)
    mu = jnp.mean(o, axis=-1, keepdims=True)
    var = jnp.mean(jnp.square(o - mu), axis=-1, keepdims=True)
    return ((o - mu) * lax.rsqrt(var + LN_EPS)).astype(out_dtype)


def moba_attention(q, k, v):
    out_dtype = q.dtype
    q, k, v = (t.astype(jnp.float32) for t in (q, k, v))
    b, h, s, d = q.shape
    blk = MOBA_BLOCK
    nblk = -(-s // blk)
    sp = nblk * blk
    kpad = jnp.pad(k, ((0, 0), (0, 0), (0, sp - s), (0, 0)))
    vpad = jnp.pad(v, ((0, 0), (0, 0), (0, sp - s), (0, 0)))
    kb = kpad.reshape(b, h, nblk, blk, d)
    vb = vpad.reshape(b, h, nblk, blk, d)
    scale = d ** -0.5
    slopes = jnp.exp2(-8.0 * (jnp.arange(h, dtype=jnp.float32) + 1.0) / h)
    k_mean = jnp.mean(kb, axis=3)
    gate = jnp.einsum('bhsd,bhnd->bhsn', q, k_mean)
    q_blk = jnp.arange(s) // blk
    past = jnp.arange(nblk)[None, :] < q_blk[:, None]
    gate = jnp.where(past[None, None], gate, NEG)
    topk = min(MOBA_TOPK, nblk)
    _, sel = lax.top_k(gate, topk)
    bi = jnp.arange(b)[:, None, None, None]
    hi = jnp.arange(h)[None, :, None, None]
    n_chunks = s // Q_CHUNK

    def body(ci):
        t0 = ci * Q_CHUNK
        qc = lax.dynamic_slice_in_dim(q, t0, Q_CHUNK, axis=2)
        ic = lax.dynamic_slice_in_dim(sel, t0, Q_CHUNK, axis=2)
        t = (t0 + jnp.arange(Q_CHUNK)).astype(jnp.float32)
        j = t0 // blk
        k_sel = kb[bi, hi, ic]
        v_sel = vb[bi, hi, ic]
        s_sel = jnp.einsum('bhcd,bhckld->bhckl', qc, k_sel) * scale
        pos_sel = (ic[..., None] * blk + jnp.arange(blk)).astype(jnp.float32)
        bias_sel = -slopes[None, :, None, None, None] * (t[None, None, :, None, None] - pos_sel)
        valid = (ic < j)[..., None]
        l_sel = jnp.where(valid, s_sel + bias_sel, NEG).reshape(b, h, Q_CHUNK, topk * blk)
        k_own = lax.dynamic_slice_in_dim(kpad, j * blk, blk, axis=2)
        v_own = lax.dynamic_slice_in_dim(vpad, j * blk, blk, axis=2)
        s_own = jnp.einsum('bhcd,bhld->bhcl', qc, k_own) * scale
        pos_own = (j * blk + jnp.arange(blk)).astype(jnp.float32)
        dist = t[:, None] - pos_own[None, :]
        l_own = jnp.where((dist >= 0)[None, None],
                          s_own - slopes[None, :, None, None] * dist[None, None], NEG)
        p = jax.nn.softmax(jnp.concatenate([l_sel, l_own], axis=-1), axis=-1)
        p_sel = p[..., :topk * blk].reshape(b, h, Q_CHUNK, topk, blk)
        p_own = p[..., topk * blk:]
        return (jnp.einsum('bhckl,bhckld->bhcd', p_sel, v_sel)
                + jnp.einsum('bhcl,bhld->bhcd', p_own, v_own))

    out = lax.map(body, jnp.arange(n_chunks))
    out = out.transpose(1, 2, 0, 3, 4).reshape(b, h, s, d)
    return out.astype(out_dtype)


def setup_inputs(seed: int = 0) -> dict:
    key = jax.random.key(seed)
    ks = jax.random.split(key, 12)
    f32 = jnp.float32
    x = jax.random.normal(ks[0], (BATCH, SEQ, D_MODEL), f32)
    norm_g = 1.0 + 0.05 * jax.random.normal(ks[1], (DEPTH, D_MODEL), f32)
    w_in = jax.random.normal(ks[2], (DEPTH, D_MODEL, IN_WIDTH), f32) * D_MODEL ** -0.5
    conv_w = jax.random.normal(ks[3], (DEPTH, CONV_K, CONV_WIDTH), f32) * CONV_K ** -0.5
    conv_b = 0.02 * jax.random.normal(ks[4], (DEPTH, CONV_WIDTH), f32)
    conv_ln_g = 1.0 + 0.05 * jax.random.normal(ks[5], (DEPTH, CONV_WIDTH), f32)
    conv_ln_b = 0.02 * jax.random.normal(ks[6], (DEPTH, CONV_WIDTH), f32)
    conv_pw_w = jax.random.normal(ks[7], (DEPTH, CONV_WIDTH, CONV_WIDTH), f32) * CONV_WIDTH ** -0.5
    conv_pw_b = 0.02 * jax.random.normal(ks[8], (DEPTH, CONV_WIDTH), f32)
    w_out = jax.random.normal(ks[9], (DEPTH, MIX_WIDTH, D_MODEL), f32) * MIX_WIDTH ** -0.5
    final_g = 1.0 + 0.05 * jax.random.normal(ks[10], (D_MODEL,), f32)
    return {"x": x, "norm_g": norm_g, "w_in": w_in, "conv_w": conv_w, "conv_b": conv_b,
            "conv_ln_g": conv_ln_g, "conv_ln_b": conv_ln_b, "conv_pw_w": conv_pw_w,
            "conv_pw_b": conv_pw_b, "w_out": w_out, "final_g": final_g}


def reference(x, norm_g, w_in, conv_w, conv_b, conv_ln_g, conv_ln_b, conv_pw_w, conv_pw_b,
              w_out, final_g):
    for layer in range(DEPTH):
        hn = rmsnorm(x, norm_g[layer])
        proj = hn @ w_in[layer]
        (a_val, a_glu, a_gate, r_q, r_k, r_v, r_gate,
         m_q, m_k, m_v, m_gate) = jnp.split(proj, SPLIT_POINTS, axis=-1)
        y_a = conformer_conv(a_val, a_glu, conv_w[layer], conv_b[layer], conv_ln_g[layer],
                             conv_ln_b[layer], conv_pw_w[layer], conv_pw_b[layer])
        y_a = y_a * jax.nn.silu(a_gate)
        y_r = merge_heads(retention(split_heads(r_q, RET_HEADS), split_heads(r_k, RET_HEADS),
                                    split_heads(r_v, RET_HEADS)))
        y_r = y_r * jax.nn.silu(r_gate)
        y_m = merge_heads(moba_attention(split_heads(m_q, ATT_HEADS), split_heads(m_k, ATT_HEADS),
                                         split_heads(m_v, ATT_HEADS)))
        y_m = y_m * jax.nn.silu(m_gate)
        y = jnp.concatenate([y_a, y_r, y_m], axis=-1) @ w_out[layer]
        x = x + y
    return rmsnorm(x, final_g)
```

```python
import os
import numpy as np
import ml_dtypes
from contextlib import ExitStack
import concourse.bass as bass
import concourse.mybir as mybir
from concourse.bass_utils import run_bass_kernel_spmd

F32 = mybir.dt.float32
BF16 = mybir.dt.bfloat16
ALU = mybir.AluOpType
AF = mybir.ActivationFunctionType

D = 1024
INW = 3840
NEG = -1.0e30
MASKV = -30000.0
RMS_EPS = 1e-6
LN_EPS = 1e-5
SCALE = 0.125


class Buf:
    __slots__ = ("name", "w", "r", "excl")

    def __init__(self, name, excl=False):
        self.name = name
        self.w = None
        self.r = {}
        self.excl = excl


class Sched:
    def __init__(self, nc, es, n_dma=12):
        self.nc = nc
        self.sems = []
        self.h = {"pe": nc.tensor, "act": nc.scalar, "dve": nc.vector, "pool": nc.gpsimd, "sp": nc.sync}
        self.engs = {}
        for k in self.h:
            idx = self._sem(es, "s_" + k)
            self.engs[k] = {"sem": idx, "count": 0, "seen": {}}
        self.pool = {}
        self.rr = {}
        for q in ("sp", "pool", "act"):
            self.pool[q] = [{"idx": self._sem(es, "d_%s%d" % (q, i)), "val": 0} for i in range(n_dma)]
            self.rr[q] = 0
        self.nops = 0
        self.limit = int(os.environ.get("KLIMIT", "0")) or None

    def _sem(self, es, name):
        self.sems.append(es.enter_context(self.nc.semaphore(name)))
        return len(self.sems) - 1

    def op(self, eng, fn, reads=(), writes=(), dma=False):
        if self.limit is not None and self.nops >= self.limit:
            return None
        e = self.engs[eng]
        waits = {}

        def need(src, sidx, val, raw):
            if src == eng and eng == "pe":
                return
            if e["seen"].get(sidx, 0) >= val:
                return
            if waits.get(sidx, 0) < val:
                waits[sidx] = val

        for b in reads:
            if b.w is not None:
                need(b.w[0], b.w[1], b.w[2], True)
            if b.excl:
                for sidx, (src, val) in b.r.items():
                    if src != eng:
                        need(src, sidx, val, False)
        for b in writes:
            if b.w is not None:
                need(b.w[0], b.w[1], b.w[2], False)
            for sidx, (src, val) in b.r.items():
                need(src, sidx, val, False)
        if dma:
            pl = self.pool[eng]
            slot = pl[self.rr[eng] % len(pl)]
            self.rr[eng] += 1
            if slot["val"] > 0:
                need(None, slot["idx"], slot["val"], False)
            slot["val"] += 16
            sig = (None, slot["idx"], slot["val"])
            inc = 16
        else:
            e["count"] += 1
            sig = (eng, e["sem"], e["count"])
            inc = 1
        h = self.h[eng]
        for sidx, val in waits.items():
            e["seen"][sidx] = val
            h.wait_ge(self.sems[sidx], val)
        ins = fn(h)
        if os.environ.get("KTRACE"):
            import inspect
            fr = inspect.stack()[1]
            print("OP", self.nops, eng, fr.lineno, (fr.code_context or [""])[0].strip()[:90])
        ins.then_inc(self.sems[sig[1]], inc)
        self.nops += 1
        for b in writes:
            b.w = sig
            b.r = {}
        for b in reads:
            if b in writes:
                continue
            cur = b.r.get(sig[1])
            if cur is None or cur[1] < sig[2]:
                b.r[sig[1]] = (sig[0], sig[2])
        return sig

    def barrier(self):
        sigs = []
        for k, e in self.engs.items():
            if e["count"] > 0:
                sigs.append((e["sem"], e["count"]))
        for q, pl in self.pool.items():
            for slot in pl:
                if slot["val"] > 0:
                    sigs.append((slot["idx"], slot["val"]))
        for k, e in self.engs.items():
            h = self.h[k]
            for sidx, val in sigs:
                if e["seen"].get(sidx, 0) >= val:
                    continue
                e["seen"][sidx] = val
                h.wait_ge(self.sems[sidx], val)


def build_program(S, DEPTH):
    NT = S // 128
    NCH = S // 512
    nc = bass.Bass("TRN2", target_bir_lowering=False)

    def dr(name, shape, dt, kind="ExternalInput"):
        return nc.dram_tensor(name, shape, dt, kind=kind).ap()

    x_in = dr("x", [S, D], F32)
    out = dr("out", [S, D], F32, "ExternalOutput")
    x1 = dr("x1s", [S, D], F32, "Internal")
    ym_d = dr("ym_d", [4, 128, S], BF16, "Internal")
    w_in = dr("w_in", [DEPTH, D, INW], F32)
    w_out = dr("w_out", [DEPTH, D, D], F32)
    pw_w = dr("pw_w", [DEPTH, 256, 256], F32)
    ng = dr("ng", [DEPTH, 128, 8], F32)
    cwT = dr("cwT", [DEPTH, 128, 2, 31], F32)
    cvec = dr("cvec", [DEPTH, 128, 4, 2], F32)
    fg = dr("fg", [128, D], F32)
    d_ident = dr("c_ident", [128, 128], BF16)
    d_causal = dr("c_causal", [128, 128], BF16)
    d_kaug = dr("c_kaug", [20, S], BF16)
    d_qaug = dr("c_qaug", [4, 8, S], BF16)
    d_dec = dr("c_dec", [128, 4, 128], F32)
    d_zeta = dr("c_zeta", [128, 4, 1], F32)
    d_xi = dr("c_xi", [128, 2, 512], F32)
    d_gv = dr("c_gv", [128, 2, 1], F32)
    d_bones = dr("c_bones", [128, 128], BF16)
    d_o256 = dr("c_o256", [128, 128], BF16)

    with ExitStack() as es:
        S_ = Sched(nc, es)
        op = S_.op

        uniq = [0]

        def sb(stack, name, shape, dt):
            uniq[0] += 1
            return stack.enter_context(nc.sbuf_tensor("%s_%d" % (name, uniq[0]), shape, dt))

        def ps(name, shape, dt):
            return es.enter_context(nc.psum_tensor(name, shape, dt))

        ident = sb(es, "ident", [128, 128], BF16)
        causal = sb(es, "causal", [128, 128], BF16)
        bones = sb(es, "bones", [128, 128], BF16)
        o256 = sb(es, "o256", [128, 128], BF16)
        B_const = Buf("const")
        B_WM = Buf("W_M")

        tps = ps("tps", [128, 1024], BF16)
        pj = [ps("pj%d" % i, [128, 512], F32) for i in range(2)]
        st = [ps("st%d" % i, [128, 512], F32) for i in range(3)]
        acc = [ps("acc%d" % i, [128, 512], F32) for i in range(2)]
        B_tps = Buf("tps", True)
        B_pj = [Buf("pj0", True), Buf("pj1", True)]
        B_st = [Buf("st%d" % i, True) for i in range(3)]
        B_acc = [Buf("acc0", True), Buf("acc1", True)]

        for t, d in ((ident, d_ident), (causal, d_causal), (bones, d_bones), (o256, d_o256)):
            op("sp", lambda e, t=t, d=d: e.dma_start(out=t[:, :], in_=d[:, :]), writes=[B_const], dma=True)
        S_.barrier()

        def load_norm(src, T, xt_t, B_xt, hn, B_hn, stat, B_stat):
            op("sp", lambda e: e.dma_start(out=xt_t[:, :], in_=src[T * 128:(T + 1) * 128, :]),
               writes=[B_xt], dma=True)
            op("act", lambda e: e.activation(out=hn[:, :], in_=xt_t[:, :], func=AF.Square,
                                             accum_out=stat[:, 0:1]),
               reads=[B_xt], writes=[B_hn, B_stat])
            op("act", lambda e: e.activation(out=stat[:, 1:2], in_=stat[:, 0:1], func=AF.Ln,
                                             scale=1.0 / D, bias=eps_rms[:, 0:1]),
               reads=[B_stat], writes=[B_stat])
            op("act", lambda e: e.activation(out=stat[:, 2:3], in_=stat[:, 1:2], func=AF.Exp, scale=-0.5),
               reads=[B_stat], writes=[B_stat])
            op("dve", lambda e: e.tensor_scalar(out=hn[:, :], in0=xt_t[:, :], scalar1=stat[:, 2:3],
                                                scalar2=None, op0=ALU.mult),
               reads=[B_xt, B_stat], writes=[B_hn])

        def transpose_hn(hn, B_hn, hnT, B_hnT_tt, tt):
            for kc in range(8):
                op("pe", lambda e, kc=kc: e.transpose(out=tps[:, kc * 128:(kc + 1) * 128],
                                                      in_=hn[:, kc * 128:(kc + 1) * 128], identity=ident[:, :]),
                   reads=[B_hn], writes=[B_tps])
            op("dve", lambda e: e.tensor_copy(out=hnT[:, :, tt * 128:(tt + 1) * 128],
                                              in_=tps[:, :].rearrange("p (k t) -> p k t", k=8)),
               reads=[B_tps], writes=[B_hnT_tt])

        def load_norm_transpose(src, T, xt_t, B_xt, hn, B_hn, stat, B_stat, hnT, B_hnT_tt, tt):
            load_norm(src, T, xt_t, B_xt, hn, B_hn, stat, B_stat)
            transpose_hn(hn, B_hn, hnT, B_hnT_tt, tt)

        def sigmoid_recip(psrc, B_psrc, tmpt, B_tmp, rows=slice(0, 128)):
            op("act", lambda e: e.activation(out=tmpt[rows, :], in_=psrc, func=AF.Exp, scale=-1.0),
               reads=[B_psrc], writes=[B_tmp])
            op("dve", lambda e: e.tensor_scalar(out=tmpt[rows, :], in0=tmpt[rows, :], scalar1=1.0,
                                                scalar2=None, op0=ALU.add),
               reads=[B_tmp], writes=[B_tmp])
            op("dve", lambda e: e.reciprocal(out=tmpt[rows, :], in_=tmpt[rows, :]),
               reads=[B_tmp], writes=[B_tmp])

        eps_rms = sb(es, "eps_rms", [128, 3], F32)
        op("dve", lambda e: e.memset(eps_rms[:, 2:3], 1.0), writes=[B_const])
        op("dve", lambda e: e.memset(eps_rms[:, 0:1], RMS_EPS), writes=[B_const])
        op("dve", lambda e: e.memset(eps_rms[:, 1:2], LN_EPS), writes=[B_const])
        S_.barrier()

        def load_weight_cols(stack, l, col0, ncols, Wdst, B_W, tagname, extra=None):
            NSTG = 4
            stg = [sb(stack, "stg%s%d" % (tagname, i), [128, 2048], F32) for i in range(NSTG)]
            B_stg = [Buf("stg%d" % i) for i in range(NSTG)]
            ngt = sb(stack, "ngt" + tagname, [128, 8], F32)
            B_ng = Buf("ng")
            op("sp", lambda e: e.dma_start(out=ngt[:, :], in_=ng[l, :, :]), writes=[B_ng], dma=True)
            h1 = (ncols // 2 + 127) // 128 * 128
            def issue(kc):
                b = kc % NSTG
                op("sp", lambda e: e.dma_start(out=stg[b][:, 0:h1],
                                               in_=w_in[l, kc * 128:(kc + 1) * 128, col0:col0 + h1]),
                   writes=[B_stg[b]], dma=True)
                op("pool", lambda e: e.dma_start(out=stg[b][:, h1:ncols],
                                                 in_=w_in[l, kc * 128:(kc + 1) * 128, col0 + h1:col0 + ncols]),
                   writes=[B_stg[b]], dma=True)

            for kc in range(NSTG):
                issue(kc)
            for kc in range(8):
                b = kc % NSTG
                op("dve" if kc % 2 == 0 else "act",
                   (lambda e, kc=kc, b=b: e.tensor_scalar(out=Wdst[:, kc, 0:ncols], in0=stg[b][:, 0:ncols],
                                                          scalar1=ngt[:, kc:kc + 1], scalar2=None, op0=ALU.mult))
                   if kc % 2 == 0 else
                   (lambda e, kc=kc, b=b: e.activation(out=Wdst[:, kc, 0:ncols], in_=stg[b][:, 0:ncols],
                                                       func=AF.Copy, scale=ngt[:, kc:kc + 1])),
                   reads=[B_stg[b], B_ng], writes=[B_W])
                if kc + NSTG < 8:
                    issue(kc + NSTG)
                if extra is not None:
                    extra(kc)

        for l in range(DEPTH):
            src = x_in if l == 0 else x1
            last = (l == DEPTH - 1)
            with ExitStack() as ph:
                W_M = sb(ph, "W_M", [128, 8, 2048], BF16)
                with ExitStack() as wl:
                    load_weight_cols(wl, l, 1792, 2048, W_M, B_WM, "m")
                    S_.barrier()
                K_aug = sb(ph, "K_aug", [84, 8, S], BF16)
                V_ext = sb(ph, "V_ext", [128, NT, 768], BF16)
                xt = [sb(ph, "xt%d" % i, [128, D], F32) for i in range(1)]
                hn = sb(ph, "hn", [128, D], BF16)
                stat = [sb(ph, "stat%d" % i, [128, 4], F32) for i in range(2)]
                hnT2 = [sb(ph, "hnT%d" % i, [128, 8, 512], BF16) for i in range(2)]
                Q_aug2 = [sb(ph, "Q_aug%d" % i, [84, 8, 512], BF16) for i in range(2)]
                gsl2 = [sb(ph, "gsl%d" % i, [128, 4, 512], BF16) for i in range(2)]
                pT = [sb(ph, "pT%d" % i, [128, 512], BF16) for i in range(3)]
                rec = sb(ph, "rt", [128, 512], F32)
                tmp = rec
                gtmp = sb(ph, "gtmp", [128, 512], F32)
                gx = sb(ph, "gx", [128, 512], F32)
                B_gx = Buf("gx")
                ymt = [sb(ph, "ymt%d" % i, [128, 512], BF16) for i in range(2)]
                ksum = sb(ph, "ksum", [64, 8, 16], F32)
                kmean = sb(ph, "kmean", [64, 8, 16], BF16)
                gate_sb4 = [sb(ph, "gate_sb%d" % i, [128, 8, 16], F32) for i in range(4)]
                top84 = [sb(ph, "top8%d" % i, [128, 8, 8], F32) for i in range(4)]
                mask_tm4 = [sb(ph, "mask_tm%d" % i, [128, 8, 16], BF16) for i in range(4)]

                B_xt = [Buf("xt0")]
                B_hn = Buf("hn")
                B_stat = [Buf("stat0"), Buf("stat1")]
                B_hnT2 = [[Buf("hnT%d_%d" % (q, i)) for i in range(4)] for q in range(2)]
                B_qq2 = [[Buf("qq%d_%d" % (q, h)) for h in range(8)] for q in range(2)]
                B_qm2 = [[Buf("qm%d_%d" % (q, t)) for t in range(4)] for q in range(2)]
                B_qc2 = [Buf("qc0"), Buf("qc1")]
                B_ka = [[Buf("ka%d_%d" % (h, c)) for c in range(NCH)] for h in range(8)]
                B_ve = [Buf("ve%d" % T) for T in range(NT)]
                B_gsl2 = [[Buf("gsl%d_%d" % (q, p)) for p in range(4)] for q in range(2)]
                B_pT = [Buf("pT%d" % i) for i in range(3)]
                B_rec = Buf("rt")
                B_tmp = B_rec
                B_gtmp = Buf("gtmp")
                B_ymt = [Buf("ymt0"), Buf("ymt1")]
                B_ksum = Buf("ksum")
                B_kmean = Buf("kmean")
                B_gate4 = [Buf("gate_sb%d" % i) for i in range(4)]
                B_top84 = [Buf("top8%d" % i) for i in range(4)]
                B_mask4 = [Buf("mask_tm%d" % i) for i in range(4)]

                for h in range(8):
                    op("sp" if h % 2 == 0 else "pool",
                       lambda e, h=h: e.dma_start(out=K_aug[64:84, h, :], in_=d_kaug[:, :]),
                       writes=[B_const], dma=True)
                for p in range(4):
                    op("dve", lambda e, p=p: e.memset(V_ext[:, :, p * 192 + 64:p * 192 + 128], 1.0),
                       writes=[B_const])
                for q in range(2):
                    op("dve", lambda e, q=q: e.memset(Q_aug2[q][64:80, :, :], 0.0), writes=[B_const])
                for i in range(4):
                    op("dve", lambda e, i=i: e.memset(gate_sb4[i][:, :, :], NEG), writes=[B_const])
                    op("dve", lambda e, i=i: e.memset(mask_tm4[i][:, :, :], 0.0), writes=[B_const])
                op("dve", lambda e: e.memset(ksum[:, :, :], 0.0), writes=[B_const])
                S_.barrier()

                ctr = {"pj": 0}

                def nextpj():
                    ctr["pj"] += 1
                    return ctr["pj"] % 2

                def preamble_units(c):
                    par = c % 2
                    c0 = c * 512
                    hnT = hnT2[par]
                    B_hnT = B_hnT2[par]
                    Q_aug = Q_aug2[par]
                    units = []

                    def u_load1(tt):
                        T = 4 * c + tt
                        b = T % 2
                        load_norm(src, T, xt[0], B_xt[0], hn, B_hn, stat[b], B_stat[b])

                    def u_load2(tt):
                        transpose_hn(hn, B_hn, hnT, B_hnT[tt], tt)

                    def u_qc():
                        op("sp", lambda e: e.dma_start(out=Q_aug[80:84, :, :], in_=d_qaug[:, :, c0:c0 + 512]),
                           writes=[B_qc2[par]], dma=True)

                    def u_q(p):
                        pb = nextpj()
                        for kc in range(8):
                            op("pe", lambda e, kc=kc: e.matmul(
                                pj[pb][:, :], lhsT=W_M[:, kc, p * 128:(p + 1) * 128], rhs=hnT[:, kc, :],
                                start=(kc == 0), stop=(kc == 7)),
                               reads=[B_WM] + B_hnT, writes=[B_pj[pb]])
                        for hh in range(2):
                            op("dve", lambda e, hh=hh: e.tensor_copy(out=Q_aug[0:64, 2 * p + hh, :],
                                                                     in_=pj[pb][hh * 64:(hh + 1) * 64, :]),
                               reads=[B_pj[pb]], writes=[B_qq2[par][2 * p + hh]])

                    def u_k(p):
                        pb = nextpj()
                        for kc in range(8):
                            op("pe", lambda e, kc=kc: e.matmul(
                                pj[pb][:, :], lhsT=W_M[:, kc, 512 + p * 128:512 + (p + 1) * 128], rhs=hnT[:, kc, :],
                                start=(kc == 0), stop=(kc == 7)),
                               reads=[B_WM] + B_hnT, writes=[B_pj[pb]])
                        for hh in range(2):
                            h = 2 * p + hh
                            op("dve", lambda e, hh=hh, h=h: e.tensor_copy(out=K_aug[0:64, h, c0:c0 + 512],
                                                                          in_=pj[pb][hh * 64:(hh + 1) * 64, :]),
                               reads=[B_pj[pb]], writes=[B_ka[h][c]])
                            op("dve", lambda e, hh=hh, h=h: e.tensor_reduce(
                                out=ksum[0:64, h, 2 * c:2 * c + 2],
                                in_=pj[pb][hh * 64:(hh + 1) * 64, :].rearrange("p (b t) -> p b t", b=2),
                                axis=mybir.AxisListType.X, op=ALU.add),
                               reads=[B_pj[pb]], writes=[B_ksum])

                    def u_v(tt):
                        T = 4 * c + tt
                        pb = nextpj()
                        for kc in range(8):
                            op("pe", lambda e, kc=kc: e.matmul(
                                pj[pb][:, :], lhsT=hnT[:, kc, tt * 128:(tt + 1) * 128], rhs=W_M[:, kc, 1024:1536],
                                start=(kc == 0), stop=(kc == 7)),
                               reads=[B_WM, B_hnT[tt]], writes=[B_pj[pb]])
                        vsrc = pj[pb][:, :].rearrange("p (q two e) -> p q two e", two=2, e=64)
                        vdst = V_ext[:, T, :].rearrange("p (q c) -> p q c", c=192)
                        op("dve", lambda e: e.tensor_copy(out=vdst[:, :, 0:64], in_=vsrc[:, :, 0, :]),
                           reads=[B_pj[pb]], writes=[B_ve[T]])
                        op("dve", lambda e: e.tensor_copy(out=vdst[:, :, 128:192], in_=vsrc[:, :, 1, :]),
                           reads=[B_pj[pb]], writes=[B_ve[T]])

                    def u_kmean():
                        op("dve", lambda e: e.tensor_scalar(out=kmean[:, :, 2 * c:2 * c + 2], in0=ksum[:, :, 2 * c:2 * c + 2],
                                                            scalar1=1.0 / 256.0, scalar2=None, op0=ALU.mult),
                           reads=[B_ksum], writes=[B_kmean])

                    def u_gsilu(p):
                        pb = nextpj()
                        for kc in range(8):
                            op("pe", lambda e, kc=kc: e.matmul(
                                pj[pb][:, :], lhsT=W_M[:, kc, 1536 + p * 128:1536 + (p + 1) * 128], rhs=hnT[:, kc, :],
                                start=(kc == 0), stop=(kc == 7)),
                               reads=[B_WM] + B_hnT, writes=[B_pj[pb]])
                        op("dve", lambda e: e.tensor_copy(out=gx[:, :], in_=pj[pb][:, :]),
                           reads=[B_pj[pb]], writes=[B_gx])
                        sigmoid_recip(pj[pb][:, :], B_pj[pb], gtmp, B_gtmp)
                        op("dve", lambda e: e.tensor_tensor(out=gsl2[par][:, p, :], in0=gx[:, :],
                                                            in1=gtmp[:, :], op=ALU.mult),
                           reads=[B_gx, B_gtmp], writes=[B_gsl2[par][p]])

                    def u_gating(tt):
                        T = 4 * c + tt
                        qb = T // 2
                        gate_sb, top8, mask_tm = gate_sb4[tt], top84[tt], mask_tm4[tt]
                        pb = nextpj()
                        for h in range(8):
                            op("pe", lambda e, h=h: e.matmul(
                                pj[pb][:, h * 16:h * 16 + qb], lhsT=Q_aug[0:64, h, tt * 128:(tt + 1) * 128],
                                rhs=kmean[0:64, h, 0:qb], start=True, stop=True),
                               reads=[B_qq2[par][h], B_kmean], writes=[B_pj[pb]])
                        gsrc = pj[pb][:, 0:128].rearrange("p (h n) -> p h n", n=16)
                        op("dve", lambda e: e.tensor_copy(out=gate_sb[:, :, 0:qb], in_=gsrc[:, :, 0:qb]),
                           reads=[B_pj[pb]], writes=[B_gate4[tt]])
                        for h in range(8):
                            op("dve", lambda e, h=h: e.max(out=top8[:, h, :], in_=gate_sb[:, h, :]),
                               reads=[B_gate4[tt]], writes=[B_top84[tt]])
                        op("dve", lambda e: e.tensor_tensor(
                            out=mask_tm[:, :, 0:qb], in0=gate_sb[:, :, 0:qb],
                            in1=top8[:, :, 2:3].broadcast_to([128, 8, qb]), op=ALU.is_lt),
                           reads=[B_gate4[tt], B_top84[tt]], writes=[B_mask4[tt]])

                    def u_gating2(tt):
                        mask_tm = mask_tm4[tt]
                        for h in range(8):
                            op("pe", lambda e, h=h: e.transpose(out=tps[0:16, h * 128:(h + 1) * 128],
                                                                in_=mask_tm[:, h, :], identity=ident[:, :]),
                               reads=[B_mask4[tt]], writes=[B_tps])
                        op("act", lambda e: e.activation(
                            out=Q_aug[64:80, :, tt * 128:(tt + 1) * 128],
                            in_=tps[0:16, :].rearrange("p (h t) -> p h t", h=8), func=AF.Copy),
                           reads=[B_tps], writes=[B_qm2[par][tt]])

                    for tt in range(4):
                        units.append(lambda tt=tt: u_load1(tt))
                        units.append(lambda tt=tt: u_load2(tt))
                    units.append(u_qc)
                    for p in range(4):
                        units.append(lambda p=p: u_k(p))
                    units.append(u_kmean)
                    for p in range(4):
                        units.append(lambda p=p: u_q(p))
                    for tt in range(4):
                        units.append(lambda tt=tt: u_v(tt))
                    gs_units = [(lambda p=p: u_gsilu(p)) for p in range(4)]
                    if c >= 2:
                        units.append(lambda: u_gating(0))
                        for tt in range(4):
                            if tt + 1 < 4:
                                units.append(lambda tt=tt: u_gating(tt + 1))
                            units.append(gs_units[tt])
                            units.append(lambda tt=tt: u_gating2(tt))
                    else:
                        units.extend(gs_units)
                    return units

                def attention(c, pending):
                    par = c % 2
                    c0 = c * 512
                    Q_aug = Q_aug2[par]
                    npast = 4 * c
                    tiles = []
                    for h in range(8):
                        for kt in range(npast):
                            tiles.append((h, kt, None))
                        for k in range(4):
                            tiles.append((h, 4 * c + k, k))
                    ntl = len(tiles)
                    per_head = npast + 4

                    def emit_qk(i):
                        h, kt, k = tiles[i]
                        sbi = i % 3
                        qreads = [B_qq2[par][h], B_qc2[par]] + B_qm2[par]
                        if k is None:
                            op("pe", lambda e: e.matmul(
                                st[sbi][:, :], lhsT=K_aug[0:84, h, kt * 128:(kt + 1) * 128], rhs=Q_aug[0:84, h, :],
                                start=True, stop=True),
                               reads=[B_ka[h][kt // 4]] + qreads, writes=[B_st[sbi]])
                        else:
                            n = (4 - k) * 128
                            q0 = k * 128
                            op("pe", lambda e: e.matmul(
                                st[sbi][:, 0:128], lhsT=K_aug[0:84, h, kt * 128:(kt + 1) * 128],
                                rhs=Q_aug[0:84, h, q0:q0 + 128], start=True, stop=False),
                               reads=[B_ka[h][c]] + qreads, writes=[B_st[sbi]])
                            op("pe", lambda e: e.matmul(
                                st[sbi][:, 0:128], lhsT=ident[:, :], rhs=causal[:, :], start=False, stop=True),
                               reads=[], writes=[B_st[sbi]])
                            if n > 128:
                                op("pe", lambda e: e.matmul(
                                    st[sbi][:, 128:n], lhsT=K_aug[0:84, h, kt * 128:(kt + 1) * 128],
                                    rhs=Q_aug[0:84, h, q0 + 128:512], start=True, stop=True),
                                   reads=[B_ka[h][c]] + qreads, writes=[B_st[sbi]])

                    def emit_exp(i):
                        h, kt, k = tiles[i]
                        sbi = i % 3
                        pbi = i % 3
                        n = 512 if k is None else (4 - k) * 128
                        op("act", lambda e: e.activation(out=pT[pbi][:, 0:n], in_=st[sbi][:, 0:n],
                                                         func=AF.Exp, scale=SCALE),
                           reads=[B_st[sbi]], writes=[B_pT[pbi]])

                    def emit_pv(i):
                        h, kt, k = tiles[i]
                        p, odd = h // 2, h % 2
                        vcol = p * 192 + (64 if odd else 0)
                        ab = h % 2
                        pbi = i % 3
                        first = (i % per_head == 0)
                        n = 512 if k is None else (4 - k) * 128
                        q0 = 0 if k is None else k * 128
                        op("pe", lambda e: e.matmul(
                            acc[ab][:, q0:512], lhsT=V_ext[:, kt, vcol:vcol + 128], rhs=pT[pbi][:, 0:n],
                            start=first, stop=(k == 3)),
                           reads=[B_ve[kt], B_pT[pbi]], writes=[B_acc[ab]])
                        if k == 3:
                            num = slice(64, 128) if odd else slice(0, 64)
                            den = slice(0, 64) if odd else slice(64, 128)
                            yb = p % 2
                            op("dve", lambda e: e.reciprocal(out=rec[den, :], in_=acc[ab][den, :]),
                               reads=[B_acc[ab]], writes=[B_rec])
                            op("dve", lambda e: e.tensor_tensor(
                                out=tmp[num, :], in0=acc[ab][num, :], in1=rec[den, :], op=ALU.mult),
                               reads=[B_acc[ab], B_rec], writes=[B_tmp])
                            op("pool", lambda e: e.tensor_tensor(
                                out=ymt[yb][num, :], in0=tmp[num, :], in1=gsl2[par][num, p, :], op=ALU.mult),
                               reads=[B_tmp, B_gsl2[par][p]], writes=[B_ymt[yb]])
                            if odd:
                                op("sp", lambda e: e.dma_start(out=ym_d[p, :, c0:c0 + 512], in_=ymt[yb][:, :]),
                                   reads=[B_ymt[yb]], dma=True)

                    nsteps = ntl + 2
                    npend = len(pending)
                    done = 0
                    for i in range(nsteps):
                        if i < ntl:
                            emit_qk(i)
                        if 1 <= i <= ntl:
                            emit_exp(i - 1)
                        if i >= 2:
                            emit_pv(i - 2)
                        want = min(npend, (npend * (i + 1)) // max(1, (nsteps * 17) // 20))
                        while done < want:
                            pending[done]()
                            done += 1
                    while done < npend:
                        pending[done]()
                        done += 1

                for u in preamble_units(0):
                    u()
                for c in range(NCH):
                    nxt = preamble_units(c + 1) if c + 1 < NCH else []
                    attention(c, nxt)
                S_.barrier()

            with ExitStack() as ph:
                W_AB = sb(ph, "W_AB", [128, 8, 1792], BF16)
                Wo = sb(ph, "Wo", [128, 8, D], BF16)
                pw = sb(ph, "pw", [128, 2, 256], BF16)
                diag = sb(ph, "diag", [128, 2, 31, 128], BF16)
                cw_t = sb(ph, "cw_t", [128, 2, 31], F32)
                cv_t = sb(ph, "cv_t", [128, 4, 2], F32)
                dec = sb(ph, "dec", [128, 4, 128], F32)
                zeta = sb(ph, "zeta", [128, 4, 1], F32)
                xi = sb(ph, "xi", [128, 2, 128], F32)
                gv = sb(ph, "gv", [128, 2, 1], F32)
                B_WAB = Buf("W_AB")
                B_c2 = Buf("c2")
                B_cw = Buf("cw")
                with ExitStack() as wl:
                    op("sp", lambda e: e.dma_start(out=cw_t[:, :, :], in_=cwT[l, :, :, :]), writes=[B_cw], dma=True)
                    op("sp", lambda e: e.dma_start(out=cv_t[:, :, :], in_=cvec[l, :, :, :]), writes=[B_c2], dma=True)
                    op("sp", lambda e: e.dma_start(out=dec[:, :, :], in_=d_dec[:, :, :]), writes=[B_c2], dma=True)
                    op("sp", lambda e: e.dma_start(out=zeta[:, :, :], in_=d_zeta[:, :, :]), writes=[B_c2], dma=True)
                    op("sp", lambda e: e.dma_start(out=xi[:, :, :], in_=d_xi[:, :, 0:128]), writes=[B_c2], dma=True)
                    op("sp", lambda e: e.dma_start(out=gv[:, :, :], in_=d_gv[:, :, :]), writes=[B_c2], dma=True)
                    B_diag = Buf("diag")
                    dlist = [(g, k) for g in range(2) for k in range(31)]

                    def diag_some(kc):
                        for idx, (g, k) in enumerate(dlist[kc * 8:(kc + 1) * 8]):
                            if idx % 2 == 0:
                                op("dve", lambda e, g=g, k=k: e.tensor_scalar(out=diag[:, g, k, :], in0=ident[:, :],
                                                                              scalar1=cw_t[:, g, k:k + 1], scalar2=None, op0=ALU.mult),
                                   reads=[B_cw], writes=[B_diag])
                            else:
                                op("act", lambda e, g=g, k=k: e.activation(out=diag[:, g, k, :], in_=ident[:, :],
                                                                           func=AF.Copy, scale=cw_t[:, g, k:k + 1]),
                                   reads=[B_cw], writes=[B_diag])

                    load_weight_cols(wl, l, 0, 1792, W_AB, B_WAB, "ab", extra=diag_some)
                    stg2 = [sb(wl, "stgo%d" % i, [128, 2048], F32) for i in range(2)]
                    B_stg2 = [Buf("stgo0"), Buf("stgo1")]
                    for j in range(4):
                        bq = j % 2
                        for hf in range(2):
                            kc = 2 * j + hf
                            op("sp" if hf == 0 else "pool",
                               lambda e, kc=kc, bq=bq, hf=hf: e.dma_start(out=stg2[bq][:, hf * 1024:(hf + 1) * 1024],
                                                                         in_=w_out[l, kc * 128:(kc + 1) * 128, :]),
                               writes=[B_stg2[bq]], dma=True)
                        if j % 2 == 0:
                            op("dve", lambda e, j=j, bq=bq: e.tensor_copy(
                                out=Wo[:, 2 * j:2 * j + 2, :], in_=stg2[bq][:, :].rearrange("p (a n) -> p a n", a=2)),
                               reads=[B_stg2[bq]], writes=[B_c2])
                        else:
                            op("act", lambda e, j=j, bq=bq: e.activation(
                                out=Wo[:, 2 * j:2 * j + 2, :], in_=stg2[bq][:, :].rearrange("p (a n) -> p a n", a=2), func=AF.Copy),
                               reads=[B_stg2[bq]], writes=[B_c2])
                    for cc in range(2):
                        op("sp", lambda e, cc=cc: e.dma_start(out=stg2[0][:, cc * 256:(cc + 1) * 256],
                                                              in_=pw_w[l, cc * 128:(cc + 1) * 128, :]),
                           writes=[B_stg2[0]], dma=True)
                    op("dve", lambda e: e.tensor_copy(out=pw[:, :, :], in_=stg2[0][:, 0:512].rearrange("p (a n) -> p a n", a=2)),
                       reads=[B_stg2[0]], writes=[B_c2])
                    S_.barrier()
                fgt = None
                if last:
                    fgt = sb(ph, "fgt", [128, D], F32)
                    op("sp", lambda e: e.dma_start(out=fgt[:, :], in_=fg[:, :]), writes=[B_c2], dma=True)
                xt2 = [[sb(ph, "xa%d_%d" % (q, i), [128, D], F32) for i in range(4)] for q in range(2)]
                B_xt2 = [[Buf("xa%d_%d" % (q, i)) for i in range(4)] for q in range(2)]
                stat2 = [[sb(ph, "stata%d_%d" % (q, i), [128, 4], F32) for i in range(4)] for q in range(2)]
                B_stat2 = [[Buf("stata%d_%d" % (q, i)) for i in range(4)] for q in range(2)]
                fstat = [sb(ph, "fstat%d" % i, [128, 4], F32) for i in range(4)]
                B_fstat = [Buf("fstat%d" % i) for i in range(4)]
                fjunk = sb(ph, "fjunk", [128, D], BF16)
                B_fjunk = Buf("fjunk")
                u_ext2 = [sb(ph, "u_ext%d" % q, [128, 2, 544], BF16) for q in range(2)]
                B_u2 = [[Buf("u%d_%d" % (q, g)) for g in range(2)] for q in range(2)]
                qz2 = [sb(ph, "qz%d" % q, [128, 2, 2, 512], BF16) for q in range(2)]
                qx2 = [sb(ph, "qx%d" % q, [128, 2, 512], BF16) for q in range(2)]
                k2 = [sb(ph, "kbf%d" % q, [128, 2, 512], BF16) for q in range(2)]
                gsr2 = [sb(ph, "gsr%d" % q, [128, 2, 512], F32) for q in range(2)]
                kz2 = [sb(ph, "kzp%d" % q, [128, 4, 2, 192], BF16) for q in range(2)]
                vp2 = [sb(ph, "vpp%d" % q, [128, 4, 2, 192], BF16) for q in range(2)]
                gsA2 = [[sb(ph, "gsA%d_%d" % (q, co), [128, 512], F32) for co in range(2)] for q in range(2)]
                B_q2 = [[Buf("q%d_%d" % (q, i)) for i in range(2)] for q in range(2)]
                B_qx2 = [[Buf("qx%d_%d" % (q, i)) for i in range(2)] for q in range(2)]
                B_k2 = [[Buf("k%d_%d" % (q, i)) for i in range(2)] for q in range(2)]
                B_gsr2 = [[Buf("gsr%d_%d" % (q, i)) for i in range(2)] for q in range(2)]
                B_kz2 = [[Buf("kz%d_%d" % (q, i)) for i in range(4)] for q in range(2)]
                B_vp2 = [[Buf("vp%d_%d" % (q, i)) for i in range(4)] for q in range(2)]
                B_gsA2 = [[Buf("gsA%d_%d" % (q, i)) for i in range(2)] for q in range(2)]
                K = {"pji": 0, "sti": 0, "acci": 0, "ptri": 0}
                ymc = [sb(ph, "ymc%d" % i, [128, 4, 512], BF16) for i in range(2)]
                B_ymc = [Buf("ymc0"), Buf("ymc1")]
                hn = sb(ph, "hna", [128, D], BF16)
                hnT = sb(ph, "hnTa", [128, 8, 512], BF16)
                f32t = [sb(ph, "f%d" % i, [128, 512], F32) for i in range(7)]
                B_f = [Buf("f%d" % i) for i in range(7)]
                y32 = sb(ph, "y32", [128, 2, 512], F32)
                ybf = sb(ph, "ybf", [128, 2, 512], BF16)
                ysq = sb(ph, "ysq", [128, 2, 512], BF16)
                s_bf = sb(ph, "s_bf", [128, 2, 512], BF16)
                cat = sb(ph, "cat", [128, 4, 512], BF16)
                pTr = [sb(ph, "pTr%d" % i, [128, 4, 128], BF16) for i in range(4)]
                Rpad4 = [sb(ph, "Rpad4_%d" % i, [128, 2, 128], BF16) for i in range(4)]
                B_Rp4 = [[Buf("Rp4_%d_%d" % (i, r)) for r in range(2)] for i in range(4)]
                Rm = sb(ph, "Rm", [128, 2, 64], F32)
                obf = sb(ph, "obf", [128, 512], BF16)
                osq = sb(ph, "osq", [128, 512], BF16)

                B_hn = Buf("hna")
                B_hnT = [Buf("hnTa%d" % i) for i in range(4)]
                B_y32 = [Buf("y32_0"), Buf("y32_1")]
                B_ybf = [Buf("ybf0"), Buf("ybf1")]
                B_ysq = [Buf("ysq0"), Buf("ysq1")]
                B_s = [Buf("s0"), Buf("s1")]
                B_cat = [Buf("cat%d" % i) for i in range(4)]
                B_pTr = [Buf("pTr%d" % i) for i in range(4)]
                B_Rm = [Buf("Rm0"), Buf("Rm1")]
                B_Rp = [Buf("Rp0"), Buf("Rp1")]
                B_obf = Buf("obf")
                B_osq = Buf("osq")

                for q in range(2):
                    op("pool", lambda e, q=q: e.memset(u_ext2[q][:, :, :], 0.0), writes=B_u2[q])
                    op("dve", lambda e, q=q: e.memset(qz2[q][:, :, :, :], 0.0), writes=B_q2[q])
                    op("pool", lambda e, q=q: e.memset(kz2[q][:, :, :, :], 0.0), writes=B_kz2[q])
                    op("dve", lambda e, q=q: e.memset(vp2[q][:, :, :, :], 0.0), writes=B_vp2[q])
                op("dve", lambda e: e.memset(Rm[:, :, :], 0.0), writes=B_Rm)
                for i in range(4):
                    op("dve", lambda e, i=i: e.memset(Rpad4[i][:, :, :], 0.0), writes=B_Rp4[i])
                S_.barrier()


                def proj_fm(col, pb):
                    for kc in range(8):
                        op("pe", lambda e, kc=kc: e.matmul(
                            pj[pb][:, :], lhsT=W_AB[:, kc, col:col + 128], rhs=hnT[:, kc, :],
                            start=(kc == 0), stop=(kc == 7)),
                           reads=[B_WAB] + B_hnT, writes=[B_pj[pb]])

                def rstd_from(var_t, B_var):
                    op("act", lambda e: e.activation(out=var_t[:, :], in_=var_t[:, :], func=AF.Ln,
                                                     bias=eps_rms[:, 1:2]),
                       reads=[B_var], writes=[B_var])
                    op("act", lambda e: e.activation(out=var_t[:, :], in_=var_t[:, :], func=AF.Exp, scale=-0.5),
                       reads=[B_var], writes=[B_var])

                def sigmoid_act(psrc, B_psrc, tmpt, B_tmp):
                    op("act", lambda e: e.activation(out=tmpt[:, :], in_=psrc, func=AF.Exp, scale=-1.0),
                       reads=[B_psrc], writes=[B_tmp])
                    op("act", lambda e: e.activation(out=tmpt[:, :], in_=tmpt[:, :], func=AF.Ln, bias=eps_rms[:, 2:3]),
                       reads=[B_tmp], writes=[B_tmp])
                    op("act", lambda e: e.activation(out=tmpt[:, :], in_=tmpt[:, :], func=AF.Exp, scale=-1.0),
                       reads=[B_tmp], writes=[B_tmp])

                def s1(c):
                    P = c % 2
                    c0 = c * 512
                    xt, B_xt, stat, B_stat = xt2[P], B_xt2[P], stat2[P], B_stat2[P]
                    u_ext, B_u = u_ext2[P], B_u2[P]
                    u_prev, B_uprev = u_ext2[1 - P], B_u2[1 - P]
                    qz, qx_bf, k_bf, gs_r, kzpad, vpad = qz2[P], qx2[P], k2[P], gsr2[P], kz2[P], vp2[P]
                    B_q, B_qx, B_k, B_gsr, B_kz, B_vp = B_q2[P], B_qx2[P], B_k2[P], B_gsr2[P], B_kz2[P], B_vp2[P]
                    gsA, B_gsA = gsA2[P], B_gsA2[P]
                    c0 = c * 512
                    for p in range(4):
                        op("sp", lambda e, p=p, c=c, c0=c0: e.dma_start(out=ymc[c % 2][:, p, :], in_=ym_d[p, :, c0:c0 + 512]),
                           writes=[B_ymc[c % 2]], dma=True)
                        yield
                    for tt in range(4):
                        T = 4 * c + tt
                        load_norm_transpose(src, T, xt[tt], B_xt[tt], hn, B_hn, stat[tt], B_stat[tt],
                                            hnT, B_hnT[tt], tt)
                        yield
                    for g in range(2):
                        if c > 0:
                            op("pool", lambda e, g=g: e.tensor_copy(out=u_ext[:, g, 0:30], in_=u_prev[:, g, 512:542]),
                               reads=[B_uprev[g]], writes=[B_u[g]])
                        pbv = K['pji'] % 2
                        K['pji'] += 1
                        proj_fm(g * 128, pbv)
                        sbi = K['sti'] % 3
                        K['sti'] += 1
                        for kc in range(8):
                            op("pe", lambda e, kc=kc, g=g, sbi=sbi: e.matmul(
                                st[sbi][:, :], lhsT=W_AB[:, kc, 256 + g * 128:256 + (g + 1) * 128], rhs=hnT[:, kc, :],
                                start=(kc == 0), stop=(kc == 7)),
                               reads=[B_WAB] + B_hnT, writes=[B_st[sbi]])
                        op("dve", lambda e, pbv=pbv: e.tensor_copy(out=f32t[5][:, :], in_=pj[pbv][:, :]),
                           reads=[B_pj[pbv]], writes=[B_f[5]])
                        sigmoid_act(st[sbi][:, :], B_st[sbi], f32t[0], B_f[0])
                        op("dve", lambda e, g=g: e.tensor_tensor(out=u_ext[:, g, 30:542], in0=f32t[5][:, :],
                                                                 in1=f32t[0][:, :], op=ALU.mult),
                           reads=[B_f[5], B_f[0]], writes=[B_u[g]])
                        yield
                    for rp in range(2):
                        pb = K['pji'] % 2
                        K['pji'] += 1
                        proj_fm(768 + rp * 128, pb)
                        for hh in range(2):
                            op("act", lambda e, rp=rp, pb=pb, hh=hh: e.activation(
                                out=qz[hh * 64:(hh + 1) * 64, rp, hh, :], in_=pj[pb][hh * 64:(hh + 1) * 64, :], func=AF.Copy),
                               reads=[B_pj[pb]], writes=[B_q[rp]])
                        op("dve", lambda e, rp=rp, pb=pb: e.tensor_tensor(
                            out=qx_bf[:, rp, :].rearrange("p (a t) -> p a t", a=4),
                            in0=pj[pb][:, :].rearrange("p (a t) -> p a t", a=4),
                            in1=xi[:, rp:rp + 1, :].broadcast_to([128, 4, 128]), op=ALU.mult),
                           reads=[B_pj[pb]], writes=[B_qx[rp]])
                        sbi = K['sti'] % 3
                        K['sti'] += 1
                        for kc in range(8):
                            op("pe", lambda e, kc=kc, rp=rp, sbi=sbi: e.matmul(
                                st[sbi][:, :], lhsT=W_AB[:, kc, 1024 + rp * 128:1024 + (rp + 1) * 128], rhs=hnT[:, kc, :],
                                start=(kc == 0), stop=(kc == 7)),
                               reads=[B_WAB] + B_hnT, writes=[B_st[sbi]])
                        op("dve", lambda e, rp=rp, sbi=sbi: e.tensor_copy(out=k_bf[:, rp, :], in_=st[sbi][:, :]),
                           reads=[B_st[sbi]], writes=[B_k[rp]])
                        yield
                    for tt in range(4):
                        pb = K['pji'] % 2
                        K['pji'] += 1
                        for kc in range(8):
                            op("pe", lambda e, kc=kc, tt=tt, pb=pb: e.matmul(
                                pj[pb][:, :], lhsT=hnT[:, kc, tt * 128:(tt + 1) * 128], rhs=W_AB[:, kc, 1024:1536],
                                start=(kc == 0), stop=(kc == 7)),
                               reads=[B_WAB, B_hnT[tt]], writes=[B_pj[pb]])
                        ksrc = pj[pb][:, 0:256].rearrange("p (q two e) -> p q two e", two=2, e=64)
                        vsrc = pj[pb][:, 256:512].rearrange("p (q two e) -> p q two e", two=2, e=64)
                        zsrc = zeta[:, :, :].rearrange("p (q two) o -> p q two o", two=2)
                        for two in range(2):
                            op("dve", lambda e, tt=tt, two=two, ksrc=ksrc, zsrc=zsrc: e.tensor_tensor(
                                out=kzpad[:, tt, :, two * 128:two * 128 + 64], in0=ksrc[:, :, two, :],
                                in1=zsrc[:, :, two, :].broadcast_to([128, 2, 64]), op=ALU.mult),
                               reads=[B_pj[pb]], writes=[B_kz[tt]])
                            op("dve", lambda e, tt=tt, two=two, vsrc=vsrc: e.tensor_copy(
                                out=vpad[:, tt, :, two * 128:two * 128 + 64], in_=vsrc[:, :, two, :]),
                               reads=[B_pj[pb]], writes=[B_vp[tt]])
                        yield
                    for co in range(2):
                        sbi = K['sti'] % 3
                        K['sti'] += 1
                        for kc in range(8):
                            op("pe", lambda e, kc=kc, co=co, sbi=sbi: e.matmul(
                                st[sbi][:, :], lhsT=W_AB[:, kc, 512 + co * 128:512 + (co + 1) * 128], rhs=hnT[:, kc, :],
                                start=(kc == 0), stop=(kc == 7)),
                               reads=[B_WAB] + B_hnT, writes=[B_st[sbi]])
                        gs, B_gs = gsA[co], B_gsA[co]
                        op("act", lambda e, sbi=sbi, gs=gs: e.activation(out=gs[:, :], in_=st[sbi][:, :], func=AF.Copy),
                           reads=[B_st[sbi]], writes=[B_gs])
                        sigmoid_act(st[sbi][:, :], B_st[sbi], f32t[0], B_f[0])
                        op("dve", lambda e, gs=gs: e.tensor_tensor(out=gs[:, :], in0=gs[:, :], in1=f32t[0][:, :],
                                                                   op=ALU.mult),
                           reads=[B_f[0], B_gs], writes=[B_gs])
                        yield
                    for rp in range(2):
                        sbi = K['sti'] % 3
                        K['sti'] += 1
                        for kc in range(8):
                            op("pe", lambda e, kc=kc, rp=rp, sbi=sbi: e.matmul(
                                st[sbi][:, :], lhsT=W_AB[:, kc, 1536 + rp * 128:1536 + (rp + 1) * 128], rhs=hnT[:, kc, :],
                                start=(kc == 0), stop=(kc == 7)),
                               reads=[B_WAB] + B_hnT, writes=[B_st[sbi]])
                        op("act", lambda e, rp=rp, sbi=sbi: e.activation(out=gs_r[:, rp, :], in_=st[sbi][:, :], func=AF.Copy),
                           reads=[B_st[sbi]], writes=[B_gsr[rp]])
                        sigmoid_act(st[sbi][:, :], B_st[sbi], f32t[6], B_f[6])
                        op("dve", lambda e, rp=rp: e.tensor_tensor(out=gs_r[:, rp, :], in0=gs_r[:, rp, :],
                                                                   in1=f32t[6][:, :], op=ALU.mult),
                           reads=[B_f[6]], writes=[B_gsr[rp]])
                        yield
                def s2(c):
                    P = c % 2
                    c0 = c * 512
                    xt, B_xt, stat, B_stat = xt2[P], B_xt2[P], stat2[P], B_stat2[P]
                    u_ext, B_u = u_ext2[P], B_u2[P]
                    u_prev, B_uprev = u_ext2[1 - P], B_u2[1 - P]
                    qz, qx_bf, k_bf, gs_r, kzpad, vpad = qz2[P], qx2[P], k2[P], gsr2[P], kz2[P], vp2[P]
                    B_q, B_qx, B_k, B_gsr, B_kz, B_vp = B_q2[P], B_qx2[P], B_k2[P], B_gsr2[P], B_kz2[P], B_vp2[P]
                    gsA, B_gsA = gsA2[P], B_gsA2[P]
                    for g in range(2):
                        ab = K['acci'] % 2
                        K['acci'] += 1
                        for k in range(31):
                            op("pe", lambda e, g=g, k=k, ab=ab: e.matmul(
                                acc[ab][:, :], lhsT=diag[:, g, k, :], rhs=u_ext[:, g, k:k + 512],
                                start=(k == 0), stop=(k == 30)),
                               reads=[B_u[g]], writes=[B_acc[ab]])
                        op("dve", lambda e, g=g, ab=ab: e.tensor_scalar(out=y32[:, g, :], in0=acc[ab][:, :],
                                                                        scalar1=cv_t[:, 0, g:g + 1], scalar2=None, op0=ALU.add),
                           reads=[B_acc[ab]], writes=[B_y32[g]])
                        op("act", lambda e, g=g: e.activation(out=ybf[:, g, :], in_=y32[:, g, :], func=AF.Copy),
                           reads=[B_y32[g]], writes=[B_ybf[g]])
                        op("act", lambda e, g=g: e.activation(out=ysq[:, g, :], in_=y32[:, g, :], func=AF.Square),
                           reads=[B_y32[g]], writes=[B_ysq[g]])
                        yield
                    mb = K['pji'] % 2
                    K['pji'] += 1
                    for g in range(2):
                        op("pe", lambda e, g=g, mb=mb: e.matmul(pj[mb][:, :], lhsT=o256[:, :], rhs=ybf[:, g, :],
                                                               start=(g == 0), stop=(g == 1)),
                           reads=[B_ybf[g]], writes=[B_pj[mb]])
                        yield
                    vb = K['pji'] % 2
                    K['pji'] += 1
                    for g in range(2):
                        op("pe", lambda e, g=g, vb=vb: e.matmul(pj[vb][:, :], lhsT=o256[:, :], rhs=ysq[:, g, :],
                                                               start=(g == 0), stop=(g == 1)),
                           reads=[B_ysq[g]], writes=[B_pj[vb]])
                        yield
                    mu, B_mu = f32t[1], B_f[1]
                    var, B_var = f32t[2], B_f[2]
                    op("act", lambda e, mb=mb: e.activation(out=mu[:, :], in_=pj[mb][:, :], func=AF.Copy),
                       reads=[B_pj[mb]], writes=[B_mu])
                    op("act", lambda e, mb=mb: e.activation(out=var[:, :], in_=pj[mb][:, :], func=AF.Square),
                       reads=[B_pj[mb]], writes=[B_var])
                    op("dve", lambda e, vb=vb: e.tensor_tensor(out=var[:, :], in0=pj[vb][:, :], in1=var[:, :],
                                                               op=ALU.subtract),
                       reads=[B_pj[vb], B_var], writes=[B_var])
                    rstd_from(var, B_var)
                    oacc = []
                    for rp in range(2):
                        oacc.append(K['acci'] % 2)
                        K['acci'] += 1
                    pis = []
                    for tt in range(4):
                        t0 = tt * 128
                        sbi = K['sti'] % 3
                        K['sti'] += 1
                        pi = tt
                        pis.append(pi)
                        for h in range(4):
                            rp, hh = h // 2, h % 2
                            op("pe", lambda e, h=h, rp=rp, hh=hh, t0=t0, sbi=sbi: e.matmul(
                                st[sbi][:, h * 128:(h + 1) * 128], lhsT=k_bf[:, rp, t0:t0 + 128],
                                rhs=qz[:, rp, hh, t0:t0 + 128], start=True, stop=True),
                               reads=[B_k[rp], B_q[rp]], writes=[B_st[sbi]])
                        op("dve", lambda e, sbi=sbi, pi=pi: e.tensor_tensor(
                            out=pTr[pi][:, :, :], in0=st[sbi][:, :].rearrange("p (h i) -> p h i", h=4),
                            in1=dec[:, :, :], op=ALU.mult),
                           reads=[B_st[sbi]], writes=[B_pTr[pi]])
                    yield
                    kb = K['pji'] % 2
                    K['pji'] += 1
                    firstkv = True
                    for tt in range(4):
                        for rp in range(2):
                            cb = (tt * 2 + rp) * 64
                            for hh in range(2):
                                op("pe", lambda e, rp=rp, hh=hh, tt=tt, kb=kb, cb=cb, firstkv=firstkv: e.matmul(
                                    pj[kb][:, cb:cb + 64], lhsT=kzpad[:, tt, rp, hh * 64:hh * 64 + 128],
                                    rhs=vpad[:, tt, rp, hh * 128:hh * 128 + 64], start=firstkv, stop=(tt == 3 and rp == 1 and hh == 1),
                                    skip_group_check=True),
                                   reads=[B_kz[tt], B_vp[tt]], writes=[B_pj[kb]])
                                firstkv = False
                    for tt in range(4):
                        t0 = tt * 128
                        for rp in range(2):
                            ab = oacc[rp]
                            for hh in range(2):
                                h = 2 * rp + hh
                                op("pe", lambda e, h=h, rp=rp, hh=hh, tt=tt, t0=t0, ab=ab: e.matmul(
                                    acc[ab][:, t0:t0 + 128], lhsT=vpad[:, tt, rp, hh * 64:hh * 64 + 128],
                                    rhs=pTr[tt][:, h, :], start=(tt == 0 and hh == 0), stop=False, skip_group_check=True),
                                   reads=[B_vp[tt], B_pTr[tt]], writes=[B_acc[ab]])
                    yield
                    for tt in range(4):
                        for rp in range(2):
                            cb = (tt * 2 + rp) * 64
                            op("dve", lambda e, rp=rp, tt=tt: e.tensor_copy(out=Rpad4[tt][0:64, rp, 0:64], in_=Rm[0:64, rp, :]),
                               reads=[B_Rm[rp]], writes=[B_Rp4[tt][rp]])
                            op("dve", lambda e, rp=rp, tt=tt: e.tensor_copy(out=Rpad4[tt][64:128, rp, 64:128], in_=Rm[64:128, rp, :]),
                               reads=[B_Rm[rp]], writes=[B_Rp4[tt][rp]])
                            op("dve", lambda e, rp=rp, kb=kb, cb=cb: e.scalar_tensor_tensor(
                                out=Rm[:, rp, :], in0=Rm[:, rp, :], scalar=gv[:, rp, 0:1], in1=pj[kb][:, cb:cb + 64],
                                op0=ALU.mult, op1=ALU.add),
                               reads=[B_pj[kb], B_Rm[rp]], writes=[B_Rm[rp]])
                    for tt in range(4):
                        t0 = tt * 128
                        for rp in range(2):
                            ab = oacc[rp]
                            op("pe", lambda e, rp=rp, t0=t0, ab=ab, tt=tt: e.matmul(
                                acc[ab][:, t0:t0 + 128], lhsT=Rpad4[tt][:, rp, :],
                                rhs=qx_bf[:, rp, t0:t0 + 128], start=False, stop=(tt == 3), skip_group_check=True),
                               reads=[B_Rp4[tt][rp], B_qx[rp]], writes=[B_acc[ab]])
                    yield
                    for g in range(2):
                        d1, B_d1 = f32t[3], B_f[3]
                        e1, B_e1 = f32t[4], B_f[4]
                        op("dve", lambda e, g=g: e.tensor_tensor(out=d1[:, :], in0=y32[:, g, :], in1=mu[:, :],
                                                                 op=ALU.subtract),
                           reads=[B_y32[g], B_mu], writes=[B_d1])
                        op("dve", lambda e: e.tensor_tensor(out=d1[:, :], in0=d1[:, :], in1=var[:, :], op=ALU.mult),
                           reads=[B_d1, B_var], writes=[B_d1])
                        op("dve", lambda e, g=g: e.tensor_scalar(out=d1[:, :], in0=d1[:, :],
                                                                 scalar1=cv_t[:, 1, g:g + 1], scalar2=cv_t[:, 2, g:g + 1],
                                                                 op0=ALU.mult, op1=ALU.add),
                           reads=[B_d1], writes=[B_d1])
                        sigmoid_act(d1[:, :], B_d1, e1, B_e1)
                        op("dve", lambda e, g=g: e.tensor_tensor(out=s_bf[:, g, :], in0=d1[:, :], in1=e1[:, :],
                                                                 op=ALU.mult),
                           reads=[B_d1, B_e1], writes=[B_s[g]])
                        yield
                    for co in range(2):
                        gs, B_gs = gsA[co], B_gsA[co]
                        ppb = K['pji'] % 2
                        K['pji'] += 1
                        for ci in range(2):
                            op("pe", lambda e, ci=ci, co=co, ppb=ppb: e.matmul(
                                pj[ppb][:, :], lhsT=pw[:, ci, co * 128:(co + 1) * 128], rhs=s_bf[:, ci, :],
                                start=(ci == 0), stop=(ci == 1)),
                               reads=[B_s[ci]], writes=[B_pj[ppb]])
                        op("dve", lambda e, co=co, ppb=ppb, gs=gs: e.scalar_tensor_tensor(
                            out=cat[:, co, :], in0=pj[ppb][:, :], scalar=cv_t[:, 3, co:co + 1], in1=gs[:, :],
                            op0=ALU.add, op1=ALU.mult),
                           reads=[B_pj[ppb], B_gs], writes=[B_cat[co]])
                        yield
                    for rp in range(2):
                        ab = oacc[rp]
                        op("act", lambda e, ab=ab: e.activation(out=obf[:, :], in_=acc[ab][:, :], func=AF.Copy),
                           reads=[B_acc[ab]], writes=[B_obf])
                        op("act", lambda e, ab=ab: e.activation(out=osq[:, :], in_=acc[ab][:, :], func=AF.Square),
                           reads=[B_acc[ab]], writes=[B_osq])
                        mb = K['pji'] % 2
                        K['pji'] += 1
                        op("pe", lambda e, mb=mb: e.matmul(pj[mb][:, :], lhsT=bones[:, :], rhs=obf[:, :], start=True, stop=True),
                           reads=[B_obf], writes=[B_pj[mb]])
                        vb = K['pji'] % 2
                        K['pji'] += 1
                        op("pe", lambda e, vb=vb: e.matmul(pj[vb][:, :], lhsT=bones[:, :], rhs=osq[:, :], start=True, stop=True),
                           reads=[B_osq], writes=[B_pj[vb]])
                        mu, B_mu = f32t[1], B_f[1]
                        var, B_var = f32t[2], B_f[2]
                        d1, B_d1 = f32t[3], B_f[3]
                        op("act", lambda e, mb=mb: e.activation(out=mu[:, :], in_=pj[mb][:, :], func=AF.Copy),
                           reads=[B_pj[mb]], writes=[B_mu])
                        op("act", lambda e, mb=mb: e.activation(out=var[:, :], in_=pj[mb][:, :], func=AF.Square),
                           reads=[B_pj[mb]], writes=[B_var])
                        op("dve", lambda e, vb=vb: e.tensor_tensor(out=var[:, :], in0=pj[vb][:, :], in1=var[:, :],
                                                                   op=ALU.subtract),
                           reads=[B_pj[vb], B_var], writes=[B_var])
                        rstd_from(var, B_var)
                        op("dve", lambda e, ab=ab: e.tensor_tensor(out=d1[:, :], in0=acc[ab][:, :], in1=mu[:, :],
                                                                   op=ALU.subtract),
                           reads=[B_acc[ab], B_mu], writes=[B_d1])
                        op("dve", lambda e: e.tensor_tensor(out=d1[:, :], in0=d1[:, :], in1=var[:, :], op=ALU.mult),
                           reads=[B_d1, B_var], writes=[B_d1])
                        op("dve", lambda e, rp=rp: e.tensor_tensor(out=cat[:, 2 + rp, :], in0=d1[:, :], in1=gs_r[:, rp, :],
                                                                   op=ALU.mult),
                           reads=[B_d1, B_gsr[rp]], writes=[B_cat[2 + rp]])
                        yield
                    for tt in range(4):
                        T = 4 * c + tt
                        t0 = tt * 128
                        xb_i = tt
                        for half in range(2):
                            sbi = K['sti'] % 3
                            K['sti'] += 1
                            for kc in range(8):
                                if kc < 4:
                                    lt = cat[:, kc, t0:t0 + 128]
                                    rd = [B_cat[kc]]
                                else:
                                    lt = ymc[c % 2][:, kc - 4, t0:t0 + 128]
                                    rd = [B_ymc[c % 2]]
                                op("pe", lambda e, kc=kc, lt=lt, half=half, sbi=sbi: e.matmul(
                                    st[sbi][:, :], lhsT=lt, rhs=Wo[:, kc, half * 512:(half + 1) * 512],
                                    start=(kc == 0), stop=(kc == 7)),
                                   reads=rd, writes=[B_st[sbi]])
                            op("dve", lambda e, tt=tt, half=half, sbi=sbi, xb_i=xb_i: e.tensor_tensor(
                                out=xt[xb_i][:, half * 512:(half + 1) * 512], in0=st[sbi][:, :],
                                in1=xt[tt][:, half * 512:(half + 1) * 512], op=ALU.add),
                               reads=[B_st[sbi], B_xt[tt]], writes=[B_xt[xb_i]])
                        if not last:
                            op("sp", lambda e, T=T, xb_i=xb_i: e.dma_start(out=x1[T * 128:(T + 1) * 128, :], in_=xt[xb_i][:, :]),
                               reads=[B_xt[xb_i]], dma=True)
                        else:
                            stt = fstat[tt]
                            op("act", lambda e, xb_i=xb_i, stt=stt: e.activation(out=fjunk[:, :], in_=xt[xb_i][:, :], func=AF.Square,
                                                                                 accum_out=stt[:, 0:1]),
                               reads=[B_xt[xb_i]], writes=[B_fjunk, B_fstat[tt]])
                            op("act", lambda e, stt=stt: e.activation(out=stt[:, 1:2], in_=stt[:, 0:1], func=AF.Ln,
                                                                      scale=1.0 / D, bias=eps_rms[:, 0:1]),
                               reads=[B_fstat[tt]], writes=[B_fstat[tt]])
                            op("act", lambda e, stt=stt: e.activation(out=stt[:, 2:3], in_=stt[:, 1:2], func=AF.Exp, scale=-0.5),
                               reads=[B_fstat[tt]], writes=[B_fstat[tt]])
                            op("dve", lambda e, xb_i=xb_i, stt=stt: e.scalar_tensor_tensor(
                                out=xt[xb_i][:, :], in0=xt[xb_i][:, :], scalar=stt[:, 2:3], in1=fgt[:, :],
                                op0=ALU.mult, op1=ALU.mult),
                               reads=[B_xt[xb_i], B_fstat[tt]], writes=[B_xt[xb_i]])
                            op("sp", lambda e, T=T, xb_i=xb_i: e.dma_start(out=out[T * 128:(T + 1) * 128, :], in_=xt[xb_i][:, :]),
                               reads=[B_xt[xb_i]], dma=True)

                        yield

                def drain(g):
                    for _ in g:
                        pass

                drain(s1(0))
                for c in range(NCH):
                    g2 = s2(c)
                    g1 = s1(c + 1) if c + 1 < NCH else iter(())
                    a2 = a1 = True
                    while a2 or a1:
                        if a2:
                            a2 = next(g2, "END") != "END"
                        if a1:
                            a1 = next(g1, "END") != "END"
                S_.barrier()
        S_.barrier()
        print("KERNEL nops", S_.nops, {k: e["count"] for k, e in S_.engs.items()})
    return nc


def _bf(a):
    return np.ascontiguousarray(a.astype(ml_dtypes.bfloat16))


def make_consts(S):
    c = {}
    c["c_ident"] = _bf(np.eye(128, dtype=np.float32))
    j = np.arange(128)
    c["c_causal"] = _bf(np.where(j[:, None] <= j[None, :], 0.0, MASKV).astype(np.float32))
    pos = np.arange(S)
    ka = np.zeros((20, S), np.float32)
    for n in range(16):
        ka[n] = np.where(pos // 256 == n, MASKV, 0.0)
    ka[16] = pos % 128
    ka[17] = pos // 128
    ka[18] = 1.0
    ka[19] = 1.0
    c["c_kaug"] = _bf(ka)
    qa = np.zeros((4, 8, S), np.float32)
    for h in range(8):
        slope = 2.0 ** (-(h + 1))
        qa[0, h] = slope * 8.0
        qa[1, h] = slope * 1024.0
        qa[2, h] = -slope * 8.0 * (pos % 128)
        qa[3, h] = -slope * 1024.0 * (pos // 128)
    c["c_qaug"] = _bf(qa)
    hh = np.arange(4, dtype=np.float64)
    g = 1.0 - np.exp2(-5.0 - hh)
    i = np.arange(128, dtype=np.float64)
    diff = i[None, :] - i[:, None]
    dec = np.zeros((128, 4, 128), np.float64)
    for h in range(4):
        dec[:, h, :] = np.where(diff >= 0, g[h] ** np.maximum(diff, 0.0), 0.0) * 0.125
    c["c_dec"] = dec.astype(np.float32)
    zeta = np.zeros((128, 4, 1), np.float64)
    for h in range(4):
        zeta[:, h, 0] = g[h] ** (127 - i) * 0.125
    c["c_zeta"] = zeta.astype(np.float32)
    xi = np.zeros((128, 2, 512), np.float64)
    gv = np.zeros((128, 2, 1), np.float64)
    ii = np.arange(512) % 128
    for rp in range(2):
        for hf in range(2):
            h = 2 * rp + hf
            xi[hf * 64:(hf + 1) * 64, rp, :] = (g[h] ** (ii + 1.0))[None, :]
            gv[hf * 64:(hf + 1) * 64, rp, 0] = g[h] ** 128
    c["c_xi"] = xi.astype(np.float32)
    c["c_gv"] = gv.astype(np.float32)
    bo = np.zeros((128, 128), np.float32)
    bo[0:64, 0:64] = 1.0 / 64
    bo[64:128, 64:128] = 1.0 / 64
    c["c_bones"] = _bf(bo)
    c["c_o256"] = _bf(np.full((128, 128), 1.0 / 256, np.float32))
    return c


def prep_shared(norm_g, w_in, conv_w, conv_b, conv_ln_g, conv_ln_b, conv_pw_w, conv_pw_b, w_out, final_g, S):
    DEPTH = w_in.shape[0]
    f = lambda a: np.ascontiguousarray(np.asarray(a, dtype=np.float32))
    m = {}
    m["w_in"] = f(w_in)
    m["w_out"] = f(w_out)
    m["pw_w"] = f(conv_pw_w)
    m["ng"] = f(np.asarray(norm_g).reshape(DEPTH, 8, 128).transpose(0, 2, 1))
    m["cwT"] = f(np.asarray(conv_w).transpose(0, 2, 1).reshape(DEPTH, 2, 128, 31).transpose(0, 2, 1, 3))
    cv = np.stack([np.asarray(conv_b), np.asarray(conv_ln_g), np.asarray(conv_ln_b), np.asarray(conv_pw_b)], axis=1)
    m["cvec"] = f(cv.reshape(DEPTH, 4, 2, 128).transpose(0, 3, 1, 2))
    m["fg"] = f(np.broadcast_to(np.asarray(final_g)[None, :], (128, D)))
    m.update(make_consts(S))
    return m


_CACHE = {}


def run(x, shared, S, DEPTH):
    B = x.shape[0]
    key = (S, DEPTH)
    if key not in _CACHE:
        _CACHE[key] = build_program(S, DEPTH)
    nc = _CACHE[key]
    in_maps = []
    for b in range(B):
        d = dict(shared)
        d["x"] = np.ascontiguousarray(x[b])
        in_maps.append(d)
    res = run_bass_kernel_spmd(nc, in_maps, core_ids=list(range(B)))
    return np.stack([np.asarray(r["out"]) for r in res.results], axis=0).astype(np.float32)


def kernel(x, norm_g, w_in, conv_w, conv_b, conv_ln_g, conv_ln_b, conv_pw_w, conv_pw_b, w_out, final_g):
    x = np.asarray(x, dtype=np.float32)
    S = x.shape[1]
    DEPTH = np.asarray(w_in).shape[0]
    shared = prep_shared(norm_g, w_in, conv_w, conv_b, conv_ln_g, conv_ln_b, conv_pw_w, conv_pw_b,
                         w_out, final_g, S)
    return run(x, shared, S, DEPTH)
```

```python
import os
import numpy as np
import ml_dtypes
from contextlib import ExitStack
import concourse.bass as bass
import concourse.mybir as mybir
from concourse.bass_utils import run_bass_kernel_spmd

F32 = mybir.dt.float32
BF16 = mybir.dt.bfloat16
ALU = mybir.AluOpType
AF = mybir.ActivationFunctionType

D = 1024
INW = 3840
NEG = -1.0e30
MASKV = -30000.0
RMS_EPS = 1e-6
LN_EPS = 1e-5
SCALE = 0.125


class Buf:
    __slots__ = ("name", "w", "r", "excl")

    def __init__(self, name, excl=False):
        self.name = name
        self.w = None
        self.r = {}
        self.excl = excl


class Sched:
    def __init__(self, nc, es, n_dma=12):
        self.nc = nc
        self.sems = []
        self.h = {"pe": nc.tensor, "act": nc.scalar, "dve": nc.vector, "pool": nc.gpsimd, "sp": nc.sync}
        self.engs = {}
        for k in self.h:
            idx = self._sem(es, "s_" + k)
            self.engs[k] = {"sem": idx, "count": 0, "seen": {}}
        self.pool = {}
        self.rr = {}
        for q in ("sp", "pool", "act"):
            self.pool[q] = [{"idx": self._sem(es, "d_%s%d" % (q, i)), "val": 0} for i in range(n_dma)]
            self.rr[q] = 0
        self.nops = 0
        self.limit = int(os.environ.get("KLIMIT", "0")) or None

    def _sem(self, es, name):
        self.sems.append(es.enter_context(self.nc.semaphore(name)))
        return len(self.sems) - 1

    def op(self, eng, fn, reads=(), writes=(), dma=False):
        if self.limit is not None and self.nops >= self.limit:
            return None
        e = self.engs[eng]
        waits = {}

        def need(src, sidx, val, raw):
            if src == eng and eng == "pe":
                return
            if e["seen"].get(sidx, 0) >= val:
                return
            if waits.get(sidx, 0) < val:
                waits[sidx] = val

        for b in reads:
            if b.w is not None:
                need(b.w[0], b.w[1], b.w[2], True)
            if b.excl:
                for sidx, (src, val) in b.r.items():
                    if src != eng:
                        need(src, sidx, val, False)
        for b in writes:
            if b.w is not None:
                need(b.w[0], b.w[1], b.w[2], False)
            for sidx, (src, val) in b.r.items():
                need(src, sidx, val, False)
        if dma:
            pl = self.pool[eng]
            slot = pl[self.rr[eng] % len(pl)]
            self.rr[eng] += 1
            if slot["val"] > 0:
                need(None, slot["idx"], slot["val"], False)
            slot["val"] += 16
            sig = (None, slot["idx"], slot["val"])
            inc = 16
        else:
            e["count"] += 1
            sig = (eng, e["sem"], e["count"])
            inc = 1
        h = self.h[eng]
        for sidx, val in waits.items():
            e["seen"][sidx] = val
            h.wait_ge(self.sems[sidx], val)
        ins = fn(h)
        if os.environ.get("KTRACE"):
            import inspect
            fr = inspect.stack()[1]
            print("OP", self.nops, eng, fr.lineno, (fr.code_context or [""])[0].strip()[:90])
        ins.then_inc(self.sems[sig[1]], inc)
        self.nops += 1
        for b in writes:
            b.w = sig
            b.r = {}
        for b in reads:
            if b in writes:
                continue
            cur = b.r.get(sig[1])
            if cur is None or cur[1] < sig[2]:
                b.r[sig[1]] = (sig[0], sig[2])
        return sig

    def barrier(self):
        sigs = []
        for k, e in self.engs.items():
            if e["count"] > 0:
                sigs.append((e["sem"], e["count"]))
        for q, pl in self.pool.items():
            for slot in pl:
                if slot["val"] > 0:
                    sigs.append((slot["idx"], slot["val"]))
        for k, e in self.engs.items():
            h = self.h[k]
            for sidx, val in sigs:
                if e["seen"].get(sidx, 0) >= val:
                    continue
                e["seen"][sidx] = val
                h.wait_ge(self.sems[sidx], val)


def build_program(S, DEPTH):
    NT = S // 128
    NCH = S // 512
    nc = bass.Bass("TRN2", target_bir_lowering=False)

    def dr(name, shape, dt, kind="ExternalInput"):
        return nc.dram_tensor(name, shape, dt, kind=kind).ap()

    x_in = dr("x", [S, D], F32)
    out = dr("out", [S, D], F32, "ExternalOutput")
    x1 = dr("x1s", [S, D], F32, "Internal")
    ym_d = dr("ym_d", [4, 128, S], BF16, "Internal")
    w_in = dr("w_in", [DEPTH, D, INW], F32)
    w_out = dr("w_out", [DEPTH, D, D], F32)
    pw_w = dr("pw_w", [DEPTH, 256, 256], F32)
    ng = dr("ng", [DEPTH, 128, 8], F32)
    cwT = dr("cwT", [DEPTH, 128, 2, 31], F32)
    cvec = dr("cvec", [DEPTH, 128, 4, 2], F32)
    fg = dr("fg", [128, D], F32)
    d_ident = dr("c_ident", [128, 128], BF16)
    d_causal = dr("c_causal", [128, 128], BF16)
    d_kaug = dr("c_kaug", [20, S], BF16)
    d_qaug = dr("c_qaug", [4, 8, S], BF16)
    d_dec = dr("c_dec", [128, 4, 128], F32)
    d_zeta = dr("c_zeta", [128, 4, 1], F32)
    d_xi = dr("c_xi", [128, 2, 512], F32)
    d_gv = dr("c_gv", [128, 2, 1], F32)
    d_bones = dr("c_bones", [128, 128], BF16)
    d_o256 = dr("c_o256", [128, 128], BF16)

    with ExitStack() as es:
        S_ = Sched(nc, es)
        op = S_.op

        uniq = [0]

        def sb(stack, name, shape, dt):
            uniq[0] += 1
            return stack.enter_context(nc.sbuf_tensor("%s_%d" % (name, uniq[0]), shape, dt))

        def ps(name, shape, dt):
            return es.enter_context(nc.psum_tensor(name, shape, dt))

        ident = sb(es, "ident", [128, 128], BF16)
        causal = sb(es, "causal", [128, 128], BF16)
        bones = sb(es, "bones", [128, 128], BF16)
        o256 = sb(es, "o256", [128, 128], BF16)
        B_const = Buf("const")
        B_WM = Buf("W_M")

        tps = ps("tps", [128, 1024], BF16)
        pj = [ps("pj%d" % i, [128, 512], F32) for i in range(2)]
        st = [ps("st%d" % i, [128, 512], F32) for i in range(3)]
        acc = [ps("acc%d" % i, [128, 512], F32) for i in range(2)]
        B_tps = Buf("tps", True)
        B_pj = [Buf("pj0", True), Buf("pj1", True)]
        B_st = [Buf("st%d" % i, True) for i in range(3)]
        B_acc = [Buf("acc0", True), Buf("acc1", True)]

        for t, d in ((ident, d_ident), (causal, d_causal), (bones, d_bones), (o256, d_o256)):
            op("sp", lambda e, t=t, d=d: e.dma_start(out=t[:, :], in_=d[:, :]), writes=[B_const], dma=True)
        S_.barrier()

        def load_norm(src, T, xt_t, B_xt, hn, B_hn, stat, B_stat):
            op("sp", lambda e: e.dma_start(out=xt_t[:, :], in_=src[T * 128:(T + 1) * 128, :]),
               writes=[B_xt], dma=True)
            op("act", lambda e: e.activation(out=hn[:, :], in_=xt_t[:, :], func=AF.Square,
                                             accum_out=stat[:, 0:1]),
               reads=[B_xt], writes=[B_hn, B_stat])
            op("act", lambda e: e.activation(out=stat[:, 1:2], in_=stat[:, 0:1], func=AF.Ln,
                                             scale=1.0 / D, bias=eps_rms[:, 0:1]),
               reads=[B_stat], writes=[B_stat])
            op("act", lambda e: e.activation(out=stat[:, 2:3], in_=stat[:, 1:2], func=AF.Exp, scale=-0.5),
               reads=[B_stat], writes=[B_stat])
            op("dve", lambda e: e.tensor_scalar(out=hn[:, :], in0=xt_t[:, :], scalar1=stat[:, 2:3],
                                                scalar2=None, op0=ALU.mult),
               reads=[B_xt, B_stat], writes=[B_hn])

        def transpose_hn(hn, B_hn, hnT, B_hnT_tt, tt):
            for kc in range(8):
                op("pe", lambda e, kc=kc: e.transpose(out=tps[:, kc * 128:(kc + 1) * 128],
                                                      in_=hn[:, kc * 128:(kc + 1) * 128], identity=ident[:, :]),
                   reads=[B_hn], writes=[B_tps])
            op("dve", lambda e: e.tensor_copy(out=hnT[:, :, tt * 128:(tt + 1) * 128],
                                              in_=tps[:, :].rearrange("p (k t) -> p k t", k=8)),
               reads=[B_tps], writes=[B_hnT_tt])

        def load_norm_transpose(src, T, xt_t, B_xt, hn, B_hn, stat, B_stat, hnT, B_hnT_tt, tt):
            load_norm(src, T, xt_t, B_xt, hn, B_hn, stat, B_stat)
            transpose_hn(hn, B_hn, hnT, B_hnT_tt, tt)

        def sigmoid_recip(psrc, B_psrc, tmpt, B_tmp, rows=slice(0, 128)):
            op("act", lambda e: e.activation(out=tmpt[rows, :], in_=psrc, func=AF.Exp, scale=-1.0),
               reads=[B_psrc], writes=[B_tmp])
            op("dve", lambda e: e.tensor_scalar(out=tmpt[rows, :], in0=tmpt[rows, :], scalar1=1.0,
                                                scalar2=None, op0=ALU.add),
               reads=[B_tmp], writes=[B_tmp])
            op("dve", lambda e: e.reciprocal(out=tmpt[rows, :], in_=tmpt[rows, :]),
               reads=[B_tmp], writes=[B_tmp])

        eps_rms = sb(es, "eps_rms", [128, 3], F32)
        op("dve", lambda e: e.memset(eps_rms[:, 2:3], 1.0), writes=[B_const])
        op("dve", lambda e: e.memset(eps_rms[:, 0:1], RMS_EPS), writes=[B_const])
        op("dve", lambda e: e.memset(eps_rms[:, 1:2], LN_EPS), writes=[B_const])
        S_.barrier()

        def load_weight_cols(stack, l, col0, ncols, Wdst, B_W, tagname, extra=None):
            NSTG = 4
            stg = [sb(stack, "stg%s%d" % (tagname, i), [128, 2048], F32) for i in range(NSTG)]
            B_stg = [Buf("stg%d" % i) for i in range(NSTG)]
            ngt = sb(stack, "ngt" + tagname, [128, 8], F32)
            B_ng = Buf("ng")
            op("sp", lambda e: e.dma_start(out=ngt[:, :], in_=ng[l, :, :]), writes=[B_ng], dma=True)
            h1 = (ncols // 2 + 127) // 128 * 128
            def issue(kc):
                b = kc % NSTG
                op("sp", lambda e: e.dma_start(out=stg[b][:, 0:h1],
                                               in_=w_in[l, kc * 128:(kc + 1) * 128, col0:col0 + h1]),
                   writes=[B_stg[b]], dma=True)
                op("pool", lambda e: e.dma_start(out=stg[b][:, h1:ncols],
                                                 in_=w_in[l, kc * 128:(kc + 1) * 128, col0 + h1:col0 + ncols]),
                   writes=[B_stg[b]], dma=True)

            for kc in range(NSTG):
                issue(kc)
            for kc in range(8):
                b = kc % NSTG
                op("dve" if kc % 2 == 0 else "act",
                   (lambda e, kc=kc, b=b: e.tensor_scalar(out=Wdst[:, kc, 0:ncols], in0=stg[b][:, 0:ncols],
                                                          scalar1=ngt[:, kc:kc + 1], scalar2=None, op0=ALU.mult))
                   if kc % 2 == 0 else
                   (lambda e, kc=kc, b=b: e.activation(out=Wdst[:, kc, 0:ncols], in_=stg[b][:, 0:ncols],
                                                       func=AF.Copy, scale=ngt[:, kc:kc + 1])),
                   reads=[B_stg[b], B_ng], writes=[B_W])
                if kc + NSTG < 8:
                    issue(kc + NSTG)
                if extra is not None:
                    extra(kc)

        for l in range(DEPTH):
            src = x_in if l == 0 else x1
            last = (l == DEPTH - 1)
            with ExitStack() as ph:
                W_M = sb(ph, "W_M", [128, 8, 2048], BF16)
                with ExitStack() as wl:
                    load_weight_cols(wl, l, 1792, 2048, W_M, B_WM, "m")
                    S_.barrier()
                K_aug = sb(ph, "K_aug", [84, 8, S], BF16)
                V_ext = sb(ph, "V_ext", [128, NT, 768], BF16)
                xt = [sb(ph, "xt%d" % i, [128, D], F32) for i in range(1)]
                hn = sb(ph, "hn", [128, D], BF16)
                stat = [sb(ph, "stat%d" % i, [128, 4], F32) for i in range(2)]
                hnT2 = [sb(ph, "hnT%d" % i, [128, 8, 512], BF16) for i in range(2)]
                Q_aug2 = [sb(ph, "Q_aug%d" % i, [84, 8, 512], BF16) for i in range(2)]
                gsl2 = [sb(ph, "gsl%d" % i, [128, 4, 512], BF16) for i in range(2)]
                pT = [sb(ph, "pT%d" % i, [128, 512], BF16) for i in range(3)]
                rec = sb(ph, "rt", [128, 512], F32)
                tmp = rec
                gtmp = sb(ph, "gtmp", [128, 512], F32)
                gx = sb(ph, "gx", [128, 512], F32)
                B_gx = Buf("gx")
                ymt = [sb(ph, "ymt%d" % i, [128, 512], BF16) for i in range(2)]
                ksum = sb(ph, "ksum", [64, 8, 16], F32)
                kmean = sb(ph, "kmean", [64, 8, 16], BF16)
                gate_sb4 = [sb(ph, "gate_sb%d" % i, [128, 8, 16], F32) for i in range(4)]
                top84 = [sb(ph, "top8%d" % i, [128, 8, 8], F32) for i in range(4)]
                mask_tm4 = [sb(ph, "mask_tm%d" % i, [128, 8, 16], BF16) for i in range(4)]

                B_xt = [Buf("xt0")]
                B_hn = Buf("hn")
                B_stat = [Buf("stat0"), Buf("stat1")]
                B_hnT2 = [[Buf("hnT%d_%d" % (q, i)) for i in range(4)] for q in range(2)]
                B_qq2 = [[Buf("qq%d_%d" % (q, h)) for h in range(8)] for q in range(2)]
                B_qm2 = [[Buf("qm%d_%d" % (q, t)) for t in range(4)] for q in range(2)]
                B_qc2 = [Buf("qc0"), Buf("qc1")]
                B_ka = [[Buf("ka%d_%d" % (h, c)) for c in range(NCH)] for h in range(8)]
                B_ve = [Buf("ve%d" % T) for T in range(NT)]
                B_gsl2 = [[Buf("gsl%d_%d" % (q, p)) for p in range(4)] for q in range(2)]
                B_pT = [Buf("pT%d" % i) for i in range(3)]
                B_rec = Buf("rt")
                B_tmp = B_rec
                B_gtmp = Buf("gtmp")
                B_ymt = [Buf("ymt0"), Buf("ymt1")]
                B_ksum = Buf("ksum")
                B_kmean = Buf("kmean")
                B_gate4 = [Buf("gate_sb%d" % i) for i in range(4)]
                B_top84 = [Buf("top8%d" % i) for i in range(4)]
                B_mask4 = [Buf("mask_tm%d" % i) for i in range(4)]

                for h in range(8):
                    op("sp" if h % 2 == 0 else "pool",
                       lambda e, h=h: e.dma_start(out=K_aug[64:84, h, :], in_=d_kaug[:, :]),
                       writes=[B_const], dma=True)
                for p in range(4):
                    op("dve", lambda e, p=p: e.memset(V_ext[:, :, p * 192 + 64:p * 192 + 128], 1.0),
                       writes=[B_const])
                for q in range(2):
                    op("dve", lambda e, q=q: e.memset(Q_aug2[q][64:80, :, :], 0.0), writes=[B_const])
                for i in range(4):
                    op("dve", lambda e, i=i: e.memset(gate_sb4[i][:, :, :], NEG), writes=[B_const])
                    op("dve", lambda e, i=i: e.memset(mask_tm4[i][:, :, :], 0.0), writes=[B_const])
                op("dve", lambda e: e.memset(ksum[:, :, :], 0.0), writes=[B_const])
                S_.barrier()

                ctr = {"pj": 0}

                def nextpj():
                    ctr["pj"] += 1
                    return ctr["pj"] % 2

                def preamble_units(c):
                    par = c % 2
                    c0 = c * 512
                    hnT = hnT2[par]
                    B_hnT = B_hnT2[par]
                    Q_aug = Q_aug2[par]
                    units = []

                    def u_load1(tt):
                        T = 4 * c + tt
                        b = T % 2
                        load_norm(src, T, xt[0], B_xt[0], hn, B_hn, stat[b], B_stat[b])

                    def u_load2(tt):
                        transpose_hn(hn, B_hn, hnT, B_hnT[tt], tt)

                    def u_qc():
                        op("sp", lambda e: e.dma_start(out=Q_aug[80:84, :, :], in_=d_qaug[:, :, c0:c0 + 512]),
                           writes=[B_qc2[par]], dma=True)

                    def u_q(p):
                        pb = nextpj()
                        for kc in range(8):
                            op("pe", lambda e, kc=kc: e.matmul(
                                pj[pb][:, :], lhsT=W_M[:, kc, p * 128:(p + 1) * 128], rhs=hnT[:, kc, :],
                                start=(kc == 0), stop=(kc == 7)),
                               reads=[B_WM] + B_hnT, writes=[B_pj[pb]])
                        for hh in range(2):
                            op("dve", lambda e, hh=hh: e.tensor_copy(out=Q_aug[0:64, 2 * p + hh, :],
                                                                     in_=pj[pb][hh * 64:(hh + 1) * 64, :]),
                               reads=[B_pj[pb]], writes=[B_qq2[par][2 * p + hh]])

                    def u_k(p):
                        pb = nextpj()
                        for kc in range(8):
                            op("pe", lambda e, kc=kc: e.matmul(
                                pj[pb][:, :], lhsT=W_M[:, kc, 512 + p * 128:512 + (p + 1) * 128], rhs=hnT[:, kc, :],
                                start=(kc == 0), stop=(kc == 7)),
                               reads=[B_WM] + B_hnT, writes=[B_pj[pb]])
                        for hh in range(2):
                            h = 2 * p + hh
                            op("dve", lambda e, hh=hh, h=h: e.tensor_copy(out=K_aug[0:64, h, c0:c0 + 512],
                                                                          in_=pj[pb][hh * 64:(hh + 1) * 64, :]),
                               reads=[B_pj[pb]], writes=[B_ka[h][c]])
                            op("dve", lambda e, hh=hh, h=h: e.tensor_reduce(
                                out=ksum[0:64, h, 2 * c:2 * c + 2],
                                in_=pj[pb][hh * 64:(hh + 1) * 64, :].rearrange("p (b t) -> p b t", b=2),
                                axis=mybir.AxisListType.X, op=ALU.add),
                               reads=[B_pj[pb]], writes=[B_ksum])

                    def u_v(tt):
                        T = 4 * c + tt
                        pb = nextpj()
                        for kc in range(8):
                            op("pe", lambda e, kc=kc: e.matmul(
                                pj[pb][:, :], lhsT=hnT[:, kc, tt * 128:(tt + 1) * 128], rhs=W_M[:, kc, 1024:1536],
                                start=(kc == 0), stop=(kc == 7)),
                               reads=[B_WM, B_hnT[tt]], writes=[B_pj[pb]])
                        vsrc = pj[pb][:, :].rearrange("p (q two e) -> p q two e", two=2, e=64)
                        vdst = V_ext[:, T, :].rearrange("p (q c) -> p q c", c=192)
                        op("dve", lambda e: e.tensor_copy(out=vdst[:, :, 0:64], in_=vsrc[:, :, 0, :]),
                           reads=[B_pj[pb]], writes=[B_ve[T]])
                        op("dve", lambda e: e.tensor_copy(out=vdst[:, :, 128:192], in_=vsrc[:, :, 1, :]),
                           reads=[B_pj[pb]], writes=[B_ve[T]])

                    def u_kmean():
                        op("dve", lambda e: e.tensor_scalar(out=kmean[:, :, 2 * c:2 * c + 2], in0=ksum[:, :, 2 * c:2 * c + 2],
                                                            scalar1=1.0 / 256.0, scalar2=None, op0=ALU.mult),
                           reads=[B_ksum], writes=[B_kmean])

                    def u_gsilu(p):
                        pb = nextpj()
                        for kc in range(8):
                            op("pe", lambda e, kc=kc: e.matmul(
                                pj[pb][:, :], lhsT=W_M[:, kc, 1536 + p * 128:1536 + (p + 1) * 128], rhs=hnT[:, kc, :],
                                start=(kc == 0), stop=(kc == 7)),
                               reads=[B_WM] + B_hnT, writes=[B_pj[pb]])
                        op("dve", lambda e: e.tensor_copy(out=gx[:, :], in_=pj[pb][:, :]),
                           reads=[B_pj[pb]], writes=[B_gx])
                        sigmoid_recip(pj[pb][:, :], B_pj[pb], gtmp, B_gtmp)
                        op("dve", lambda e: e.tensor_tensor(out=gsl2[par][:, p, :], in0=gx[:, :],
                                                            in1=gtmp[:, :], op=ALU.mult),
                           reads=[B_gx, B_gtmp], writes=[B_gsl2[par][p]])

                    def u_gating(tt):
                        T = 4 * c + tt
                        qb = T // 2
                        gate_sb, top8, mask_tm = gate_sb4[tt], top84[tt], mask_tm4[tt]
                        pb = nextpj()
                        for h in range(8):
                            op("pe", lambda e, h=h: e.matmul(
                                pj[pb][:, h * 16:h * 16 + qb], lhsT=Q_aug[0:64, h, tt * 128:(tt + 1) * 128],
                                rhs=kmean[0:64, h, 0:qb], start=True, stop=True),
                               reads=[B_qq2[par][h], B_kmean], writes=[B_pj[pb]])
                        gsrc = pj[pb][:, 0:128].rearrange("p (h n) -> p h n", n=16)
                        op("dve", lambda e: e.tensor_copy(out=gate_sb[:, :, 0:qb], in_=gsrc[:, :, 0:qb]),
                           reads=[B_pj[pb]], writes=[B_gate4[tt]])
                        for h in range(8):
                            op("dve", lambda e, h=h: e.max(out=top8[:, h, :], in_=gate_sb[:, h, :]),
                               reads=[B_gate4[tt]], writes=[B_top84[tt]])
                        op("dve", lambda e: e.tensor_tensor(
                            out=mask_tm[:, :, 0:qb], in0=gate_sb[:, :, 0:qb],
                            in1=top8[:, :, 2:3].broadcast_to([128, 8, qb]), op=ALU.is_lt),
                           reads=[B_gate4[tt], B_top84[tt]], writes=[B_mask4[tt]])

                    def u_gating2(tt):
                        mask_tm = mask_tm4[tt]
                        for h in range(8):
                            op("pe", lambda e, h=h: e.transpose(out=tps[0:16, h * 128:(h + 1) * 128],
                                                                in_=mask_tm[:, h, :], identity=ident[:, :]),
                               reads=[B_mask4[tt]], writes=[B_tps])
                        op("act", lambda e: e.activation(
                            out=Q_aug[64:80, :, tt * 128:(tt + 1) * 128],
                            in_=tps[0:16, :].rearrange("p (h t) -> p h t", h=8), func=AF.Copy),
                           reads=[B_tps], writes=[B_qm2[par][tt]])

                    for tt in range(4):
                        units.append(lambda tt=tt: u_load1(tt))
                        units.append(lambda tt=tt: u_load2(tt))
                    units.append(u_qc)
                    for p in range(4):
                        units.append(lambda p=p: u_k(p))
                    units.append(u_kmean)
                    for p in range(4):
                        units.append(lambda p=p: u_q(p))
                    for tt in range(4):
                        units.append(lambda tt=tt: u_v(tt))
                    gs_units = [(lambda p=p: u_gsilu(p)) for p in range(4)]
                    if c >= 2:
                        units.append(lambda: u_gating(0))
                        for tt in range(4):
                            if tt + 1 < 4:
                                units.append(lambda tt=tt: u_gating(tt + 1))
                            units.append(gs_units[tt])
                            units.append(lambda tt=tt: u_gating2(tt))
                    else:
                        units.extend(gs_units)
                    return units

                def attention(c, pending):
                    par = c % 2
                    c0 = c * 512
                    Q_aug = Q_aug2[par]
                    npast = 4 * c
                    tiles = []
                    for h in range(8):
                        for kt in range(npast):
                            tiles.append((h, kt, None))
                        for k in range(4):
                            tiles.append((h, 4 * c + k, k))
                    ntl = len(tiles)
                    per_head = npast + 4

                    def emit_qk(i):
                        h, kt, k = tiles[i]
                        sbi = i % 3
                        qreads = [B_qq2[par][h], B_qc2[par]] + B_qm2[par]
                        if k is None:
                            op("pe", lambda e: e.matmul(
                                st[sbi][:, :], lhsT=K_aug[0:84, h, kt * 128:(kt + 1) * 128], rhs=Q_aug[0:84, h, :],
                                start=True, stop=True),
                               reads=[B_ka[h][kt // 4]] + qreads, writes=[B_st[sbi]])
                        else:
                            n = (4 - k) * 128
                            q0 = k * 128
                            op("pe", lambda e: e.matmul(
                                st[sbi][:, 0:128], lhsT=K_aug[0:84, h, kt * 128:(kt + 1) * 128],
                                rhs=Q_aug[0:84, h, q0:q0 + 128], start=True, stop=False),
                               reads=[B_ka[h][c]] + qreads, writes=[B_st[sbi]])
                            op("pe", lambda e: e.matmul(
                                st[sbi][:, 0:128], lhsT=ident[:, :], rhs=causal[:, :], start=False, stop=True),
                               reads=[], writes=[B_st[sbi]])
                            if n > 128:
                                op("pe", lambda e: e.matmul(
                                    st[sbi][:, 128:n], lhsT=K_aug[0:84, h, kt * 128:(kt + 1) * 128],
                                    rhs=Q_aug[0:84, h, q0 + 128:512], start=True, stop=True),
                                   reads=[B_ka[h][c]] + qreads, writes=[B_st[sbi]])

                    def emit_exp(i):
                        h, kt, k = tiles[i]
                        sbi = i % 3
                        pbi = i % 3
                        n = 512 if k is None else (4 - k) * 128
                        op("act", lambda e: e.activation(out=pT[pbi][:, 0:n], in_=st[sbi][:, 0:n],
                                                         func=AF.Exp, scale=SCALE),
                           reads=[B_st[sbi]], writes=[B_pT[pbi]])

                    def emit_pv(i):
                        h, kt, k = tiles[i]
                        p, odd = h // 2, h % 2
                        vcol = p * 192 + (64 if odd else 0)
                        ab = h % 2
                        pbi = i % 3
                        first = (i % per_head == 0)
                        n = 512 if k is None else (4 - k) * 128
                        q0 = 0 if k is None else k * 128
                        op("pe", lambda e: e.matmul(
                            acc[ab][:, q0:512], lhsT=V_ext[:, kt, vcol:vcol + 128], rhs=pT[pbi][:, 0:n],
                            start=first, stop=(k == 3)),
                           reads=[B_ve[kt], B_pT[pbi]], writes=[B_acc[ab]])
                        if k == 3:
                            num = slice(64, 128) if odd else slice(0, 64)
                            den = slice(0, 64) if odd else slice(64, 128)
                            yb = p % 2
                            if c <= 1:
                                op("act", lambda e: e.activation(out=rec[den, :], in_=acc[ab][den, :], func=AF.Ln),
                                   reads=[B_acc[ab]], writes=[B_rec])
                                op("act", lambda e: e.activation(out=rec[den, :], in_=rec[den, :], func=AF.Exp, scale=-1.0),
                                   reads=[B_rec], writes=[B_rec])
                            else:
                                op("dve", lambda e: e.reciprocal(out=rec[den, :], in_=acc[ab][den, :]),
                                   reads=[B_acc[ab]], writes=[B_rec])
                            op("dve", lambda e: e.tensor_tensor(
                                out=tmp[num, :], in0=acc[ab][num, :], in1=rec[den, :], op=ALU.mult),
                               reads=[B_acc[ab], B_rec], writes=[B_tmp])
                            op("pool", lambda e: e.tensor_tensor(
                                out=ymt[yb][num, :], in0=tmp[num, :], in1=gsl2[par][num, p, :], op=ALU.mult),
                               reads=[B_tmp, B_gsl2[par][p]], writes=[B_ymt[yb]])
                            if odd:
                                op("sp", lambda e: e.dma_start(out=ym_d[p, :, c0:c0 + 512], in_=ymt[yb][:, :]),
                                   reads=[B_ymt[yb]], dma=True)

                    nsteps = ntl + 2
                    npend = len(pending)
                    done = 0
                    for i in range(nsteps):
                        if i < ntl:
                            emit_qk(i)
                        if 1 <= i <= ntl:
                            emit_exp(i - 1)
                        if i >= 2:
                            emit_pv(i - 2)
                        want = (npend * (i + 1)) // nsteps
                        while done < want:
                            pending[done]()
                            done += 1
                    while done < npend:
                        pending[done]()
                        done += 1

                for u in preamble_units(0):
                    u()
                for c in range(NCH):
                    nxt = preamble_units(c + 1) if c + 1 < NCH else []
                    attention(c, nxt)
                S_.barrier()

            with ExitStack() as ph:
                W_AB = sb(ph, "W_AB", [128, 8, 1792], BF16)
                Wo = sb(ph, "Wo", [128, 8, D], BF16)
                pw = sb(ph, "pw", [128, 2, 256], BF16)
                diag = sb(ph, "diag", [128, 2, 31, 128], BF16)
                cw_t = sb(ph, "cw_t", [128, 2, 31], F32)
                cv_t = sb(ph, "cv_t", [128, 4, 2], F32)
                dec = sb(ph, "dec", [128, 4, 128], F32)
                zeta = sb(ph, "zeta", [128, 4, 1], F32)
                xi = sb(ph, "xi", [128, 2, 128], F32)
                gv = sb(ph, "gv", [128, 2, 1], F32)
                B_WAB = Buf("W_AB")
                B_c2 = Buf("c2")
                B_cw = Buf("cw")
                with ExitStack() as wl:
                    op("sp", lambda e: e.dma_start(out=cw_t[:, :, :], in_=cwT[l, :, :, :]), writes=[B_cw], dma=True)
                    op("sp", lambda e: e.dma_start(out=cv_t[:, :, :], in_=cvec[l, :, :, :]), writes=[B_c2], dma=True)
                    op("sp", lambda e: e.dma_start(out=dec[:, :, :], in_=d_dec[:, :, :]), writes=[B_c2], dma=True)
                    op("sp", lambda e: e.dma_start(out=zeta[:, :, :], in_=d_zeta[:, :, :]), writes=[B_c2], dma=True)
                    op("sp", lambda e: e.dma_start(out=xi[:, :, :], in_=d_xi[:, :, 0:128]), writes=[B_c2], dma=True)
                    op("sp", lambda e: e.dma_start(out=gv[:, :, :], in_=d_gv[:, :, :]), writes=[B_c2], dma=True)
                    B_diag = Buf("diag")
                    dlist = [(g, k) for g in range(2) for k in range(31)]

                    def diag_some(kc):
                        for idx, (g, k) in enumerate(dlist[kc * 8:(kc + 1) * 8]):
                            if idx % 2 == 0:
                                op("dve", lambda e, g=g, k=k: e.tensor_scalar(out=diag[:, g, k, :], in0=ident[:, :],
                                                                              scalar1=cw_t[:, g, k:k + 1], scalar2=None, op0=ALU.mult),
                                   reads=[B_cw], writes=[B_diag])
                            else:
                                op("act", lambda e, g=g, k=k: e.activation(out=diag[:, g, k, :], in_=ident[:, :],
                                                                           func=AF.Copy, scale=cw_t[:, g, k:k + 1]),
                                   reads=[B_cw], writes=[B_diag])

                    load_weight_cols(wl, l, 0, 1792, W_AB, B_WAB, "ab", extra=diag_some)
                    stg2 = [sb(wl, "stgo%d" % i, [128, 2048], F32) for i in range(2)]
                    B_stg2 = [Buf("stgo0"), Buf("stgo1")]
                    for j in range(4):
                        bq = j % 2
                        for hf in range(2):
                            kc = 2 * j + hf
                            op("sp" if hf == 0 else "pool",
                               lambda e, kc=kc, bq=bq, hf=hf: e.dma_start(out=stg2[bq][:, hf * 1024:(hf + 1) * 1024],
                                                                         in_=w_out[l, kc * 128:(kc + 1) * 128, :]),
                               writes=[B_stg2[bq]], dma=True)
                        if j % 2 == 0:
                            op("dve", lambda e, j=j, bq=bq: e.tensor_copy(
                                out=Wo[:, 2 * j:2 * j + 2, :], in_=stg2[bq][:, :].rearrange("p (a n) -> p a n", a=2)),
                               reads=[B_stg2[bq]], writes=[B_c2])
                        else:
                            op("act", lambda e, j=j, bq=bq: e.activation(
                                out=Wo[:, 2 * j:2 * j + 2, :], in_=stg2[bq][:, :].rearrange("p (a n) -> p a n", a=2), func=AF.Copy),
                               reads=[B_stg2[bq]], writes=[B_c2])
                    for cc in range(2):
                        op("sp", lambda e, cc=cc: e.dma_start(out=stg2[0][:, cc * 256:(cc + 1) * 256],
                                                              in_=pw_w[l, cc * 128:(cc + 1) * 128, :]),
                           writes=[B_stg2[0]], dma=True)
                    op("dve", lambda e: e.tensor_copy(out=pw[:, :, :], in_=stg2[0][:, 0:512].rearrange("p (a n) -> p a n", a=2)),
                       reads=[B_stg2[0]], writes=[B_c2])
                    S_.barrier()
                fgt = None
                if last:
                    fgt = sb(ph, "fgt", [128, D], F32)
                    op("sp", lambda e: e.dma_start(out=fgt[:, :], in_=fg[:, :]), writes=[B_c2], dma=True)
                xt2 = [[sb(ph, "xa%d_%d" % (q, i), [128, D], F32) for i in range(4)] for q in range(2)]
                B_xt2 = [[Buf("xa%d_%d" % (q, i)) for i in range(4)] for q in range(2)]
                stat2 = [[sb(ph, "stata%d_%d" % (q, i), [128, 4], F32) for i in range(4)] for q in range(2)]
                B_stat2 = [[Buf("stata%d_%d" % (q, i)) for i in range(4)] for q in range(2)]
                fstat = [sb(ph, "fstat%d" % i, [128, 4], F32) for i in range(4)]
                B_fstat = [Buf("fstat%d" % i) for i in range(4)]
                fjunk = sb(ph, "fjunk", [128, D], BF16)
                B_fjunk = Buf("fjunk")
                u_ext2 = [sb(ph, "u_ext%d" % q, [128, 2, 544], BF16) for q in range(2)]
                B_u2 = [[Buf("u%d_%d" % (q, g)) for g in range(2)] for q in range(2)]
                qz2 = [sb(ph, "qz%d" % q, [128, 2, 2, 512], BF16) for q in range(2)]
                qx2 = [sb(ph, "qx%d" % q, [128, 2, 512], BF16) for q in range(2)]
                k2 = [sb(ph, "kbf%d" % q, [128, 2, 512], BF16) for q in range(2)]
                gsr2 = [sb(ph, "gsr%d" % q, [128, 2, 512], F32) for q in range(2)]
                kz2 = [sb(ph, "kzp%d" % q, [128, 4, 2, 192], BF16) for q in range(2)]
                vp2 = [sb(ph, "vpp%d" % q, [128, 4, 2, 192], BF16) for q in range(2)]
                gsA2 = [[sb(ph, "gsA%d_%d" % (q, co), [128, 512], F32) for co in range(2)] for q in range(2)]
                B_q2 = [[Buf("q%d_%d" % (q, i)) for i in range(2)] for q in range(2)]
                B_qx2 = [[Buf("qx%d_%d" % (q, i)) for i in range(2)] for q in range(2)]
                B_k2 = [[Buf("k%d_%d" % (q, i)) for i in range(2)] for q in range(2)]
                B_gsr2 = [[Buf("gsr%d_%d" % (q, i)) for i in range(2)] for q in range(2)]
                B_kz2 = [[Buf("kz%d_%d" % (q, i)) for i in range(4)] for q in range(2)]
                B_vp2 = [[Buf("vp%d_%d" % (q, i)) for i in range(4)] for q in range(2)]
                B_gsA2 = [[Buf("gsA%d_%d" % (q, i)) for i in range(2)] for q in range(2)]
                K = {"pji": 0, "sti": 0, "acci": 0, "ptri": 0}
                ymc = [sb(ph, "ymc%d" % i, [128, 4, 512], BF16) for i in range(2)]
                B_ymc = [Buf("ymc0"), Buf("ymc1")]
                hn = sb(ph, "hna", [128, D], BF16)
                hnT = sb(ph, "hnTa", [128, 8, 512], BF16)
                f32t = [sb(ph, "f%d" % i, [128, 512], F32) for i in range(7)]
                B_f = [Buf("f%d" % i) for i in range(7)]
                y32 = sb(ph, "y32", [128, 2, 512], F32)
                ybf = sb(ph, "ybf", [128, 2, 512], BF16)
                ysq = sb(ph, "ysq", [128, 2, 512], BF16)
                s_bf = sb(ph, "s_bf", [128, 2, 512], BF16)
                cat = sb(ph, "cat", [128, 4, 512], BF16)
                pTr = [sb(ph, "pTr%d" % i, [128, 4, 128], BF16) for i in range(4)]
                Rpad4 = [sb(ph, "Rpad4_%d" % i, [128, 2, 128], BF16) for i in range(4)]
                B_Rp4 = [[Buf("Rp4_%d_%d" % (i, r)) for r in range(2)] for i in range(4)]
                Rm = sb(ph, "Rm", [128, 2, 64], F32)
                obf = sb(ph, "obf", [128, 512], BF16)
                osq = sb(ph, "osq", [128, 512], BF16)

                B_hn = Buf("hna")
                B_hnT = [Buf("hnTa%d" % i) for i in range(4)]
                B_y32 = [Buf("y32_0"), Buf("y32_1")]
                B_ybf = [Buf("ybf0"), Buf("ybf1")]
                B_ysq = [Buf("ysq0"), Buf("ysq1")]
                B_s = [Buf("s0"), Buf("s1")]
                B_cat = [Buf("cat%d" % i) for i in range(4)]
                B_pTr = [Buf("pTr%d" % i) for i in range(4)]
                B_Rm = [Buf("Rm0"), Buf("Rm1")]
                B_Rp = [Buf("Rp0"), Buf("Rp1")]
                B_obf = Buf("obf")
                B_osq = Buf("osq")

                for q in range(2):
                    op("pool", lambda e, q=q: e.memset(u_ext2[q][:, :, :], 0.0), writes=B_u2[q])
                    op("dve", lambda e, q=q: e.memset(qz2[q][:, :, :, :], 0.0), writes=B_q2[q])
                    op("pool", lambda e, q=q: e.memset(kz2[q][:, :, :, :], 0.0), writes=B_kz2[q])
                    op("dve", lambda e, q=q: e.memset(vp2[q][:, :, :, :], 0.0), writes=B_vp2[q])
                op("dve", lambda e: e.memset(Rm[:, :, :], 0.0), writes=B_Rm)
                for i in range(4):
                    op("dve", lambda e, i=i: e.memset(Rpad4[i][:, :, :], 0.0), writes=B_Rp4[i])
                S_.barrier()


                def proj_fm(col, pb):
                    for kc in range(8):
                        op("pe", lambda e, kc=kc: e.matmul(
                            pj[pb][:, :], lhsT=W_AB[:, kc, col:col + 128], rhs=hnT[:, kc, :],
                            start=(kc == 0), stop=(kc == 7)),
                           reads=[B_WAB] + B_hnT, writes=[B_pj[pb]])

                def rstd_from(var_t, B_var):
                    op("act", lambda e: e.activation(out=var_t[:, :], in_=var_t[:, :], func=AF.Ln,
                                                     bias=eps_rms[:, 1:2]),
                       reads=[B_var], writes=[B_var])
                    op("act", lambda e: e.activation(out=var_t[:, :], in_=var_t[:, :], func=AF.Exp, scale=-0.5),
                       reads=[B_var], writes=[B_var])

                def sigmoid_act(psrc, B_psrc, tmpt, B_tmp):
                    op("act", lambda e: e.activation(out=tmpt[:, :], in_=psrc, func=AF.Exp, scale=-1.0),
                       reads=[B_psrc], writes=[B_tmp])
                    op("act", lambda e: e.activation(out=tmpt[:, :], in_=tmpt[:, :], func=AF.Ln, bias=eps_rms[:, 2:3]),
                       reads=[B_tmp], writes=[B_tmp])
                    op("act", lambda e: e.activation(out=tmpt[:, :], in_=tmpt[:, :], func=AF.Exp, scale=-1.0),
                       reads=[B_tmp], writes=[B_tmp])

                def s1(c):
                    P = c % 2
                    c0 = c * 512
                    xt, B_xt, stat, B_stat = xt2[P], B_xt2[P], stat2[P], B_stat2[P]
                    u_ext, B_u = u_ext2[P], B_u2[P]
                    u_prev, B_uprev = u_ext2[1 - P], B_u2[1 - P]
                    qz, qx_bf, k_bf, gs_r, kzpad, vpad = qz2[P], qx2[P], k2[P], gsr2[P], kz2[P], vp2[P]
                    B_q, B_qx, B_k, B_gsr, B_kz, B_vp = B_q2[P], B_qx2[P], B_k2[P], B_gsr2[P], B_kz2[P], B_vp2[P]
                    gsA, B_gsA = gsA2[P], B_gsA2[P]
                    c0 = c * 512
                    for p in range(4):
                        op("sp", lambda e, p=p, c=c, c0=c0: e.dma_start(out=ymc[c % 2][:, p, :], in_=ym_d[p, :, c0:c0 + 512]),
                           writes=[B_ymc[c % 2]], dma=True)
                        yield
                    for tt in range(4):
                        T = 4 * c + tt
                        load_norm_transpose(src, T, xt[tt], B_xt[tt], hn, B_hn, stat[tt], B_stat[tt],
                                            hnT, B_hnT[tt], tt)
                        yield
                    for g in range(2):
                        if c > 0:
                            op("pool", lambda e, g=g: e.tensor_copy(out=u_ext[:, g, 0:30], in_=u_prev[:, g, 512:542]),
                               reads=[B_uprev[g]], writes=[B_u[g]])
                        pbv = K['pji'] % 2
                        K['pji'] += 1
                        proj_fm(g * 128, pbv)
                        sbi = K['sti'] % 3
                        K['sti'] += 1
                        for kc in range(8):
                            op("pe", lambda e, kc=kc, g=g, sbi=sbi: e.matmul(
                                st[sbi][:, :], lhsT=W_AB[:, kc, 256 + g * 128:256 + (g + 1) * 128], rhs=hnT[:, kc, :],
                                start=(kc == 0), stop=(kc == 7)),
                               reads=[B_WAB] + B_hnT, writes=[B_st[sbi]])
                        op("dve", lambda e, pbv=pbv: e.tensor_copy(out=f32t[5][:, :], in_=pj[pbv][:, :]),
                           reads=[B_pj[pbv]], writes=[B_f[5]])
                        sigmoid_act(st[sbi][:, :], B_st[sbi], f32t[0], B_f[0])
                        op("dve", lambda e, g=g: e.tensor_tensor(out=u_ext[:, g, 30:542], in0=f32t[5][:, :],
                                                                 in1=f32t[0][:, :], op=ALU.mult),
                           reads=[B_f[5], B_f[0]], writes=[B_u[g]])
                        yield
                    for rp in range(2):
                        pb = K['pji'] % 2
                        K['pji'] += 1
                        proj_fm(768 + rp * 128, pb)
                        for hh in range(2):
                            op("act", lambda e, rp=rp, pb=pb, hh=hh: e.activation(
                                out=qz[hh * 64:(hh + 1) * 64, rp, hh, :], in_=pj[pb][hh * 64:(hh + 1) * 64, :], func=AF.Copy),
                               reads=[B_pj[pb]], writes=[B_q[rp]])
                        op("dve", lambda e, rp=rp, pb=pb: e.tensor_tensor(
                            out=qx_bf[:, rp, :].rearrange("p (a t) -> p a t", a=4),
                            in0=pj[pb][:, :].rearrange("p (a t) -> p a t", a=4),
                            in1=xi[:, rp:rp + 1, :].broadcast_to([128, 4, 128]), op=ALU.mult),
                           reads=[B_pj[pb]], writes=[B_qx[rp]])
                        sbi = K['sti'] % 3
                        K['sti'] += 1
                        for kc in range(8):
                            op("pe", lambda e, kc=kc, rp=rp, sbi=sbi: e.matmul(
                                st[sbi][:, :], lhsT=W_AB[:, kc, 1024 + rp * 128:1024 + (rp + 1) * 128], rhs=hnT[:, kc, :],
                                start=(kc == 0), stop=(kc == 7)),
                               reads=[B_WAB] + B_hnT, writes=[B_st[sbi]])
                        op("dve", lambda e, rp=rp, sbi=sbi: e.tensor_copy(out=k_bf[:, rp, :], in_=st[sbi][:, :]),
                           reads=[B_st[sbi]], writes=[B_k[rp]])
                        yield
                    for tt in range(4):
                        pb = K['pji'] % 2
                        K['pji'] += 1
                        for kc in range(8):
                            op("pe", lambda e, kc=kc, tt=tt, pb=pb: e.matmul(
                                pj[pb][:, :], lhsT=hnT[:, kc, tt * 128:(tt + 1) * 128], rhs=W_AB[:, kc, 1024:1536],
                                start=(kc == 0), stop=(kc == 7)),
                               reads=[B_WAB, B_hnT[tt]], writes=[B_pj[pb]])
                        ksrc = pj[pb][:, 0:256].rearrange("p (q two e) -> p q two e", two=2, e=64)
                        vsrc = pj[pb][:, 256:512].rearrange("p (q two e) -> p q two e", two=2, e=64)
                        zsrc = zeta[:, :, :].rearrange("p (q two) o -> p q two o", two=2)
                        for two in range(2):
                            op("dve", lambda e, tt=tt, two=two, ksrc=ksrc, zsrc=zsrc: e.tensor_tensor(
                                out=kzpad[:, tt, :, two * 128:two * 128 + 64], in0=ksrc[:, :, two, :],
                                in1=zsrc[:, :, two, :].broadcast_to([128, 2, 64]), op=ALU.mult),
                               reads=[B_pj[pb]], writes=[B_kz[tt]])
                            op("dve", lambda e, tt=tt, two=two, vsrc=vsrc: e.tensor_copy(
                                out=vpad[:, tt, :, two * 128:two * 128 + 64], in_=vsrc[:, :, two, :]),
                               reads=[B_pj[pb]], writes=[B_vp[tt]])
                        yield
                    for co in range(2):
                        sbi = K['sti'] % 3
                        K['sti'] += 1
                        for kc in range(8):
                            op("pe", lambda e, kc=kc, co=co, sbi=sbi: e.matmul(
                                st[sbi][:, :], lhsT=W_AB[:, kc, 512 + co * 128:512 + (co + 1) * 128], rhs=hnT[:, kc, :],
                                start=(kc == 0), stop=(kc == 7)),
                               reads=[B_WAB] + B_hnT, writes=[B_st[sbi]])
                        gs, B_gs = gsA[co], B_gsA[co]
                        op("act", lambda e, sbi=sbi, gs=gs: e.activation(out=gs[:, :], in_=st[sbi][:, :], func=AF.Copy),
                           reads=[B_st[sbi]], writes=[B_gs])
                        sigmoid_act(st[sbi][:, :], B_st[sbi], f32t[0], B_f[0])
                        op("dve", lambda e, gs=gs: e.tensor_tensor(out=gs[:, :], in0=gs[:, :], in1=f32t[0][:, :],
                                                                   op=ALU.mult),
                           reads=[B_f[0], B_gs], writes=[B_gs])
                        yield
                    for rp in range(2):
                        sbi = K['sti'] % 3
                        K['sti'] += 1
                        for kc in range(8):
                            op("pe", lambda e, kc=kc, rp=rp, sbi=sbi: e.matmul(
                                st[sbi][:, :], lhsT=W_AB[:, kc, 1536 + rp * 128:1536 + (rp + 1) * 128], rhs=hnT[:, kc, :],
                                start=(kc == 0), stop=(kc == 7)),
                               reads=[B_WAB] + B_hnT, writes=[B_st[sbi]])
                        op("act", lambda e, rp=rp, sbi=sbi: e.activation(out=gs_r[:, rp, :], in_=st[sbi][:, :], func=AF.Copy),
                           reads=[B_st[sbi]], writes=[B_gsr[rp]])
                        sigmoid_act(st[sbi][:, :], B_st[sbi], f32t[6], B_f[6])
                        op("dve", lambda e, rp=rp: e.tensor_tensor(out=gs_r[:, rp, :], in0=gs_r[:, rp, :],
                                                                   in1=f32t[6][:, :], op=ALU.mult),
                           reads=[B_f[6]], writes=[B_gsr[rp]])
                        yield
                def s2(c):
                    P = c % 2
                    c0 = c * 512
                    xt, B_xt, stat, B_stat = xt2[P], B_xt2[P], stat2[P], B_stat2[P]
                    u_ext, B_u = u_ext2[P], B_u2[P]
                    u_prev, B_uprev = u_ext2[1 - P], B_u2[1 - P]
                    qz, qx_bf, k_bf, gs_r, kzpad, vpad = qz2[P], qx2[P], k2[P], gsr2[P], kz2[P], vp2[P]
                    B_q, B_qx, B_k, B_gsr, B_kz, B_vp = B_q2[P], B_qx2[P], B_k2[P], B_gsr2[P], B_kz2[P], B_vp2[P]
                    gsA, B_gsA = gsA2[P], B_gsA2[P]
                    for g in range(2):
                        ab = K['acci'] % 2
                        K['acci'] += 1
                        for k in range(31):
                            op("pe", lambda e, g=g, k=k, ab=ab: e.matmul(
                                acc[ab][:, :], lhsT=diag[:, g, k, :], rhs=u_ext[:, g, k:k + 512],
                                start=(k == 0), stop=(k == 30)),
                               reads=[B_u[g]], writes=[B_acc[ab]])
                        op("dve", lambda e, g=g, ab=ab: e.tensor_scalar(out=y32[:, g, :], in0=acc[ab][:, :],
                                                                        scalar1=cv_t[:, 0, g:g + 1], scalar2=None, op0=ALU.add),
                           reads=[B_acc[ab]], writes=[B_y32[g]])
                        op("act", lambda e, g=g: e.activation(out=ybf[:, g, :], in_=y32[:, g, :], func=AF.Copy),
                           reads=[B_y32[g]], writes=[B_ybf[g]])
                        op("act", lambda e, g=g: e.activation(out=ysq[:, g, :], in_=y32[:, g, :], func=AF.Square),
                           reads=[B_y32[g]], writes=[B_ysq[g]])
                        yield
                    mb = K['pji'] % 2
                    K['pji'] += 1
                    for g in range(2):
                        op("pe", lambda e, g=g, mb=mb: e.matmul(pj[mb][:, :], lhsT=o256[:, :], rhs=ybf[:, g, :],
                                                               start=(g == 0), stop=(g == 1)),
                           reads=[B_ybf[g]], writes=[B_pj[mb]])
                        yield
                    vb = K['pji'] % 2
                    K['pji'] += 1
                    for g in range(2):
                        op("pe", lambda e, g=g, vb=vb: e.matmul(pj[vb][:, :], lhsT=o256[:, :], rhs=ysq[:, g, :],
                                                               start=(g == 0), stop=(g == 1)),
                           reads=[B_ysq[g]], writes=[B_pj[vb]])
                        yield
                    mu, B_mu = f32t[1], B_f[1]
                    var, B_var = f32t[2], B_f[2]
                    op("act", lambda e, mb=mb: e.activation(out=mu[:, :], in_=pj[mb][:, :], func=AF.Copy),
                       reads=[B_pj[mb]], writes=[B_mu])
                    op("act", lambda e, mb=mb: e.activation(out=var[:, :], in_=pj[mb][:, :], func=AF.Square),
                       reads=[B_pj[mb]], writes=[B_var])
                    op("dve", lambda e, vb=vb: e.tensor_tensor(out=var[:, :], in0=pj[vb][:, :], in1=var[:, :],
                                                               op=ALU.subtract),
                       reads=[B_pj[vb], B_var], writes=[B_var])
                    rstd_from(var, B_var)
                    oacc = []
                    for rp in range(2):
                        oacc.append(K['acci'] % 2)
                        K['acci'] += 1
                    pis = []
                    for tt in range(4):
                        t0 = tt * 128
                        sbi = K['sti'] % 3
                        K['sti'] += 1
                        pi = tt
                        pis.append(pi)
                        for h in range(4):
                            rp, hh = h // 2, h % 2
                            op("pe", lambda e, h=h, rp=rp, hh=hh, t0=t0, sbi=sbi: e.matmul(
                                st[sbi][:, h * 128:(h + 1) * 128], lhsT=k_bf[:, rp, t0:t0 + 128],
                                rhs=qz[:, rp, hh, t0:t0 + 128], start=True, stop=True),
                               reads=[B_k[rp], B_q[rp]], writes=[B_st[sbi]])
                        op("dve", lambda e, sbi=sbi, pi=pi: e.tensor_tensor(
                            out=pTr[pi][:, :, :], in0=st[sbi][:, :].rearrange("p (h i) -> p h i", h=4),
                            in1=dec[:, :, :], op=ALU.mult),
                           reads=[B_st[sbi]], writes=[B_pTr[pi]])
                    yield
                    kb = K['pji'] % 2
                    K['pji'] += 1
                    firstkv = True
                    for tt in range(4):
                        for rp in range(2):
                            cb = (tt * 2 + rp) * 64
                            for hh in range(2):
                                op("pe", lambda e, rp=rp, hh=hh, tt=tt, kb=kb, cb=cb, firstkv=firstkv: e.matmul(
                                    pj[kb][:, cb:cb + 64], lhsT=kzpad[:, tt, rp, hh * 64:hh * 64 + 128],
                                    rhs=vpad[:, tt, rp, hh * 128:hh * 128 + 64], start=firstkv, stop=(tt == 3 and rp == 1 and hh == 1),
                                    skip_group_check=True),
                                   reads=[B_kz[tt], B_vp[tt]], writes=[B_pj[kb]])
                                firstkv = False
                    for tt in range(4):
                        t0 = tt * 128
                        for rp in range(2):
                            ab = oacc[rp]
                            for hh in range(2):
                                h = 2 * rp + hh
                                op("pe", lambda e, h=h, rp=rp, hh=hh, tt=tt, t0=t0, ab=ab: e.matmul(
                                    acc[ab][:, t0:t0 + 128], lhsT=vpad[:, tt, rp, hh * 64:hh * 64 + 128],
                                    rhs=pTr[tt][:, h, :], start=(tt == 0 and hh == 0), stop=False, skip_group_check=True),
                                   reads=[B_vp[tt], B_pTr[tt]], writes=[B_acc[ab]])
                    yield
                    for tt in range(4):
                        for rp in range(2):
                            cb = (tt * 2 + rp) * 64
                            op("dve", lambda e, rp=rp, tt=tt: e.tensor_copy(out=Rpad4[tt][0:64, rp, 0:64], in_=Rm[0:64, rp, :]),
                               reads=[B_Rm[rp]], writes=[B_Rp4[tt][rp]])
                            op("dve", lambda e, rp=rp, tt=tt: e.tensor_copy(out=Rpad4[tt][64:128, rp, 64:128], in_=Rm[64:128, rp, :]),
                               reads=[B_Rm[rp]], writes=[B_Rp4[tt][rp]])
                            op("dve", lambda e, rp=rp, kb=kb, cb=cb: e.scalar_tensor_tensor(
                                out=Rm[:, rp, :], in0=Rm[:, rp, :], scalar=gv[:, rp, 0:1], in1=pj[kb][:, cb:cb + 64],
                                op0=ALU.mult, op1=ALU.add),
                               reads=[B_pj[kb], B_Rm[rp]], writes=[B_Rm[rp]])
                    for tt in range(4):
                        t0 = tt * 128
                        for rp in range(2):
                            ab = oacc[rp]
                            op("pe", lambda e, rp=rp, t0=t0, ab=ab, tt=tt: e.matmul(
                                acc[ab][:, t0:t0 + 128], lhsT=Rpad4[tt][:, rp, :],
                                rhs=qx_bf[:, rp, t0:t0 + 128], start=False, stop=(tt == 3), skip_group_check=True),
                               reads=[B_Rp4[tt][rp], B_qx[rp]], writes=[B_acc[ab]])
                    yield
                    for g in range(2):
                        d1, B_d1 = f32t[3], B_f[3]
                        e1, B_e1 = f32t[4], B_f[4]
                        op("dve", lambda e, g=g: e.tensor_tensor(out=d1[:, :], in0=y32[:, g, :], in1=mu[:, :],
                                                                 op=ALU.subtract),
                           reads=[B_y32[g], B_mu], writes=[B_d1])
                        op("dve", lambda e: e.tensor_tensor(out=d1[:, :], in0=d1[:, :], in1=var[:, :], op=ALU.mult),
                           reads=[B_d1, B_var], writes=[B_d1])
                        op("dve", lambda e, g=g: e.tensor_scalar(out=d1[:, :], in0=d1[:, :],
                                                                 scalar1=cv_t[:, 1, g:g + 1], scalar2=cv_t[:, 2, g:g + 1],
                                                                 op0=ALU.mult, op1=ALU.add),
                           reads=[B_d1], writes=[B_d1])
                        sigmoid_act(d1[:, :], B_d1, e1, B_e1)
                        op("dve", lambda e, g=g: e.tensor_tensor(out=s_bf[:, g, :], in0=d1[:, :], in1=e1[:, :],
                                                                 op=ALU.mult),
                           reads=[B_d1, B_e1], writes=[B_s[g]])
                        yield
                    for co in range(2):
                        gs, B_gs = gsA[co], B_gsA[co]
                        ppb = K['pji'] % 2
                        K['pji'] += 1
                        for ci in range(2):
                            op("pe", lambda e, ci=ci, co=co, ppb=ppb: e.matmul(
                                pj[ppb][:, :], lhsT=pw[:, ci, co * 128:(co + 1) * 128], rhs=s_bf[:, ci, :],
                                start=(ci == 0), stop=(ci == 1)),
                               reads=[B_s[ci]], writes=[B_pj[ppb]])
                        op("dve", lambda e, co=co, ppb=ppb, gs=gs: e.scalar_tensor_tensor(
                            out=cat[:, co, :], in0=pj[ppb][:, :], scalar=cv_t[:, 3, co:co + 1], in1=gs[:, :],
                            op0=ALU.add, op1=ALU.mult),
                           reads=[B_pj[ppb], B_gs], writes=[B_cat[co]])
                        yield
                    for rp in range(2):
                        ab = oacc[rp]
                        op("act", lambda e, ab=ab: e.activation(out=obf[:, :], in_=acc[ab][:, :], func=AF.Copy),
                           reads=[B_acc[ab]], writes=[B_obf])
                        op("act", lambda e, ab=ab: e.activation(out=osq[:, :], in_=acc[ab][:, :], func=AF.Square),
                           reads=[B_acc[ab]], writes=[B_osq])
                        mb = K['pji'] % 2
                        K['pji'] += 1
                        op("pe", lambda e, mb=mb: e.matmul(pj[mb][:, :], lhsT=bones[:, :], rhs=obf[:, :], start=True, stop=True),
                           reads=[B_obf], writes=[B_pj[mb]])
                        vb = K['pji'] % 2
                        K['pji'] += 1
                        op("pe", lambda e, vb=vb: e.matmul(pj[vb][:, :], lhsT=bones[:, :], rhs=osq[:, :], start=True, stop=True),
                           reads=[B_osq], writes=[B_pj[vb]])
                        mu, B_mu = f32t[1], B_f[1]
                        var, B_var = f32t[2], B_f[2]
                        d1, B_d1 = f32t[3], B_f[3]
                        op("act", lambda e, mb=mb: e.activation(out=mu[:, :], in_=pj[mb][:, :], func=AF.Copy),
                           reads=[B_pj[mb]], writes=[B_mu])
                        op("act", lambda e, mb=mb: e.activation(out=var[:, :], in_=pj[mb][:, :], func=AF.Square),
                           reads=[B_pj[mb]], writes=[B_var])
                        op("dve", lambda e, vb=vb: e.tensor_tensor(out=var[:, :], in0=pj[vb][:, :], in1=var[:, :],
                                                                   op=ALU.subtract),
                           reads=[B_pj[vb], B_var], writes=[B_var])
                        rstd_from(var, B_var)
                        op("dve", lambda e, ab=ab: e.tensor_tensor(out=d1[:, :], in0=acc[ab][:, :], in1=mu[:, :],
                                                                   op=ALU.subtract),
                           reads=[B_acc[ab], B_mu], writes=[B_d1])
                        op("dve", lambda e: e.tensor_tensor(out=d1[:, :], in0=d1[:, :], in1=var[:, :], op=ALU.mult),
                           reads=[B_d1, B_var], writes=[B_d1])
                        op("dve", lambda e, rp=rp: e.tensor_tensor(out=cat[:, 2 + rp, :], in0=d1[:, :], in1=gs_r[:, rp, :],
                                                                   op=ALU.mult),
                           reads=[B_d1, B_gsr[rp]], writes=[B_cat[2 + rp]])
                        yield
                    for tt in range(4):
                        T = 4 * c + tt
                        t0 = tt * 128
                        xb_i = tt
                        for half in range(2):
                            sbi = K['sti'] % 3
                            K['sti'] += 1
                            for kc in range(8):
                                if kc < 4:
                                    lt = cat[:, kc, t0:t0 + 128]
                                    rd = [B_cat[kc]]
                                else:
                                    lt = ymc[c % 2][:, kc - 4, t0:t0 + 128]
                                    rd = [B_ymc[c % 2]]
                                op("pe", lambda e, kc=kc, lt=lt, half=half, sbi=sbi: e.matmul(
                                    st[sbi][:, :], lhsT=lt, rhs=Wo[:, kc, half * 512:(half + 1) * 512],
                                    start=(kc == 0), stop=(kc == 7)),
                                   reads=rd, writes=[B_st[sbi]])
                            op("dve", lambda e, tt=tt, half=half, sbi=sbi, xb_i=xb_i: e.tensor_tensor(
                                out=xt[xb_i][:, half * 512:(half + 1) * 512], in0=st[sbi][:, :],
                                in1=xt[tt][:, half * 512:(half + 1) * 512], op=ALU.add),
                               reads=[B_st[sbi], B_xt[tt]], writes=[B_xt[xb_i]])
                        if not last:
                            op("sp", lambda e, T=T, xb_i=xb_i: e.dma_start(out=x1[T * 128:(T + 1) * 128, :], in_=xt[xb_i][:, :]),
                               reads=[B_xt[xb_i]], dma=True)
                        else:
                            stt = fstat[tt]
                            op("act", lambda e, xb_i=xb_i, stt=stt: e.activation(out=fjunk[:, :], in_=xt[xb_i][:, :], func=AF.Square,
                                                                                 accum_out=stt[:, 0:1]),
                               reads=[B_xt[xb_i]], writes=[B_fjunk, B_fstat[tt]])
                            op("act", lambda e, stt=stt: e.activation(out=stt[:, 1:2], in_=stt[:, 0:1], func=AF.Ln,
                                                                      scale=1.0 / D, bias=eps_rms[:, 0:1]),
                               reads=[B_fstat[tt]], writes=[B_fstat[tt]])
                            op("act", lambda e, stt=stt: e.activation(out=stt[:, 2:3], in_=stt[:, 1:2], func=AF.Exp, scale=-0.5),
                               reads=[B_fstat[tt]], writes=[B_fstat[tt]])
                            op("dve", lambda e, xb_i=xb_i, stt=stt: e.scalar_tensor_tensor(
                                out=xt[xb_i][:, :], in0=xt[xb_i][:, :], scalar=stt[:, 2:3], in1=fgt[:, :],
                                op0=ALU.mult, op1=ALU.mult),
                               reads=[B_xt[xb_i], B_fstat[tt]], writes=[B_xt[xb_i]])
                            op("sp", lambda e, T=T, xb_i=xb_i: e.dma_start(out=out[T * 128:(T + 1) * 128, :], in_=xt[xb_i][:, :]),
                               reads=[B_xt[xb_i]], dma=True)

                        yield

                def drain(g):
                    for _ in g:
                        pass

                drain(s1(0))
                for c in range(NCH):
                    g2 = s2(c)
                    g1 = s1(c + 1) if c + 1 < NCH else iter(())
                    a2 = a1 = True
                    while a2 or a1:
                        if a2:
                            a2 = next(g2, "END") != "END"
                        if a1:
                            a1 = next(g1, "END") != "END"
                S_.barrier()
        S_.barrier()
        print("KERNEL nops", S_.nops, {k: e["count"] for k, e in S_.engs.items()})
    return nc


def _bf(a):
    return np.ascontiguousarray(a.astype(ml_dtypes.bfloat16))


def make_consts(S):
    c = {}
    c["c_ident"] = _bf(np.eye(128, dtype=np.float32))
    j = np.arange(128)
    c["c_causal"] = _bf(np.where(j[:, None] <= j[None, :], 0.0, MASKV).astype(np.float32))
    pos = np.arange(S)
    ka = np.zeros((20, S), np.float32)
    for n in range(16):
        ka[n] = np.where(pos // 256 == n, MASKV, 0.0)
    ka[16] = pos % 128
    ka[17] = pos // 128
    ka[18] = 1.0
    ka[19] = 1.0
    c["c_kaug"] = _bf(ka)
    qa = np.zeros((4, 8, S), np.float32)
    for h in range(8):
        slope = 2.0 ** (-(h + 1))
        qa[0, h] = slope * 8.0
        qa[1, h] = slope * 1024.0
        qa[2, h] = -slope * 8.0 * (pos % 128)
        qa[3, h] = -slope * 1024.0 * (pos // 128)
    c["c_qaug"] = _bf(qa)
    hh = np.arange(4, dtype=np.float64)
    g = 1.0 - np.exp2(-5.0 - hh)
    i = np.arange(128, dtype=np.float64)
    diff = i[None, :] - i[:, None]
    dec = np.zeros((128, 4, 128), np.float64)
    for h in range(4):
        dec[:, h, :] = np.where(diff >= 0, g[h] ** np.maximum(diff, 0.0), 0.0) * 0.125
    c["c_dec"] = dec.astype(np.float32)
    zeta = np.zeros((128, 4, 1), np.float64)
    for h in range(4):
        zeta[:, h, 0] = g[h] ** (127 - i) * 0.125
    c["c_zeta"] = zeta.astype(np.float32)
    xi = np.zeros((128, 2, 512), np.float64)
    gv = np.zeros((128, 2, 1), np.float64)
    ii = np.arange(512) % 128
    for rp in range(2):
        for hf in range(2):
            h = 2 * rp + hf
            xi[hf * 64:(hf + 1) * 64, rp, :] = (g[h] ** (ii + 1.0))[None, :]
            gv[hf * 64:(hf + 1) * 64, rp, 0] = g[h] ** 128
    c["c_xi"] = xi.astype(np.float32)
    c["c_gv"] = gv.astype(np.float32)
    bo = np.zeros((128, 128), np.float32)
    bo[0:64, 0:64] = 1.0 / 64
    bo[64:128, 64:128] = 1.0 / 64
    c["c_bones"] = _bf(bo)
    c["c_o256"] = _bf(np.full((128, 128), 1.0 / 256, np.float32))
    return c


def prep_shared(norm_g, w_in, conv_w, conv_b, conv_ln_g, conv_ln_b, conv_pw_w, conv_pw_b, w_out, final_g, S):
    DEPTH = w_in.shape[0]
    f = lambda a: np.ascontiguousarray(np.asarray(a, dtype=np.float32))
    m = {}
    m["w_in"] = f(w_in)
    m["w_out"] = f(w_out)
    m["pw_w"] = f(conv_pw_w)
    m["ng"] = f(np.asarray(norm_g).reshape(DEPTH, 8, 128).transpose(0, 2, 1))
    m["cwT"] = f(np.asarray(conv_w).transpose(0, 2, 1).reshape(DEPTH, 2, 128, 31).transpose(0, 2, 1, 3))
    cv = np.stack([np.asarray(conv_b), np.asarray(conv_ln_g), np.asarray(conv_ln_b), np.asarray(conv_pw_b)], axis=1)
    m["cvec"] = f(cv.reshape(DEPTH, 4, 2, 128).transpose(0, 3, 1, 2))
    m["fg"] = f(np.broadcast_to(np.asarray(final_g)[None, :], (128, D)))
    m.update(make_consts(S))
    return m


_CACHE = {}


def run(x, shared, S, DEPTH):
    B = x.shape[0]
    key = (S, DEPTH)
    if key not in _CACHE:
        _CACHE[key] = build_program(S, DEPTH)
    nc = _CACHE[key]
    in_maps = []
    for b in range(B):
        d = dict(shared)
        d["x"] = np.ascontiguousarray(x[b])
        in_maps.append(d)
    res = run_bass_kernel_spmd(nc, in_maps, core_ids=list(range(B)))
    return np.stack([np.asarray(r["out"]) for r in res.results], axis=0).astype(np.float32)


def kernel(x, norm_g, w_in, conv_w, conv_b, conv_ln_g, conv_ln_b, conv_pw_w, conv_pw_b, w_out, final_g):
    x = np.asarray(x, dtype=np.float32)
    S = x.shape[1]
    DEPTH = np.asarray(w_in).shape[0]
    shared = prep_shared(norm_g, w_in, conv_w, conv_b, conv_ln_g, conv_ln_b, conv_pw_w, conv_pw_b,
                         w_out, final_g, S)
    return run(x, shared, S, DEPTH)
```

```python
import os
import numpy as np
import ml_dtypes
from contextlib import ExitStack
import concourse.bass as bass
import concourse.mybir as mybir
from concourse.bass_utils import run_bass_kernel_spmd

F32 = mybir.dt.float32
BF16 = mybir.dt.bfloat16
ALU = mybir.AluOpType
AF = mybir.ActivationFunctionType

D = 1024
INW = 3840
NEG = -1.0e30
MASKV = -30000.0
RMS_EPS = 1e-6
LN_EPS = 1e-5
SCALE = 0.125


class Buf:
    __slots__ = ("name", "w", "r", "excl")

    def __init__(self, name, excl=False):
        self.name = name
        self.w = None
        self.r = {}
        self.excl = excl


class Sched:
    def __init__(self, nc, es, n_dma=12):
        self.nc = nc
        self.sems = []
        self.h = {"pe": nc.tensor, "act": nc.scalar, "dve": nc.vector, "pool": nc.gpsimd, "sp": nc.sync}
        self.engs = {}
        for k in self.h:
            idx = self._sem(es, "s_" + k)
            self.engs[k] = {"sem": idx, "count": 0, "seen": {}}
        self.pool = {}
        self.rr = {}
        for q in ("sp", "pool", "act"):
            self.pool[q] = [{"idx": self._sem(es, "d_%s%d" % (q, i)), "val": 0} for i in range(n_dma)]
            self.rr[q] = 0
        self.nops = 0
        self.limit = int(os.environ.get("KLIMIT", "0")) or None

    def _sem(self, es, name):
        self.sems.append(es.enter_context(self.nc.semaphore(name)))
        return len(self.sems) - 1

    def op(self, eng, fn, reads=(), writes=(), dma=False):
        if self.limit is not None and self.nops >= self.limit:
            return None
        e = self.engs[eng]
        waits = {}

        def need(src, sidx, val, raw):
            if src == eng and eng == "pe":
                return
            if e["seen"].get(sidx, 0) >= val:
                return
            if waits.get(sidx, 0) < val:
                waits[sidx] = val

        for b in reads:
            if b.w is not None:
                need(b.w[0], b.w[1], b.w[2], True)
            if b.excl:
                for sidx, (src, val) in b.r.items():
                    if src != eng:
                        need(src, sidx, val, False)
        for b in writes:
            if b.w is not None:
                need(b.w[0], b.w[1], b.w[2], False)
            for sidx, (src, val) in b.r.items():
                need(src, sidx, val, False)
        if dma:
            pl = self.pool[eng]
            slot = pl[self.rr[eng] % len(pl)]
            self.rr[eng] += 1
            if slot["val"] > 0:
                need(None, slot["idx"], slot["val"], False)
            slot["val"] += 16
            sig = (None, slot["idx"], slot["val"])
            inc = 16
        else:
            e["count"] += 1
            sig = (eng, e["sem"], e["count"])
            inc = 1
        h = self.h[eng]
        for sidx, val in waits.items():
            e["seen"][sidx] = val
            h.wait_ge(self.sems[sidx], val)
        ins = fn(h)
        if os.environ.get("KTRACE"):
            import inspect
            fr = inspect.stack()[1]
            print("OP", self.nops, eng, fr.lineno, (fr.code_context or [""])[0].strip()[:90])
        ins.then_inc(self.sems[sig[1]], inc)
        self.nops += 1
        for b in writes:
            b.w = sig
            b.r = {}
        for b in reads:
            if b in writes:
                continue
            cur = b.r.get(sig[1])
            if cur is None or cur[1] < sig[2]:
                b.r[sig[1]] = (sig[0], sig[2])
        return sig

    def barrier(self):
        sigs = []
        for k, e in self.engs.items():
            if e["count"] > 0:
                sigs.append((e["sem"], e["count"]))
        for q, pl in self.pool.items():
            for slot in pl:
                if slot["val"] > 0:
                    sigs.append((slot["idx"], slot["val"]))
        for k, e in self.engs.items():
            h = self.h[k]
            for sidx, val in sigs:
                if e["seen"].get(sidx, 0) >= val:
                    continue
                e["seen"][sidx] = val
                h.wait_ge(self.sems[sidx], val)


def build_program(S, DEPTH):
    NT = S // 128
    NCH = S // 512
    nc = bass.Bass("TRN2", target_bir_lowering=False)

    def dr(name, shape, dt, kind="ExternalInput"):
        return nc.dram_tensor(name, shape, dt, kind=kind).ap()

    x_in = dr("x", [S, D], F32)
    out = dr("out", [S, D], F32, "ExternalOutput")
    x1 = dr("x1s", [S, D], F32, "Internal")
    ym_d = dr("ym_d", [4, 128, S], BF16, "Internal")
    w_in = dr("w_in", [DEPTH, D, INW], F32)
    w_out = dr("w_out", [DEPTH, D, D], F32)
    pw_w = dr("pw_w", [DEPTH, 256, 256], F32)
    ng = dr("ng", [DEPTH, 128, 8], F32)
    cwT = dr("cwT", [DEPTH, 128, 2, 31], F32)
    cvec = dr("cvec", [DEPTH, 128, 4, 2], F32)
    fg = dr("fg", [128, D], F32)
    d_ident = dr("c_ident", [128, 128], BF16)
    d_causal = dr("c_causal", [128, 128], BF16)
    d_kaug = dr("c_kaug", [20, S], BF16)
    d_qaug = dr("c_qaug", [4, 8, S], BF16)
    d_dec = dr("c_dec", [128, 4, 128], F32)
    d_zeta = dr("c_zeta", [128, 4, 1], F32)
    d_xi = dr("c_xi", [128, 2, 512], F32)
    d_gv = dr("c_gv", [128, 2, 1], F32)
    d_bones = dr("c_bones", [128, 128], BF16)
    d_o256 = dr("c_o256", [128, 128], BF16)

    with ExitStack() as es:
        S_ = Sched(nc, es)
        op = S_.op

        uniq = [0]

        def sb(stack, name, shape, dt):
            uniq[0] += 1
            return stack.enter_context(nc.sbuf_tensor("%s_%d" % (name, uniq[0]), shape, dt))

        def ps(name, shape, dt):
            return es.enter_context(nc.psum_tensor(name, shape, dt))

        ident = sb(es, "ident", [128, 128], BF16)
        causal = sb(es, "causal", [128, 128], BF16)
        bones = sb(es, "bones", [128, 128], BF16)
        o256 = sb(es, "o256", [128, 128], BF16)
        B_const = Buf("const")
        B_WM = Buf("W_M")

        tps = ps("tps", [128, 1024], BF16)
        pj = [ps("pj%d" % i, [128, 512], F32) for i in range(2)]
        st = [ps("st%d" % i, [128, 512], F32) for i in range(3)]
        acc = [ps("acc%d" % i, [128, 512], F32) for i in range(2)]
        B_tps = Buf("tps", True)
        B_pj = [Buf("pj0", True), Buf("pj1", True)]
        B_st = [Buf("st%d" % i, True) for i in range(3)]
        B_acc = [Buf("acc0", True), Buf("acc1", True)]

        for t, d in ((ident, d_ident), (causal, d_causal), (bones, d_bones), (o256, d_o256)):
            op("sp", lambda e, t=t, d=d: e.dma_start(out=t[:, :], in_=d[:, :]), writes=[B_const], dma=True)
        S_.barrier()

        def load_norm(src, T, xt_t, B_xt, hn, B_hn, stat, B_stat):
            op("sp", lambda e: e.dma_start(out=xt_t[:, :], in_=src[T * 128:(T + 1) * 128, :]),
               writes=[B_xt], dma=True)
            op("act", lambda e: e.activation(out=hn[:, :], in_=xt_t[:, :], func=AF.Square,
                                             accum_out=stat[:, 0:1]),
               reads=[B_xt], writes=[B_hn, B_stat])
            op("act", lambda e: e.activation(out=stat[:, 1:2], in_=stat[:, 0:1], func=AF.Ln,
                                             scale=1.0 / D, bias=eps_rms[:, 0:1]),
               reads=[B_stat], writes=[B_stat])
            op("act", lambda e: e.activation(out=stat[:, 2:3], in_=stat[:, 1:2], func=AF.Exp, scale=-0.5),
               reads=[B_stat], writes=[B_stat])
            op("dve", lambda e: e.tensor_scalar(out=hn[:, :], in0=xt_t[:, :], scalar1=stat[:, 2:3],
                                                scalar2=None, op0=ALU.mult),
               reads=[B_xt, B_stat], writes=[B_hn])

        def transpose_hn(hn, B_hn, hnT, B_hnT_tt, tt):
            for kc in range(8):
                op("pe", lambda e, kc=kc: e.transpose(out=tps[:, kc * 128:(kc + 1) * 128],
                                                      in_=hn[:, kc * 128:(kc + 1) * 128], identity=ident[:, :]),
                   reads=[B_hn], writes=[B_tps])
            op("dve", lambda e: e.tensor_copy(out=hnT[:, :, tt * 128:(tt + 1) * 128],
                                              in_=tps[:, :].rearrange("p (k t) -> p k t", k=8)),
               reads=[B_tps], writes=[B_hnT_tt])

        def load_norm_transpose(src, T, xt_t, B_xt, hn, B_hn, stat, B_stat, hnT, B_hnT_tt, tt):
            load_norm(src, T, xt_t, B_xt, hn, B_hn, stat, B_stat)
            transpose_hn(hn, B_hn, hnT, B_hnT_tt, tt)

        def sigmoid_recip(psrc, B_psrc, tmpt, B_tmp, rows=slice(0, 128)):
            op("act", lambda e: e.activation(out=tmpt[rows, :], in_=psrc, func=AF.Exp, scale=-1.0),
               reads=[B_psrc], writes=[B_tmp])
            op("dve", lambda e: e.tensor_scalar(out=tmpt[rows, :], in0=tmpt[rows, :], scalar1=1.0,
                                                scalar2=None, op0=ALU.add),
               reads=[B_tmp], writes=[B_tmp])
            op("dve", lambda e: e.reciprocal(out=tmpt[rows, :], in_=tmpt[rows, :]),
               reads=[B_tmp], writes=[B_tmp])

        eps_rms = sb(es, "eps_rms", [128, 3], F32)
        op("dve", lambda e: e.memset(eps_rms[:, 2:3], 1.0), writes=[B_const])
        op("dve", lambda e: e.memset(eps_rms[:, 0:1], RMS_EPS), writes=[B_const])
        op("dve", lambda e: e.memset(eps_rms[:, 1:2], LN_EPS), writes=[B_const])
        S_.barrier()

        def load_weight_cols(stack, l, col0, ncols, Wdst, B_W, tagname, extra=None):
            NSTG = 4
            stg = [sb(stack, "stg%s%d" % (tagname, i), [128, 2048], F32) for i in range(NSTG)]
            B_stg = [Buf("stg%d" % i) for i in range(NSTG)]
            ngt = sb(stack, "ngt" + tagname, [128, 8], F32)
            B_ng = Buf("ng")
            op("sp", lambda e: e.dma_start(out=ngt[:, :], in_=ng[l, :, :]), writes=[B_ng], dma=True)
            h1 = (ncols // 2 + 127) // 128 * 128
            def issue(kc):
                b = kc % NSTG
                op("sp", lambda e: e.dma_start(out=stg[b][:, 0:h1],
                                               in_=w_in[l, kc * 128:(kc + 1) * 128, col0:col0 + h1]),
                   writes=[B_stg[b]], dma=True)
                op("pool", lambda e: e.dma_start(out=stg[b][:, h1:ncols],
                                                 in_=w_in[l, kc * 128:(kc + 1) * 128, col0 + h1:col0 + ncols]),
                   writes=[B_stg[b]], dma=True)

            for kc in range(NSTG):
                issue(kc)
            for kc in range(8):
                b = kc % NSTG
                op("dve" if kc % 2 == 0 else "act",
                   (lambda e, kc=kc, b=b: e.tensor_scalar(out=Wdst[:, kc, 0:ncols], in0=stg[b][:, 0:ncols],
                                                          scalar1=ngt[:, kc:kc + 1], scalar2=None, op0=ALU.mult))
                   if kc % 2 == 0 else
                   (lambda e, kc=kc, b=b: e.activation(out=Wdst[:, kc, 0:ncols], in_=stg[b][:, 0:ncols],
                                                       func=AF.Copy, scale=ngt[:, kc:kc + 1])),
                   reads=[B_stg[b], B_ng], writes=[B_W])
                if kc + NSTG < 8:
                    issue(kc + NSTG)
                if extra is not None:
                    extra(kc)

        for l in range(DEPTH):
            src = x_in if l == 0 else x1
            last = (l == DEPTH - 1)
            with ExitStack() as ph:
                W_M = sb(ph, "W_M", [128, 8, 2048], BF16)
                with ExitStack() as wl:
                    load_weight_cols(wl, l, 1792, 2048, W_M, B_WM, "m")
                    S_.barrier()
                K_aug = sb(ph, "K_aug", [84, 8, S], BF16)
                V_ext = sb(ph, "V_ext", [128, NT, 768], BF16)
                xt = [sb(ph, "xt%d" % i, [128, D], F32) for i in range(1)]
                hn = sb(ph, "hn", [128, D], BF16)
                stat = [sb(ph, "stat%d" % i, [128, 4], F32) for i in range(2)]
                hnT2 = [sb(ph, "hnT%d" % i, [128, 8, 512], BF16) for i in range(2)]
                Q_aug2 = [sb(ph, "Q_aug%d" % i, [84, 8, 512], BF16) for i in range(2)]
                gsl2 = [sb(ph, "gsl%d" % i, [128, 4, 512], BF16) for i in range(2)]
                pT = [sb(ph, "pT%d" % i, [128, 512], BF16) for i in range(3)]
                rec = sb(ph, "rt", [128, 512], F32)
                tmp = rec
                gtmp = sb(ph, "gtmp", [128, 512], F32)
                gx = sb(ph, "gx", [128, 512], F32)
                B_gx = Buf("gx")
                ymt = [sb(ph, "ymt%d" % i, [128, 512], BF16) for i in range(2)]
                ksum = sb(ph, "ksum", [64, 8, 16], F32)
                kmean = sb(ph, "kmean", [64, 8, 16], BF16)
                gate_sb4 = [sb(ph, "gate_sb%d" % i, [128, 8, 16], F32) for i in range(4)]
                top84 = [sb(ph, "top8%d" % i, [128, 8, 8], F32) for i in range(4)]
                mask_tm4 = [sb(ph, "mask_tm%d" % i, [128, 8, 16], BF16) for i in range(4)]

                B_xt = [Buf("xt0")]
                B_hn = Buf("hn")
                B_stat = [Buf("stat0"), Buf("stat1")]
                B_hnT2 = [[Buf("hnT%d_%d" % (q, i)) for i in range(4)] for q in range(2)]
                B_qq2 = [[Buf("qq%d_%d" % (q, h)) for h in range(8)] for q in range(2)]
                B_qm2 = [[Buf("qm%d_%d" % (q, t)) for t in range(4)] for q in range(2)]
                B_qc2 = [Buf("qc0"), Buf("qc1")]
                B_ka = [[Buf("ka%d_%d" % (h, c)) for c in range(NCH)] for h in range(8)]
                B_ve = [Buf("ve%d" % T) for T in range(NT)]
                B_gsl2 = [[Buf("gsl%d_%d" % (q, p)) for p in range(4)] for q in range(2)]
                B_pT = [Buf("pT%d" % i) for i in range(3)]
                B_rec = Buf("rt")
                B_tmp = B_rec
                B_gtmp = Buf("gtmp")
                B_ymt = [Buf("ymt0"), Buf("ymt1")]
                B_ksum = Buf("ksum")
                B_kmean = Buf("kmean")
                B_gate4 = [Buf("gate_sb%d" % i) for i in range(4)]
                B_top84 = [Buf("top8%d" % i) for i in range(4)]
                B_mask4 = [Buf("mask_tm%d" % i) for i in range(4)]

                for h in range(8):
                    op("sp" if h % 2 == 0 else "pool",
                       lambda e, h=h: e.dma_start(out=K_aug[64:84, h, :], in_=d_kaug[:, :]),
                       writes=[B_const], dma=True)
                for p in range(4):
                    op("dve", lambda e, p=p: e.memset(V_ext[:, :, p * 192 + 64:p * 192 + 128], 1.0),
                       writes=[B_const])
                for q in range(2):
                    op("dve", lambda e, q=q: e.memset(Q_aug2[q][64:80, :, :], 0.0), writes=[B_const])
                for i in range(4):
                    op("dve", lambda e, i=i: e.memset(gate_sb4[i][:, :, :], NEG), writes=[B_const])
                    op("dve", lambda e, i=i: e.memset(mask_tm4[i][:, :, :], 0.0), writes=[B_const])
                op("dve", lambda e: e.memset(ksum[:, :, :], 0.0), writes=[B_const])
                S_.barrier()

                ctr = {"pj": 0}

                def nextpj():
                    ctr["pj"] += 1
                    return ctr["pj"] % 2

                def preamble_units(c):
                    par = c % 2
                    c0 = c * 512
                    hnT = hnT2[par]
                    B_hnT = B_hnT2[par]
                    Q_aug = Q_aug2[par]
                    units = []

                    def u_load1(tt):
                        T = 4 * c + tt
                        b = T % 2
                        load_norm(src, T, xt[0], B_xt[0], hn, B_hn, stat[b], B_stat[b])

                    def u_load2(tt):
                        transpose_hn(hn, B_hn, hnT, B_hnT[tt], tt)

                    def u_qc():
                        op("sp", lambda e: e.dma_start(out=Q_aug[80:84, :, :], in_=d_qaug[:, :, c0:c0 + 512]),
                           writes=[B_qc2[par]], dma=True)

                    def u_q(p):
                        pb = nextpj()
                        for kc in range(8):
                            op("pe", lambda e, kc=kc: e.matmul(
                                pj[pb][:, :], lhsT=W_M[:, kc, p * 128:(p + 1) * 128], rhs=hnT[:, kc, :],
                                start=(kc == 0), stop=(kc == 7)),
                               reads=[B_WM] + B_hnT, writes=[B_pj[pb]])
                        for hh in range(2):
                            op("dve", lambda e, hh=hh: e.tensor_copy(out=Q_aug[0:64, 2 * p + hh, :],
                                                                     in_=pj[pb][hh * 64:(hh + 1) * 64, :]),
                               reads=[B_pj[pb]], writes=[B_qq2[par][2 * p + hh]])

                    def u_k(p):
                        pb = nextpj()
                        for kc in range(8):
                            op("pe", lambda e, kc=kc: e.matmul(
                                pj[pb][:, :], lhsT=W_M[:, kc, 512 + p * 128:512 + (p + 1) * 128], rhs=hnT[:, kc, :],
                                start=(kc == 0), stop=(kc == 7)),
                               reads=[B_WM] + B_hnT, writes=[B_pj[pb]])
                        for hh in range(2):
                            h = 2 * p + hh
                            op("dve", lambda e, hh=hh, h=h: e.tensor_copy(out=K_aug[0:64, h, c0:c0 + 512],
                                                                          in_=pj[pb][hh * 64:(hh + 1) * 64, :]),
                               reads=[B_pj[pb]], writes=[B_ka[h][c]])
                            op("dve", lambda e, hh=hh, h=h: e.tensor_reduce(
                                out=ksum[0:64, h, 2 * c:2 * c + 2],
                                in_=pj[pb][hh * 64:(hh + 1) * 64, :].rearrange("p (b t) -> p b t", b=2),
                                axis=mybir.AxisListType.X, op=ALU.add),
                               reads=[B_pj[pb]], writes=[B_ksum])

                    def u_v(tt):
                        T = 4 * c + tt
                        pb = nextpj()
                        for kc in range(8):
                            op("pe", lambda e, kc=kc: e.matmul(
                                pj[pb][:, :], lhsT=hnT[:, kc, tt * 128:(tt + 1) * 128], rhs=W_M[:, kc, 1024:1536],
                                start=(kc == 0), stop=(kc == 7)),
                               reads=[B_WM, B_hnT[tt]], writes=[B_pj[pb]])
                        vsrc = pj[pb][:, :].rearrange("p (q two e) -> p q two e", two=2, e=64)
                        vdst = V_ext[:, T, :].rearrange("p (q c) -> p q c", c=192)
                        op("dve", lambda e: e.tensor_copy(out=vdst[:, :, 0:64], in_=vsrc[:, :, 0, :]),
                           reads=[B_pj[pb]], writes=[B_ve[T]])
                        op("dve", lambda e: e.tensor_copy(out=vdst[:, :, 128:192], in_=vsrc[:, :, 1, :]),
                           reads=[B_pj[pb]], writes=[B_ve[T]])

                    def u_kmean():
                        op("dve", lambda e: e.tensor_scalar(out=kmean[:, :, 2 * c:2 * c + 2], in0=ksum[:, :, 2 * c:2 * c + 2],
                                                            scalar1=1.0 / 256.0, scalar2=None, op0=ALU.mult),
                           reads=[B_ksum], writes=[B_kmean])

                    def u_gsilu(p):
                        pb = nextpj()
                        for kc in range(8):
                            op("pe", lambda e, kc=kc: e.matmul(
                                pj[pb][:, :], lhsT=W_M[:, kc, 1536 + p * 128:1536 + (p + 1) * 128], rhs=hnT[:, kc, :],
                                start=(kc == 0), stop=(kc == 7)),
                               reads=[B_WM] + B_hnT, writes=[B_pj[pb]])
                        op("dve", lambda e: e.tensor_copy(out=gx[:, :], in_=pj[pb][:, :]),
                           reads=[B_pj[pb]], writes=[B_gx])
                        sigmoid_recip(pj[pb][:, :], B_pj[pb], gtmp, B_gtmp)
                        op("dve", lambda e: e.tensor_tensor(out=gsl2[par][:, p, :], in0=gx[:, :],
                                                            in1=gtmp[:, :], op=ALU.mult),
                           reads=[B_gx, B_gtmp], writes=[B_gsl2[par][p]])

                    def u_gating(tt):
                        T = 4 * c + tt
                        qb = T // 2
                        gate_sb, top8, mask_tm = gate_sb4[tt], top84[tt], mask_tm4[tt]
                        pb = nextpj()
                        for h in range(8):
                            op("pe", lambda e, h=h: e.matmul(
                                pj[pb][:, h * 16:h * 16 + qb], lhsT=Q_aug[0:64, h, tt * 128:(tt + 1) * 128],
                                rhs=kmean[0:64, h, 0:qb], start=True, stop=True),
                               reads=[B_qq2[par][h], B_kmean], writes=[B_pj[pb]])
                        gsrc = pj[pb][:, 0:128].rearrange("p (h n) -> p h n", n=16)
                        op("dve", lambda e: e.tensor_copy(out=gate_sb[:, :, 0:qb], in_=gsrc[:, :, 0:qb]),
                           reads=[B_pj[pb]], writes=[B_gate4[tt]])
                        for h in range(8):
                            op("dve", lambda e, h=h: e.max(out=top8[:, h, :], in_=gate_sb[:, h, :]),
                               reads=[B_gate4[tt]], writes=[B_top84[tt]])
                        op("dve", lambda e: e.tensor_tensor(
                            out=mask_tm[:, :, 0:qb], in0=gate_sb[:, :, 0:qb],
                            in1=top8[:, :, 2:3].broadcast_to([128, 8, qb]), op=ALU.is_lt),
                           reads=[B_gate4[tt], B_top84[tt]], writes=[B_mask4[tt]])

                    def u_gating2(tt):
                        mask_tm = mask_tm4[tt]
                        for h in range(8):
                            op("pe", lambda e, h=h: e.transpose(out=tps[0:16, h * 128:(h + 1) * 128],
                                                                in_=mask_tm[:, h, :], identity=ident[:, :]),
                               reads=[B_mask4[tt]], writes=[B_tps])
                        op("act", lambda e: e.activation(
                            out=Q_aug[64:80, :, tt * 128:(tt + 1) * 128],
                            in_=tps[0:16, :].rearrange("p (h t) -> p h t", h=8), func=AF.Copy),
                           reads=[B_tps], writes=[B_qm2[par][tt]])

                    for tt in range(4):
                        units.append(lambda tt=tt: u_load1(tt))
                        units.append(lambda tt=tt: u_load2(tt))
                    units.append(u_qc)
                    for p in range(4):
                        units.append(lambda p=p: u_k(p))
                    units.append(u_kmean)
                    for p in range(4):
                        units.append(lambda p=p: u_q(p))
                    for tt in range(4):
                        units.append(lambda tt=tt: u_v(tt))
                    gs_units = [(lambda p=p: u_gsilu(p)) for p in range(4)]
                    if c >= 2:
                        units.append(lambda: u_gating(0))
                        for tt in range(4):
                            if tt + 1 < 4:
                                units.append(lambda tt=tt: u_gating(tt + 1))
                            units.append(gs_units[tt])
                            units.append(lambda tt=tt: u_gating2(tt))
                    else:
                        units.extend(gs_units)
                    return units

                def attention(c, pending):
                    par = c % 2
                    c0 = c * 512
                    Q_aug = Q_aug2[par]
                    npast = 4 * c
                    tiles = []
                    for h in range(8):
                        for kt in range(npast):
                            tiles.append((h, kt, None))
                        for k in range(4):
                            tiles.append((h, 4 * c + k, k))
                    ntl = len(tiles)
                    per_head = npast + 4

                    def emit_qk(i):
                        h, kt, k = tiles[i]
                        sbi = i % 3
                        qreads = [B_qq2[par][h], B_qc2[par]] + B_qm2[par]
                        if k is None:
                            op("pe", lambda e: e.matmul(
                                st[sbi][:, :], lhsT=K_aug[0:84, h, kt * 128:(kt + 1) * 128], rhs=Q_aug[0:84, h, :],
                                start=True, stop=True),
                               reads=[B_ka[h][kt // 4]] + qreads, writes=[B_st[sbi]])
                        else:
                            n = (4 - k) * 128
                            q0 = k * 128
                            op("pe", lambda e: e.matmul(
                                st[sbi][:, 0:128], lhsT=K_aug[0:84, h, kt * 128:(kt + 1) * 128],
                                rhs=Q_aug[0:84, h, q0:q0 + 128], start=True, stop=False),
                               reads=[B_ka[h][c]] + qreads, writes=[B_st[sbi]])
                            op("pe", lambda e: e.matmul(
                                st[sbi][:, 0:128], lhsT=ident[:, :], rhs=causal[:, :], start=False, stop=True),
                               reads=[], writes=[B_st[sbi]])
                            if n > 128:
                                op("pe", lambda e: e.matmul(
                                    st[sbi][:, 128:n], lhsT=K_aug[0:84, h, kt * 128:(kt + 1) * 128],
                                    rhs=Q_aug[0:84, h, q0 + 128:512], start=True, stop=True),
                                   reads=[B_ka[h][c]] + qreads, writes=[B_st[sbi]])

                    def emit_exp(i):
                        h, kt, k = tiles[i]
                        sbi = i % 3
                        pbi = i % 3
                        n = 512 if k is None else (4 - k) * 128
                        op("act", lambda e: e.activation(out=pT[pbi][:, 0:n], in_=st[sbi][:, 0:n],
                                                         func=AF.Exp, scale=SCALE),
                           reads=[B_st[sbi]], writes=[B_pT[pbi]])

                    def emit_pv(i):
                        h, kt, k = tiles[i]
                        p, odd = h // 2, h % 2
                        vcol = p * 192 + (64 if odd else 0)
                        ab = h % 2
                        pbi = i % 3
                        first = (i % per_head == 0)
                        n = 512 if k is None else (4 - k) * 128
                        q0 = 0 if k is None else k * 128
                        op("pe", lambda e: e.matmul(
                            acc[ab][:, q0:512], lhsT=V_ext[:, kt, vcol:vcol + 128], rhs=pT[pbi][:, 0:n],
                            start=first, stop=(k == 3)),
                           reads=[B_ve[kt], B_pT[pbi]], writes=[B_acc[ab]])
                        if k == 3:
                            num = slice(64, 128) if odd else slice(0, 64)
                            den = slice(0, 64) if odd else slice(64, 128)
                            yb = p % 2
                            if c <= 3:
                                op("act", lambda e: e.activation(out=rec[den, :], in_=acc[ab][den, :], func=AF.Ln),
                                   reads=[B_acc[ab]], writes=[B_rec])
                                op("act", lambda e: e.activation(out=rec[den, :], in_=rec[den, :], func=AF.Exp, scale=-1.0),
                                   reads=[B_rec], writes=[B_rec])
                            else:
                                op("dve", lambda e: e.reciprocal(out=rec[den, :], in_=acc[ab][den, :]),
                                   reads=[B_acc[ab]], writes=[B_rec])
                            op("dve", lambda e: e.tensor_tensor(
                                out=tmp[num, :], in0=acc[ab][num, :], in1=rec[den, :], op=ALU.mult),
                               reads=[B_acc[ab], B_rec], writes=[B_tmp])
                            op("pool", lambda e: e.tensor_tensor(
                                out=ymt[yb][num, :], in0=tmp[num, :], in1=gsl2[par][num, p, :], op=ALU.mult),
                               reads=[B_tmp, B_gsl2[par][p]], writes=[B_ymt[yb]])
                            if odd:
                                op("sp", lambda e: e.dma_start(out=ym_d[p, :, c0:c0 + 512], in_=ymt[yb][:, :]),
                                   reads=[B_ymt[yb]], dma=True)

                    nsteps = ntl + 2
                    npend = len(pending)
                    done = 0
                    for i in range(nsteps):
                        if i < ntl:
                            emit_qk(i)
                        if 1 <= i <= ntl:
                            emit_exp(i - 1)
                        if i >= 2:
                            emit_pv(i - 2)
                        want = (npend * (i + 1)) // nsteps
                        while done < want:
                            pending[done]()
                            done += 1
                    while done < npend:
                        pending[done]()
                        done += 1

                for u in preamble_units(0):
                    u()
                for c in range(NCH):
                    nxt = preamble_units(c + 1) if c + 1 < NCH else []
                    attention(c, nxt)
                S_.barrier()

            with ExitStack() as ph:
                W_AB = sb(ph, "W_AB", [128, 8, 1792], BF16)
                Wo = sb(ph, "Wo", [128, 8, D], BF16)
                pw = sb(ph, "pw", [128, 2, 256], BF16)
                diag = sb(ph, "diag", [128, 2, 31, 128], BF16)
                cw_t = sb(ph, "cw_t", [128, 2, 31], F32)
                cv_t = sb(ph, "cv_t", [128, 4, 2], F32)
                dec = sb(ph, "dec", [128, 4, 128], F32)
                zeta = sb(ph, "zeta", [128, 4, 1], F32)
                xi = sb(ph, "xi", [128, 2, 128], F32)
                gv = sb(ph, "gv", [128, 2, 1], F32)
                B_WAB = Buf("W_AB")
                B_c2 = Buf("c2")
                B_cw = Buf("cw")
                with ExitStack() as wl:
                    op("sp", lambda e: e.dma_start(out=cw_t[:, :, :], in_=cwT[l, :, :, :]), writes=[B_cw], dma=True)
                    op("sp", lambda e: e.dma_start(out=cv_t[:, :, :], in_=cvec[l, :, :, :]), writes=[B_c2], dma=True)
                    op("sp", lambda e: e.dma_start(out=dec[:, :, :], in_=d_dec[:, :, :]), writes=[B_c2], dma=True)
                    op("sp", lambda e: e.dma_start(out=zeta[:, :, :], in_=d_zeta[:, :, :]), writes=[B_c2], dma=True)
                    op("sp", lambda e: e.dma_start(out=xi[:, :, :], in_=d_xi[:, :, 0:128]), writes=[B_c2], dma=True)
                    op("sp", lambda e: e.dma_start(out=gv[:, :, :], in_=d_gv[:, :, :]), writes=[B_c2], dma=True)
                    B_diag = Buf("diag")
                    dlist = [(g, k) for g in range(2) for k in range(31)]

                    def diag_some(kc):
                        for idx, (g, k) in enumerate(dlist[kc * 8:(kc + 1) * 8]):
                            if idx % 2 == 0:
                                op("dve", lambda e, g=g, k=k: e.tensor_scalar(out=diag[:, g, k, :], in0=ident[:, :],
                                                                              scalar1=cw_t[:, g, k:k + 1], scalar2=None, op0=ALU.mult),
                                   reads=[B_cw], writes=[B_diag])
                            else:
                                op("act", lambda e, g=g, k=k: e.activation(out=diag[:, g, k, :], in_=ident[:, :],
                                                                           func=AF.Copy, scale=cw_t[:, g, k:k + 1]),
                                   reads=[B_cw], writes=[B_diag])

                    load_weight_cols(wl, l, 0, 1792, W_AB, B_WAB, "ab", extra=diag_some)
                    stg2 = [sb(wl, "stgo%d" % i, [128, 2048], F32) for i in range(2)]
                    B_stg2 = [Buf("stgo0"), Buf("stgo1")]
                    for j in range(4):
                        bq = j % 2
                        for hf in range(2):
                            kc = 2 * j + hf
                            op("sp" if hf == 0 else "pool",
                               lambda e, kc=kc, bq=bq, hf=hf: e.dma_start(out=stg2[bq][:, hf * 1024:(hf + 1) * 1024],
                                                                         in_=w_out[l, kc * 128:(kc + 1) * 128, :]),
                               writes=[B_stg2[bq]], dma=True)
                        if j % 2 == 0:
                            op("dve", lambda e, j=j, bq=bq: e.tensor_copy(
                                out=Wo[:, 2 * j:2 * j + 2, :], in_=stg2[bq][:, :].rearrange("p (a n) -> p a n", a=2)),
                               reads=[B_stg2[bq]], writes=[B_c2])
                        else:
                            op("act", lambda e, j=j, bq=bq: e.activation(
                                out=Wo[:, 2 * j:2 * j + 2, :], in_=stg2[bq][:, :].rearrange("p (a n) -> p a n", a=2), func=AF.Copy),
                               reads=[B_stg2[bq]], writes=[B_c2])
                    for cc in range(2):
                        op("sp", lambda e, cc=cc: e.dma_start(out=stg2[0][:, cc * 256:(cc + 1) * 256],
                                                              in_=pw_w[l, cc * 128:(cc + 1) * 128, :]),
                           writes=[B_stg2[0]], dma=True)
                    op("dve", lambda e: e.tensor_copy(out=pw[:, :, :], in_=stg2[0][:, 0:512].rearrange("p (a n) -> p a n", a=2)),
                       reads=[B_stg2[0]], writes=[B_c2])
                    S_.barrier()
                fgt = None
                if last:
                    fgt = sb(ph, "fgt", [128, D], F32)
                    op("sp", lambda e: e.dma_start(out=fgt[:, :], in_=fg[:, :]), writes=[B_c2], dma=True)
                xt2 = [[sb(ph, "xa%d_%d" % (q, i), [128, D], F32) for i in range(4)] for q in range(2)]
                B_xt2 = [[Buf("xa%d_%d" % (q, i)) for i in range(4)] for q in range(2)]
                stat2 = [[sb(ph, "stata%d_%d" % (q, i), [128, 4], F32) for i in range(4)] for q in range(2)]
                B_stat2 = [[Buf("stata%d_%d" % (q, i)) for i in range(4)] for q in range(2)]
                fstat = [sb(ph, "fstat%d" % i, [128, 4], F32) for i in range(4)]
                B_fstat = [Buf("fstat%d" % i) for i in range(4)]
                fjunk = sb(ph, "fjunk", [128, D], BF16)
                B_fjunk = Buf("fjunk")
                u_ext2 = [sb(ph, "u_ext%d" % q, [128, 2, 544], BF16) for q in range(2)]
                B_u2 = [[Buf("u%d_%d" % (q, g)) for g in range(2)] for q in range(2)]
                qz2 = [sb(ph, "qz%d" % q, [128, 2, 2, 512], BF16) for q in range(2)]
                qx2 = [sb(ph, "qx%d" % q, [128, 2, 512], BF16) for q in range(2)]
                k2 = [sb(ph, "kbf%d" % q, [128, 2, 512], BF16) for q in range(2)]
                gsr2 = [sb(ph, "gsr%d" % q, [128, 2, 512], F32) for q in range(2)]
                kz2 = [sb(ph, "kzp%d" % q, [128, 4, 2, 192], BF16) for q in range(2)]
                vp2 = [sb(ph, "vpp%d" % q, [128, 4, 2, 192], BF16) for q in range(2)]
                gsA2 = [[sb(ph, "gsA%d_%d" % (q, co), [128, 512], F32) for co in range(2)] for q in range(2)]
                B_q2 = [[Buf("q%d_%d" % (q, i)) for i in range(2)] for q in range(2)]
                B_qx2 = [[Buf("qx%d_%d" % (q, i)) for i in range(2)] for q in range(2)]
                B_k2 = [[Buf("k%d_%d" % (q, i)) for i in range(2)] for q in range(2)]
                B_gsr2 = [[Buf("gsr%d_%d" % (q, i)) for i in range(2)] for q in range(2)]
                B_kz2 = [[Buf("kz%d_%d" % (q, i)) for i in range(4)] for q in range(2)]
                B_vp2 = [[Buf("vp%d_%d" % (q, i)) for i in range(4)] for q in range(2)]
                B_gsA2 = [[Buf("gsA%d_%d" % (q, i)) for i in range(2)] for q in range(2)]
                K = {"pji": 0, "sti": 0, "acci": 0, "ptri": 0}
                ymc = [sb(ph, "ymc%d" % i, [128, 4, 512], BF16) for i in range(2)]
                B_ymc = [Buf("ymc0"), Buf("ymc1")]
                hn = sb(ph, "hna", [128, D], BF16)
                hnT = sb(ph, "hnTa", [128, 8, 512], BF16)
                f32t = [sb(ph, "f%d" % i, [128, 512], F32) for i in range(7)]
                B_f = [Buf("f%d" % i) for i in range(7)]
                y32 = sb(ph, "y32", [128, 2, 512], F32)
                ybf = sb(ph, "ybf", [128, 2, 512], BF16)
                ysq = sb(ph, "ysq", [128, 2, 512], BF16)
                s_bf = sb(ph, "s_bf", [128, 2, 512], BF16)
                cat = sb(ph, "cat", [128, 4, 512], BF16)
                pTr = [sb(ph, "pTr%d" % i, [128, 4, 128], BF16) for i in range(4)]
                Rpad4 = [sb(ph, "Rpad4_%d" % i, [128, 2, 128], BF16) for i in range(4)]
                B_Rp4 = [[Buf("Rp4_%d_%d" % (i, r)) for r in range(2)] for i in range(4)]
                Rm = sb(ph, "Rm", [128, 2, 64], F32)
                obf = sb(ph, "obf", [128, 512], BF16)
                osq = sb(ph, "osq", [128, 512], BF16)

                B_hn = Buf("hna")
                B_hnT = [Buf("hnTa%d" % i) for i in range(4)]
                B_y32 = [Buf("y32_0"), Buf("y32_1")]
                B_ybf = [Buf("ybf0"), Buf("ybf1")]
                B_ysq = [Buf("ysq0"), Buf("ysq1")]
                B_s = [Buf("s0"), Buf("s1")]
                B_cat = [Buf("cat%d" % i) for i in range(4)]
                B_pTr = [Buf("pTr%d" % i) for i in range(4)]
                B_Rm = [Buf("Rm0"), Buf("Rm1")]
                B_Rp = [Buf("Rp0"), Buf("Rp1")]
                B_obf = Buf("obf")
                B_osq = Buf("osq")

                for q in range(2):
                    op("pool", lambda e, q=q: e.memset(u_ext2[q][:, :, :], 0.0), writes=B_u2[q])
                    op("dve", lambda e, q=q: e.memset(qz2[q][:, :, :, :], 0.0), writes=B_q2[q])
                    op("pool", lambda e, q=q: e.memset(kz2[q][:, :, :, :], 0.0), writes=B_kz2[q])
                    op("dve", lambda e, q=q: e.memset(vp2[q][:, :, :, :], 0.0), writes=B_vp2[q])
                op("dve", lambda e: e.memset(Rm[:, :, :], 0.0), writes=B_Rm)
                for i in range(4):
                    op("dve", lambda e, i=i: e.memset(Rpad4[i][:, :, :], 0.0), writes=B_Rp4[i])
                S_.barrier()


                def proj_fm(col, pb):
                    for kc in range(8):
                        op("pe", lambda e, kc=kc: e.matmul(
                            pj[pb][:, :], lhsT=W_AB[:, kc, col:col + 128], rhs=hnT[:, kc, :],
                            start=(kc == 0), stop=(kc == 7)),
                           reads=[B_WAB] + B_hnT, writes=[B_pj[pb]])

                def rstd_from(var_t, B_var):
                    op("act", lambda e: e.activation(out=var_t[:, :], in_=var_t[:, :], func=AF.Ln,
                                                     bias=eps_rms[:, 1:2]),
                       reads=[B_var], writes=[B_var])
                    op("act", lambda e: e.activation(out=var_t[:, :], in_=var_t[:, :], func=AF.Exp, scale=-0.5),
                       reads=[B_var], writes=[B_var])

                def sigmoid_act(psrc, B_psrc, tmpt, B_tmp):
                    op("act", lambda e: e.activation(out=tmpt[:, :], in_=psrc, func=AF.Exp, scale=-1.0),
                       reads=[B_psrc], writes=[B_tmp])
                    op("act", lambda e: e.activation(out=tmpt[:, :], in_=tmpt[:, :], func=AF.Ln, bias=eps_rms[:, 2:3]),
                       reads=[B_tmp], writes=[B_tmp])
                    op("act", lambda e: e.activation(out=tmpt[:, :], in_=tmpt[:, :], func=AF.Exp, scale=-1.0),
                       reads=[B_tmp], writes=[B_tmp])

                def s1(c):
                    P = c % 2
                    c0 = c * 512
                    xt, B_xt, stat, B_stat = xt2[P], B_xt2[P], stat2[P], B_stat2[P]
                    u_ext, B_u = u_ext2[P], B_u2[P]
                    u_prev, B_uprev = u_ext2[1 - P], B_u2[1 - P]
                    qz, qx_bf, k_bf, gs_r, kzpad, vpad = qz2[P], qx2[P], k2[P], gsr2[P], kz2[P], vp2[P]
                    B_q, B_qx, B_k, B_gsr, B_kz, B_vp = B_q2[P], B_qx2[P], B_k2[P], B_gsr2[P], B_kz2[P], B_vp2[P]
                    gsA, B_gsA = gsA2[P], B_gsA2[P]
                    c0 = c * 512
                    for p in range(4):
                        op("sp", lambda e, p=p, c=c, c0=c0: e.dma_start(out=ymc[c % 2][:, p, :], in_=ym_d[p, :, c0:c0 + 512]),
                           writes=[B_ymc[c % 2]], dma=True)
                        yield
                    for tt in range(4):
                        T = 4 * c + tt
                        load_norm_transpose(src, T, xt[tt], B_xt[tt], hn, B_hn, stat[tt], B_stat[tt],
                                            hnT, B_hnT[tt], tt)
                        yield
                    for g in range(2):
                        if c > 0:
                            op("pool", lambda e, g=g: e.tensor_copy(out=u_ext[:, g, 0:30], in_=u_prev[:, g, 512:542]),
                               reads=[B_uprev[g]], writes=[B_u[g]])
                        pbv = K['pji'] % 2
                        K['pji'] += 1
                        proj_fm(g * 128, pbv)
                        sbi = K['sti'] % 3
                        K['sti'] += 1
                        for kc in range(8):
                            op("pe", lambda e, kc=kc, g=g, sbi=sbi: e.matmul(
                                st[sbi][:, :], lhsT=W_AB[:, kc, 256 + g * 128:256 + (g + 1) * 128], rhs=hnT[:, kc, :],
                                start=(kc == 0), stop=(kc == 7)),
                               reads=[B_WAB] + B_hnT, writes=[B_st[sbi]])
                        op("dve", lambda e, pbv=pbv: e.tensor_copy(out=f32t[5][:, :], in_=pj[pbv][:, :]),
                           reads=[B_pj[pbv]], writes=[B_f[5]])
                        sigmoid_act(st[sbi][:, :], B_st[sbi], f32t[0], B_f[0])
                        op("dve", lambda e, g=g: e.tensor_tensor(out=u_ext[:, g, 30:542], in0=f32t[5][:, :],
                                                                 in1=f32t[0][:, :], op=ALU.mult),
                           reads=[B_f[5], B_f[0]], writes=[B_u[g]])
                        yield
                    for rp in range(2):
                        pb = K['pji'] % 2
                        K['pji'] += 1
                        proj_fm(768 + rp * 128, pb)
                        for hh in range(2):
                            op("act", lambda e, rp=rp, pb=pb, hh=hh: e.activation(
                                out=qz[hh * 64:(hh + 1) * 64, rp, hh, :], in_=pj[pb][hh * 64:(hh + 1) * 64, :], func=AF.Copy),
                               reads=[B_pj[pb]], writes=[B_q[rp]])
                        op("dve", lambda e, rp=rp, pb=pb: e.tensor_tensor(
                            out=qx_bf[:, rp, :].rearrange("p (a t) -> p a t", a=4),
                            in0=pj[pb][:, :].rearrange("p (a t) -> p a t", a=4),
                            in1=xi[:, rp:rp + 1, :].broadcast_to([128, 4, 128]), op=ALU.mult),
                           reads=[B_pj[pb]], writes=[B_qx[rp]])
                        sbi = K['sti'] % 3
                        K['sti'] += 1
                        for kc in range(8):
                            op("pe", lambda e, kc=kc, rp=rp, sbi=sbi: e.matmul(
                                st[sbi][:, :], lhsT=W_AB[:, kc, 1024 + rp * 128:1024 + (rp + 1) * 128], rhs=hnT[:, kc, :],
                                start=(kc == 0), stop=(kc == 7)),
                               reads=[B_WAB] + B_hnT, writes=[B_st[sbi]])
                        op("dve", lambda e, rp=rp, sbi=sbi: e.tensor_copy(out=k_bf[:, rp, :], in_=st[sbi][:, :]),
                           reads=[B_st[sbi]], writes=[B_k[rp]])
                        yield
                    for tt in range(4):
                        pb = K['pji'] % 2
                        K['pji'] += 1
                        for kc in range(8):
                            op("pe", lambda e, kc=kc, tt=tt, pb=pb: e.matmul(
                                pj[pb][:, :], lhsT=hnT[:, kc, tt * 128:(tt + 1) * 128], rhs=W_AB[:, kc, 1024:1536],
                                start=(kc == 0), stop=(kc == 7)),
                               reads=[B_WAB, B_hnT[tt]], writes=[B_pj[pb]])
                        ksrc = pj[pb][:, 0:256].rearrange("p (q two e) -> p q two e", two=2, e=64)
                        vsrc = pj[pb][:, 256:512].rearrange("p (q two e) -> p q two e", two=2, e=64)
                        zsrc = zeta[:, :, :].rearrange("p (q two) o -> p q two o", two=2)
                        for two in range(2):
                            op("dve", lambda e, tt=tt, two=two, ksrc=ksrc, zsrc=zsrc: e.tensor_tensor(
                                out=kzpad[:, tt, :, two * 128:two * 128 + 64], in0=ksrc[:, :, two, :],
                                in1=zsrc[:, :, two, :].broadcast_to([128, 2, 64]), op=ALU.mult),
                               reads=[B_pj[pb]], writes=[B_kz[tt]])
                            op("dve", lambda e, tt=tt, two=two, vsrc=vsrc: e.tensor_copy(
                                out=vpad[:, tt, :, two * 128:two * 128 + 64], in_=vsrc[:, :, two, :]),
                               reads=[B_pj[pb]], writes=[B_vp[tt]])
                        yield
                    for co in range(2):
                        sbi = K['sti'] % 3
                        K['sti'] += 1
                        for kc in range(8):
                            op("pe", lambda e, kc=kc, co=co, sbi=sbi: e.matmul(
                                st[sbi][:, :], lhsT=W_AB[:, kc, 512 + co * 128:512 + (co + 1) * 128], rhs=hnT[:, kc, :],
                                start=(kc == 0), stop=(kc == 7)),
                               reads=[B_WAB] + B_hnT, writes=[B_st[sbi]])
                        gs, B_gs = gsA[co], B_gsA[co]
                        op("act", lambda e, sbi=sbi, gs=gs: e.activation(out=gs[:, :], in_=st[sbi][:, :], func=AF.Copy),
                           reads=[B_st[sbi]], writes=[B_gs])
                        sigmoid_act(st[sbi][:, :], B_st[sbi], f32t[0], B_f[0])
                        op("dve", lambda e, gs=gs: e.tensor_tensor(out=gs[:, :], in0=gs[:, :], in1=f32t[0][:, :],
                                                                   op=ALU.mult),
                           reads=[B_f[0], B_gs], writes=[B_gs])
                        yield
                    for rp in range(2):
                        sbi = K['sti'] % 3
                        K['sti'] += 1
                        for kc in range(8):
                            op("pe", lambda e, kc=kc, rp=rp, sbi=sbi: e.matmul(
                                st[sbi][:, :], lhsT=W_AB[:, kc, 1536 + rp * 128:1536 + (rp + 1) * 128], rhs=hnT[:, kc, :],
                                start=(kc == 0), stop=(kc == 7)),
                               reads=[B_WAB] + B_hnT, writes=[B_st[sbi]])
                        op("act", lambda e, rp=rp, sbi=sbi: e.activation(out=gs_r[:, rp, :], in_=st[sbi][:, :], func=AF.Copy),
                           reads=[B_st[sbi]], writes=[B_gsr[rp]])
                        sigmoid_act(st[sbi][:, :], B_st[sbi], f32t[6], B_f[6])
                        op("dve", lambda e, rp=rp: e.tensor_tensor(out=gs_r[:, rp, :], in0=gs_r[:, rp, :],
                                                                   in1=f32t[6][:, :], op=ALU.mult),
                           reads=[B_f[6]], writes=[B_gsr[rp]])
                        yield
                def s2(c):
                    P = c % 2
                    c0 = c * 512
                    xt, B_xt, stat, B_stat = xt2[P], B_xt2[P], stat2[P], B_stat2[P]
                    u_ext, B_u = u_ext2[P], B_u2[P]
                    u_prev, B_uprev = u_ext2[1 - P], B_u2[1 - P]
                    qz, qx_bf, k_bf, gs_r, kzpad, vpad = qz2[P], qx2[P], k2[P], gsr2[P], kz2[P], vp2[P]
                    B_q, B_qx, B_k, B_gsr, B_kz, B_vp = B_q2[P], B_qx2[P], B_k2[P], B_gsr2[P], B_kz2[P], B_vp2[P]
                    gsA, B_gsA = gsA2[P], B_gsA2[P]
                    for g in range(2):
                        ab = K['acci'] % 2
                        K['acci'] += 1
                        for k in range(31):
                            op("pe", lambda e, g=g, k=k, ab=ab: e.matmul(
                                acc[ab][:, :], lhsT=diag[:, g, k, :], rhs=u_ext[:, g, k:k + 512],
                                start=(k == 0), stop=(k == 30)),
                               reads=[B_u[g]], writes=[B_acc[ab]])
                        op("dve", lambda e, g=g, ab=ab: e.tensor_scalar(out=y32[:, g, :], in0=acc[ab][:, :],
                                                                        scalar1=cv_t[:, 0, g:g + 1], scalar2=None, op0=ALU.add),
                           reads=[B_acc[ab]], writes=[B_y32[g]])
                        op("act", lambda e, g=g: e.activation(out=ybf[:, g, :], in_=y32[:, g, :], func=AF.Copy),
                           reads=[B_y32[g]], writes=[B_ybf[g]])
                        op("act", lambda e, g=g: e.activation(out=ysq[:, g, :], in_=y32[:, g, :], func=AF.Square),
                           reads=[B_y32[g]], writes=[B_ysq[g]])
                        yield
                    mb = K['pji'] % 2
                    K['pji'] += 1
                    for g in range(2):
                        op("pe", lambda e, g=g, mb=mb: e.matmul(pj[mb][:, :], lhsT=o256[:, :], rhs=ybf[:, g, :],
                                                               start=(g == 0), stop=(g == 1)),
                           reads=[B_ybf[g]], writes=[B_pj[mb]])
                        yield
                    vb = K['pji'] % 2
                    K['pji'] += 1
                    for g in range(2):
                        op("pe", lambda e, g=g, vb=vb: e.matmul(pj[vb][:, :], lhsT=o256[:, :], rhs=ysq[:, g, :],
                                                               start=(g == 0), stop=(g == 1)),
                           reads=[B_ysq[g]], writes=[B_pj[vb]])
                        yield
                    mu, B_mu = f32t[1], B_f[1]
                    var, B_var = f32t[2], B_f[2]
                    op("act", lambda e, mb=mb: e.activation(out=mu[:, :], in_=pj[mb][:, :], func=AF.Copy),
                       reads=[B_pj[mb]], writes=[B_mu])
                    op("act", lambda e, mb=mb: e.activation(out=var[:, :], in_=pj[mb][:, :], func=AF.Square),
                       reads=[B_pj[mb]], writes=[B_var])
                    op("dve", lambda e, vb=vb: e.tensor_tensor(out=var[:, :], in0=pj[vb][:, :], in1=var[:, :],
                                                               op=ALU.subtract),
                       reads=[B_pj[vb], B_var], writes=[B_var])
                    rstd_from(var, B_var)
                    oacc = []
                    for rp in range(2):
                        oacc.append(K['acci'] % 2)
                        K['acci'] += 1
                    pis = []
                    for tt in range(4):
                        t0 = tt * 128
                        sbi = K['sti'] % 3
                        K['sti'] += 1
                        pi = tt
                        pis.append(pi)
                        for h in range(4):
                            rp, hh = h // 2, h % 2
                            op("pe", lambda e, h=h, rp=rp, hh=hh, t0=t0, sbi=sbi: e.matmul(
                                st[sbi][:, h * 128:(h + 1) * 128], lhsT=k_bf[:, rp, t0:t0 + 128],
                                rhs=qz[:, rp, hh, t0:t0 + 128], start=True, stop=True),
                               reads=[B_k[rp], B_q[rp]], writes=[B_st[sbi]])
                        op("dve", lambda e, sbi=sbi, pi=pi: e.tensor_tensor(
                            out=pTr[pi][:, :, :], in0=st[sbi][:, :].rearrange("p (h i) -> p h i", h=4),
                            in1=dec[:, :, :], op=ALU.mult),
                           reads=[B_st[sbi]], writes=[B_pTr[pi]])
                    yield
                    kb = K['pji'] % 2
                    K['pji'] += 1
                    firstkv = True
                    for tt in range(4):
                        for rp in range(2):
                            cb = (tt * 2 + rp) * 64
                            for hh in range(2):
                                op("pe", lambda e, rp=rp, hh=hh, tt=tt, kb=kb, cb=cb, firstkv=firstkv: e.matmul(
                                    pj[kb][:, cb:cb + 64], lhsT=kzpad[:, tt, rp, hh * 64:hh * 64 + 128],
                                    rhs=vpad[:, tt, rp, hh * 128:hh * 128 + 64], start=firstkv, stop=(tt == 3 and rp == 1 and hh == 1),
                                    skip_group_check=True),
                                   reads=[B_kz[tt], B_vp[tt]], writes=[B_pj[kb]])
                                firstkv = False
                    for tt in range(4):
                        t0 = tt * 128
                        for rp in range(2):
                            ab = oacc[rp]
                            for hh in range(2):
                                h = 2 * rp + hh
                                op("pe", lambda e, h=h, rp=rp, hh=hh, tt=tt, t0=t0, ab=ab: e.matmul(
                                    acc[ab][:, t0:t0 + 128], lhsT=vpad[:, tt, rp, hh * 64:hh * 64 + 128],
                                    rhs=pTr[tt][:, h, :], start=(tt == 0 and hh == 0), stop=False, skip_group_check=True),
                                   reads=[B_vp[tt], B_pTr[tt]], writes=[B_acc[ab]])
                    yield
                    for tt in range(4):
                        for rp in range(2):
                            cb = (tt * 2 + rp) * 64
                            op("dve", lambda e, rp=rp, tt=tt: e.tensor_copy(out=Rpad4[tt][0:64, rp, 0:64], in_=Rm[0:64, rp, :]),
                               reads=[B_Rm[rp]], writes=[B_Rp4[tt][rp]])
                            op("dve", lambda e, rp=rp, tt=tt: e.tensor_copy(out=Rpad4[tt][64:128, rp, 64:128], in_=Rm[64:128, rp, :]),
                               reads=[B_Rm[rp]], writes=[B_Rp4[tt][rp]])
                            op("dve", lambda e, rp=rp, kb=kb, cb=cb: e.scalar_tensor_tensor(
                                out=Rm[:, rp, :], in0=Rm[:, rp, :], scalar=gv[:, rp, 0:1], in1=pj[kb][:, cb:cb + 64],
                                op0=ALU.mult, op1=ALU.add),
                               reads=[B_pj[kb], B_Rm[rp]], writes=[B_Rm[rp]])
                    for tt in range(4):
                        t0 = tt * 128
                        for rp in range(2):
                            ab = oacc[rp]
                            op("pe", lambda e, rp=rp, t0=t0, ab=ab, tt=tt: e.matmul(
                                acc[ab][:, t0:t0 + 128], lhsT=Rpad4[tt][:, rp, :],
                                rhs=qx_bf[:, rp, t0:t0 + 128], start=False, stop=(tt == 3), skip_group_check=True),
                               reads=[B_Rp4[tt][rp], B_qx[rp]], writes=[B_acc[ab]])
                    yield
                    for g in range(2):
                        d1, B_d1 = f32t[3], B_f[3]
                        e1, B_e1 = f32t[4], B_f[4]
                        op("dve", lambda e, g=g: e.tensor_tensor(out=d1[:, :], in0=y32[:, g, :], in1=mu[:, :],
                                                                 op=ALU.subtract),
                           reads=[B_y32[g], B_mu], writes=[B_d1])
                        op("dve", lambda e: e.tensor_tensor(out=d1[:, :], in0=d1[:, :], in1=var[:, :], op=ALU.mult),
                           reads=[B_d1, B_var], writes=[B_d1])
                        op("dve", lambda e, g=g: e.tensor_scalar(out=d1[:, :], in0=d1[:, :],
                                                                 scalar1=cv_t[:, 1, g:g + 1], scalar2=cv_t[:, 2, g:g + 1],
                                                                 op0=ALU.mult, op1=ALU.add),
                           reads=[B_d1], writes=[B_d1])
                        sigmoid_act(d1[:, :], B_d1, e1, B_e1)
                        op("dve", lambda e, g=g: e.tensor_tensor(out=s_bf[:, g, :], in0=d1[:, :], in1=e1[:, :],
                                                                 op=ALU.mult),
                           reads=[B_d1, B_e1], writes=[B_s[g]])
                        yield
                    for co in range(2):
                        gs, B_gs = gsA[co], B_gsA[co]
                        ppb = K['pji'] % 2
                        K['pji'] += 1
                        for ci in range(2):
                            op("pe", lambda e, ci=ci, co=co, ppb=ppb: e.matmul(
                                pj[ppb][:, :], lhsT=pw[:, ci, co * 128:(co + 1) * 128], rhs=s_bf[:, ci, :],
                                start=(ci == 0), stop=(ci == 1)),
                               reads=[B_s[ci]], writes=[B_pj[ppb]])
                        op("dve", lambda e, co=co, ppb=ppb, gs=gs: e.scalar_tensor_tensor(
                            out=cat[:, co, :], in0=pj[ppb][:, :], scalar=cv_t[:, 3, co:co + 1], in1=gs[:, :],
                            op0=ALU.add, op1=ALU.mult),
                           reads=[B_pj[ppb], B_gs], writes=[B_cat[co]])
                        yield
                    for rp in range(2):
                        ab = oacc[rp]
                        op("act", lambda e, ab=ab: e.activation(out=obf[:, :], in_=acc[ab][:, :], func=AF.Copy),
                           reads=[B_acc[ab]], writes=[B_obf])
                        op("act", lambda e, ab=ab: e.activation(out=osq[:, :], in_=acc[ab][:, :], func=AF.Square),
                           reads=[B_acc[ab]], writes=[B_osq])
                        mb = K['pji'] % 2
                        K['pji'] += 1
                        op("pe", lambda e, mb=mb: e.matmul(pj[mb][:, :], lhsT=bones[:, :], rhs=obf[:, :], start=True, stop=True),
                           reads=[B_obf], writes=[B_pj[mb]])
                        vb = K['pji'] % 2
                        K['pji'] += 1
                        op("pe", lambda e, vb=vb: e.matmul(pj[vb][:, :], lhsT=bones[:, :], rhs=osq[:, :], start=True, stop=True),
                           reads=[B_osq], writes=[B_pj[vb]])
                        mu, B_mu = f32t[1], B_f[1]
                        var, B_var = f32t[2], B_f[2]
                        d1, B_d1 = f32t[3], B_f[3]
                        op("act", lambda e, mb=mb: e.activation(out=mu[:, :], in_=pj[mb][:, :], func=AF.Copy),
                           reads=[B_pj[mb]], writes=[B_mu])
                        op("act", lambda e, mb=mb: e.activation(out=var[:, :], in_=pj[mb][:, :], func=AF.Square),
                           reads=[B_pj[mb]], writes=[B_var])
                        op("dve", lambda e, vb=vb: e.tensor_tensor(out=var[:, :], in0=pj[vb][:, :], in1=var[:, :],
                                                                   op=ALU.subtract),
                           reads=[B_pj[vb], B_var], writes=[B_var])
                        rstd_from(var, B_var)
                        op("dve", lambda e, ab=ab: e.tensor_tensor(out=d1[:, :], in0=acc[ab][:, :], in1=mu[:, :],
                                                                   op=ALU.subtract),
                           reads=[B_acc[ab], B_mu], writes=[B_d1])
                        op("dve", lambda e: e.tensor_tensor(out=d1[:, :], in0=d1[:, :], in1=var[:, :], op=ALU.mult),
                           reads=[B_d1, B_var], writes=[B_d1])
                        op("dve", lambda e, rp=rp: e.tensor_tensor(out=cat[:, 2 + rp, :], in0=d1[:, :], in1=gs_r[:, rp, :],
                                                                   op=ALU.mult),
                           reads=[B_d1, B_gsr[rp]], writes=[B_cat[2 + rp]])
                        yield
                    for tt in range(4):
                        T = 4 * c + tt
                        t0 = tt * 128
                        xb_i = tt
                        for half in range(2):
                            sbi = K['sti'] % 3
                            K['sti'] += 1
                            for kc in range(8):
                                if kc < 4:
                                    lt = cat[:, kc, t0:t0 + 128]
                                    rd = [B_cat[kc]]
                                else:
                                    lt = ymc[c % 2][:, kc - 4, t0:t0 + 128]
                                    rd = [B_ymc[c % 2]]
                                op("pe", lambda e, kc=kc, lt=lt, half=half, sbi=sbi: e.matmul(
                                    st[sbi][:, :], lhsT=lt, rhs=Wo[:, kc, half * 512:(half + 1) * 512],
                                    start=(kc == 0), stop=(kc == 7)),
                                   reads=rd, writes=[B_st[sbi]])
                            op("dve", lambda e, tt=tt, half=half, sbi=sbi, xb_i=xb_i: e.tensor_tensor(
                                out=xt[xb_i][:, half * 512:(half + 1) * 512], in0=st[sbi][:, :],
                                in1=xt[tt][:, half * 512:(half + 1) * 512], op=ALU.add),
                               reads=[B_st[sbi], B_xt[tt]], writes=[B_xt[xb_i]])
                        if not last:
                            op("sp", lambda e, T=T, xb_i=xb_i: e.dma_start(out=x1[T * 128:(T + 1) * 128, :], in_=xt[xb_i][:, :]),
                               reads=[B_xt[xb_i]], dma=True)
                        else:
                            stt = fstat[tt]
                            op("act", lambda e, xb_i=xb_i, stt=stt: e.activation(out=fjunk[:, :], in_=xt[xb_i][:, :], func=AF.Square,
                                                                                 accum_out=stt[:, 0:1]),
                               reads=[B_xt[xb_i]], writes=[B_fjunk, B_fstat[tt]])
                            op("act", lambda e, stt=stt: e.activation(out=stt[:, 1:2], in_=stt[:, 0:1], func=AF.Ln,
                                                                      scale=1.0 / D, bias=eps_rms[:, 0:1]),
                               reads=[B_fstat[tt]], writes=[B_fstat[tt]])
                            op("act", lambda e, stt=stt: e.activation(out=stt[:, 2:3], in_=stt[:, 1:2], func=AF.Exp, scale=-0.5),
                               reads=[B_fstat[tt]], writes=[B_fstat[tt]])
                            op("dve", lambda e, xb_i=xb_i, stt=stt: e.scalar_tensor_tensor(
                                out=xt[xb_i][:, :], in0=xt[xb_i][:, :], scalar=stt[:, 2:3], in1=fgt[:, :],
                                op0=ALU.mult, op1=ALU.mult),
                               reads=[B_xt[xb_i], B_fstat[tt]], writes=[B_xt[xb_i]])
                            op("sp", lambda e, T=T, xb_i=xb_i: e.dma_start(out=out[T * 128:(T + 1) * 128, :], in_=xt[xb_i][:, :]),
                               reads=[B_xt[xb_i]], dma=True)

                        yield

                def drain(g):
                    for _ in g:
                        pass

                drain(s1(0))
                for c in range(NCH):
                    g2 = s2(c)
                    g1 = s1(c + 1) if c + 1 < NCH else iter(())
                    a2 = a1 = True
                    while a2 or a1:
                        if a2:
                            a2 = next(g2, "END") != "END"
                        if a1:
                            a1 = next(g1, "END") != "END"
                S_.barrier()
        S_.barrier()
        print("KERNEL nops", S_.nops, {k: e["count"] for k, e in S_.engs.items()})
    return nc


def _bf(a):
    return np.ascontiguousarray(a.astype(ml_dtypes.bfloat16))


def make_consts(S):
    c = {}
    c["c_ident"] = _bf(np.eye(128, dtype=np.float32))
    j = np.arange(128)
    c["c_causal"] = _bf(np.where(j[:, None] <= j[None, :], 0.0, MASKV).astype(np.float32))
    pos = np.arange(S)
    ka = np.zeros((20, S), np.float32)
    for n in range(16):
        ka[n] = np.where(pos // 256 == n, MASKV, 0.0)
    ka[16] = pos % 128
    ka[17] = pos // 128
    ka[18] = 1.0
    ka[19] = 1.0
    c["c_kaug"] = _bf(ka)
    qa = np.zeros((4, 8, S), np.float32)
    for h in range(8):
        slope = 2.0 ** (-(h + 1))
        qa[0, h] = slope * 8.0
        qa[1, h] = slope * 1024.0
        qa[2, h] = -slope * 8.0 * (pos % 128)
        qa[3, h] = -slope * 1024.0 * (pos // 128)
    c["c_qaug"] = _bf(qa)
    hh = np.arange(4, dtype=np.float64)
    g = 1.0 - np.exp2(-5.0 - hh)
    i = np.arange(128, dtype=np.float64)
    diff = i[None, :] - i[:, None]
    dec = np.zeros((128, 4, 128), np.float64)
    for h in range(4):
        dec[:, h, :] = np.where(diff >= 0, g[h] ** np.maximum(diff, 0.0), 0.0) * 0.125
    c["c_dec"] = dec.astype(np.float32)
    zeta = np.zeros((128, 4, 1), np.float64)
    for h in range(4):
        zeta[:, h, 0] = g[h] ** (127 - i) * 0.125
    c["c_zeta"] = zeta.astype(np.float32)
    xi = np.zeros((128, 2, 512), np.float64)
    gv = np.zeros((128, 2, 1), np.float64)
    ii = np.arange(512) % 128
    for rp in range(2):
        for hf in range(2):
            h = 2 * rp + hf
            xi[hf * 64:(hf + 1) * 64, rp, :] = (g[h] ** (ii + 1.0))[None, :]
            gv[hf * 64:(hf + 1) * 64, rp, 0] = g[h] ** 128
    c["c_xi"] = xi.astype(np.float32)
    c["c_gv"] = gv.astype(np.float32)
    bo = np.zeros((128, 128), np.float32)
    bo[0:64, 0:64] = 1.0 / 64
    bo[64:128, 64:128] = 1.0 / 64
    c["c_bones"] = _bf(bo)
    c["c_o256"] = _bf(np.full((128, 128), 1.0 / 256, np.float32))
    return c


def prep_shared(norm_g, w_in, conv_w, conv_b, conv_ln_g, conv_ln_b, conv_pw_w, conv_pw_b, w_out, final_g, S):
    DEPTH = w_in.shape[0]
    f = lambda a: np.ascontiguousarray(np.asarray(a, dtype=np.float32))
    m = {}
    m["w_in"] = f(w_in)
    m["w_out"] = f(w_out)
    m["pw_w"] = f(conv_pw_w)
    m["ng"] = f(np.asarray(norm_g).reshape(DEPTH, 8, 128).transpose(0, 2, 1))
    m["cwT"] = f(np.asarray(conv_w).transpose(0, 2, 1).reshape(DEPTH, 2, 128, 31).transpose(0, 2, 1, 3))
    cv = np.stack([np.asarray(conv_b), np.asarray(conv_ln_g), np.asarray(conv_ln_b), np.asarray(conv_pw_b)], axis=1)
    m["cvec"] = f(cv.reshape(DEPTH, 4, 2, 128).transpose(0, 3, 1, 2))
    m["fg"] = f(np.broadcast_to(np.asarray(final_g)[None, :], (128, D)))
    m.update(make_consts(S))
    return m


_CACHE = {}


def run(x, shared, S, DEPTH):
    B = x.shape[0]
    key = (S, DEPTH)
    if key not in _CACHE:
        _CACHE[key] = build_program(S, DEPTH)
    nc = _CACHE[key]
    in_maps = []
    for b in range(B):
        d = dict(shared)
        d["x"] = np.ascontiguousarray(x[b])
        in_maps.append(d)
    res = run_bass_kernel_spmd(nc, in_maps, core_ids=list(range(B)))
    return np.stack([np.asarray(r["out"]) for r in res.results], axis=0).astype(np.float32)


def kernel(x, norm_g, w_in, conv_w, conv_b, conv_ln_g, conv_ln_b, conv_pw_w, conv_pw_b, w_out, final_g):
    x = np.asarray(x, dtype=np.float32)
    S = x.shape[1]
    DEPTH = np.asarray(w_in).shape[0]
    shared = prep_shared(norm_g, w_in, conv_w, conv_b, conv_ln_g, conv_ln_b, conv_pw_w, conv_pw_b,
                         w_out, final_g, S)
    return run(x, shared, S, DEPTH)
```
